# Optimizing a Trainium2 kernel written in Bass

```python
import math
import jax
import jax.numpy as jnp
from jax import lax
import numpy as np

D_MODEL = 2048
BATCH = 8
SEQ = 4096
DEPTH = 2

D_MIX = D_MODEL
RET_HEADS = 6
RET_WIDTH = 3 * D_MIX // 8
RET_HEAD_DIM = RET_WIDTH // RET_HEADS
RET_CHUNK = 128
ROPE_BASE = 10000.0
DIFF_HEADS = 6
DIFF_WIDTH = 3 * D_MIX // 8
DIFF_V_DIM = DIFF_WIDTH // DIFF_HEADS
DIFF_QK_DIM = DIFF_V_DIM // 2
ATTN_BLOCK = 128
GMLP_GROUPS = 4
GMLP_WIDTH = D_MIX - RET_WIDTH - DIFF_WIDTH
GMLP_GROUP_DIM = GMLP_WIDTH // GMLP_GROUPS
GMLP_CHUNK = 128
IN_WIDTHS = (RET_WIDTH,) * 4 + (DIFF_WIDTH,) * 3 + (GMLP_WIDTH,) * 2
IN_SPLITS = tuple(int(s) for s in np.cumsum(IN_WIDTHS)[:-1])
D_IN = sum(IN_WIDTHS)
N_GROUPS = 4
EXPERTS_PER_GROUP = 8
N_EXPERTS = N_GROUPS * EXPERTS_PER_GROUP
TOP_K = 2
EXPERT_HIDDEN = D_MODEL // 2
MOE_BLOCK = 128
N_MOD = 6
EPS = 1e-6

kernel_name = 'hybrid_retention_diffattn_gmlp_hmoe'


def _rmsnorm(x, w):
    xf = x.astype(jnp.float32)
    y = xf * lax.rsqrt(jnp.mean(xf * xf, axis=-1, keepdims=True) + EPS)
    return (y * w.astype(jnp.float32)).astype(x.dtype)


def _layernorm(x, w, b=None):
    xf = x.astype(jnp.float32)
    mu = jnp.mean(xf, axis=-1, keepdims=True)
    xc = xf - mu
    y = xc * lax.rsqrt(jnp.mean(xc * xc, axis=-1, keepdims=True) + EPS) * w.astype(jnp.float32)
    if b is not None:
        y = y + b.astype(jnp.float32)
    return y.astype(x.dtype)


def _rotary(x, pos):
    half = x.shape[-1] // 2
    inv = ROPE_BASE ** (-jnp.arange(half, dtype=jnp.float32) / half)
    ang = pos.astype(jnp.float32)[:, None] * inv[None, :]
    cos = jnp.cos(ang)[None, :, None, :]
    sin = jnp.sin(ang)[None, :, None, :]
    xf = x.astype(jnp.float32)
    x1, x2 = xf[..., :half], xf[..., half:]
    return jnp.concatenate([x1 * cos - x2 * sin, x2 * cos + x1 * sin], axis=-1).astype(x.dtype)


def _chunkwise_retention(q, k, v):
    B, S, H, dk = q.shape
    dv = v.shape[-1]
    C = RET_CHUNK
    NC = S // C
    dt = q.dtype

    def to_chunks(t):
        return t.reshape(B, NC, C, H, t.shape[-1]).transpose(0, 3, 1, 2, 4)

    qc, kc, vc = to_chunks(q), to_chunks(k), to_chunks(v)
    log_g = jnp.log(1.0 - jnp.power(2.0, -5.0 - jnp.arange(H, dtype=jnp.float32)))
    idx = jnp.arange(C, dtype=jnp.float32)
    rel = idx[:, None] - idx[None, :]
    decay = jnp.where(rel[None] >= 0,
                      jnp.exp(jnp.maximum(rel, 0.0)[None] * log_g[:, None, None]),
                      0.0).astype(dt)
    scores = jnp.einsum('bhncd,bhnmd->bhncm', qc, kc) * decay[None, :, None]
    y_inner = jnp.einsum('bhncm,bhnme->bhnce', scores, vc)
    zeta = jnp.exp((C - 1.0 - idx)[None, :] * log_g[:, None]).astype(dt)
    kv = jnp.einsum('bhncd,bhnce->nbhde', kc * zeta[None, :, None, :, None], vc)
    chunk_decay = jnp.exp(C * log_g).astype(dt)[None, :, None, None]

    def step(state, kv_n):
        return state * chunk_decay + kv_n, state

    _, states = lax.scan(step, jnp.zeros((B, H, dk, dv), dt), kv)
    xi = jnp.exp((idx + 1.0)[None, :] * log_g[:, None]).astype(dt)
    y_cross = jnp.einsum('bhncd,nbhde->bhnce', qc, states) * xi[None, :, None, :, None]
    y = y_inner + y_cross
    return y.transpose(0, 2, 3, 1, 4).reshape(B, S, H, dv)


def _retention(q, k, v, g, norm_w):
    B, S, _ = q.shape
    H, d = RET_HEADS, RET_HEAD_DIM
    pos = jnp.arange(S)
    q = _rotary(q.reshape(B, S, H, d), pos)
    k = _rotary(k.reshape(B, S, H, d), pos) * (d ** -0.5)
    v = v.reshape(B, S, H, d)
    y = _chunkwise_retention(q, k, v)
    y = _layernorm(y, norm_w.reshape(H, d))
    return jax.nn.silu(g) * y.reshape(B, S, H * d)


def _diff_attention(q, k, v, q_norm_w, k_norm_w, lam_params, subln_w, lam_init):
    B, S, _ = q.shape
    H, dh, dv = DIFF_HEADS, DIFF_QK_DIM, DIFF_V_DIM
    q = _rmsnorm(q.reshape(B, S, H, 2, dh), q_norm_w).transpose(0, 2, 3, 1, 4)
    k = _rmsnorm(k.reshape(B, S, H, 2, dh), k_norm_w).transpose(0, 2, 3, 1, 4)
    v = v.reshape(B, S, H, dv).transpose(0, 2, 1, 3)
    lp = lam_params.astype(jnp.float32)
    lam = jnp.exp(jnp.sum(lp[0] * lp[1])) - jnp.exp(jnp.sum(lp[2] * lp[3])) + lam_init
    scale = dh ** -0.5
    outs = []
    for i in range(S // ATTN_BLOCK):
        q0 = i * ATTN_BLOCK
        L = q0 + ATTN_BLOCK
        s = jnp.einsum('bhjqd,bhjkd->bhjqk', q[:, :, :, q0:L], k[:, :, :, :L]).astype(jnp.float32) * scale
        mask = jnp.arange(L)[None, :] <= (q0 + jnp.arange(ATTN_BLOCK))[:, None]
        p = jax.nn.softmax(jnp.where(mask, s, -jnp.inf), axis=-1)
        a = (p[:, :, 0] - lam * p[:, :, 1]).astype(v.dtype)
        outs.append(jnp.einsum('bhqk,bhke->bhqe', a, v[:, :, :L]))
    o = jnp.concatenate(outs, axis=2)
    o = _rmsnorm(o, subln_w) * (1.0 - lam_init)
    return o.transpose(0, 2, 1, 3).reshape(B, S, H * dv)


def _gmlp(u, v, norm_w, norm_b, ws, bs):
    B, S, _ = u.shape
    G, cg, C = GMLP_GROUPS, GMLP_GROUP_DIM, GMLP_CHUNK
    NC = S // C
    u = jax.nn.gelu(u, approximate=False)
    v = _layernorm(jax.nn.gelu(v, approximate=False), norm_w, norm_b)
    v = v.reshape(B, NC, C, G, cg)
    w = jnp.where(jnp.tril(jnp.ones((C, C), dtype=bool))[None], ws, 0.0).astype(v.dtype)
    f = jnp.einsum('gts,bnsgc->bntgc', w, v) + bs.T[None, None, :, :, None]
    return u * f.reshape(B, S, G * cg)


def _hier_moe(h, wg, bg, we, be, w_gu, w_dn):
    T, D = h.shape
    gl = (h @ wg + bg).astype(jnp.float32)
    g_idx = jnp.argmax(gl, axis=-1)
    p_g = jnp.take_along_axis(jax.nn.softmax(gl, axis=-1), g_idx[:, None], axis=-1)
    el = (h @ we + be).astype(jnp.float32).reshape(T, N_GROUPS, EXPERTS_PER_GROUP)
    el = jnp.take_along_axis(el, g_idx[:, None, None], axis=1)[:, 0]
    top_v, top_i = lax.top_k(el, TOP_K)
    gate = (p_g * jax.nn.softmax(top_v, axis=-1)).reshape(-1)
    eid = (g_idx[:, None] * EXPERTS_PER_GROUP + top_i).reshape(-1).astype(jnp.int32)
    tok = jnp.repeat(jnp.arange(T, dtype=jnp.int32), TOP_K)
    A = T * TOP_K
    order = jnp.argsort(eid)
    e_s, tok_s, gate_s = eid[order], tok[order], gate[order]
    counts = jnp.bincount(eid, length=N_EXPERTS)
    starts = jnp.cumsum(counts) - counts
    padded = (counts + MOE_BLOCK - 1) // MOE_BLOCK * MOE_BLOCK
    pends = jnp.cumsum(padded)
    pstarts = pends - padded
    dest = pstarts[e_s] + jnp.arange(A, dtype=jnp.int32) - starts[e_s]
    NB = -(-A // MOE_BLOCK) + N_EXPERTS
    P = NB * MOE_BLOCK
    slot_tok = jnp.full((P,), T, jnp.int32).at[dest].set(tok_s)
    slot_gate = jnp.zeros((P,), h.dtype).at[dest].set(gate_s.astype(h.dtype))
    blk_exp = jnp.minimum(jnp.searchsorted(pends, jnp.arange(NB, dtype=jnp.int32) * MOE_BLOCK, side='right'),
                          N_EXPERTS - 1)
    h_pad = jnp.concatenate([h, jnp.zeros((1, D), h.dtype)], axis=0)
    xb = h_pad[slot_tok].reshape(NB, MOE_BLOCK, D)

    def expert_block(args):
        xblk, e = args
        a, b = jnp.split(xblk @ w_gu[e], 2, axis=-1)
        return (jax.nn.silu(a) * b) @ w_dn[e]

    yb = lax.map(expert_block, (xb, blk_exp)).reshape(P, D)
    out = jnp.zeros((T + 1, D), h.dtype).at[slot_tok].add(yb * slot_gate[:, None])
    return out[:T]


def setup_inputs(seed: int = 0) -> dict:
    key = jax.random.key(seed)
    ks = jax.random.split(key, 24)
    L, D = DEPTH, D_MODEL

    def nrm(k, shape, s):
        return jax.random.normal(k, shape, jnp.float32) * s

    def gain(k, shape):
        return 1.0 + nrm(k, shape, 0.01)

    return {
        'x': nrm(ks[0], (BATCH, SEQ, D), 1.0),
        'c': nrm(ks[1], (BATCH, D), 1.0),
        'ada_w': nrm(ks[2], (L, D, N_MOD * D), 0.5 * D ** -0.5),
        'ada_b': nrm(ks[3], (L, N_MOD * D), 0.01),
        'mix_norm_w': gain(ks[4], (L, D)),
        'w_in': nrm(ks[5], (L, D, D_IN), D ** -0.5),
        'w_out': nrm(ks[6], (L, D_MIX, D), D_MIX ** -0.5),
        'ret_norm_w': gain(ks[7], (L, RET_WIDTH)),
        'diff_q_norm_w': gain(ks[8], (L, DIFF_QK_DIM)),
        'diff_k_norm_w': gain(ks[9], (L, DIFF_QK_DIM)),
        'diff_lambda': nrm(ks[10], (L, 4, DIFF_QK_DIM), 0.1),
        'diff_subln_w': gain(ks[11], (L, DIFF_V_DIM)),
        'gmlp_norm_w': gain(ks[12], (L, GMLP_WIDTH)),
        'gmlp_norm_b': nrm(ks[13], (L, GMLP_WIDTH), 0.01),
        'gmlp_ws': nrm(ks[14], (L, GMLP_GROUPS, GMLP_CHUNK, GMLP_CHUNK), GMLP_CHUNK ** -0.5),
        'gmlp_bs': gain(ks[15], (L, GMLP_GROUPS, GMLP_CHUNK)),
        'ffn_norm_w': gain(ks[16], (L, D)),
        'router_group_w': nrm(ks[17], (L, D, N_GROUPS), D ** -0.5),
        'router_group_b': nrm(ks[18], (L, N_GROUPS), 0.01),
        'router_expert_w': nrm(ks[19], (L, D, N_EXPERTS), D ** -0.5),
        'router_expert_b': nrm(ks[20], (L, N_EXPERTS), 0.01),
        'expert_w_gate_up': nrm(ks[21], (L, N_EXPERTS, D, 2 * EXPERT_HIDDEN), D ** -0.5),
        'expert_w_down': nrm(ks[22], (L, N_EXPERTS, EXPERT_HIDDEN, D), EXPERT_HIDDEN ** -0.5),
    }


def reference(x, c, ada_w, ada_b, mix_norm_w, w_in, w_out, ret_norm_w, diff_q_norm_w, diff_k_norm_w,
              diff_lambda, diff_subln_w, gmlp_norm_w, gmlp_norm_b, gmlp_ws, gmlp_bs, ffn_norm_w,
              router_group_w, router_group_b, router_expert_w, router_expert_b,
              expert_w_gate_up, expert_w_down):
    B, S, D = x.shape
    c_act = jax.nn.silu(c)
    for l in range(DEPTH):
        mod = c_act @ ada_w[l] + ada_b[l]
        sh1, sc1, g1, sh2, sc2, g2 = jnp.split(mod, N_MOD, axis=-1)
        h = _rmsnorm(x, mix_norm_w[l]) * (1.0 + sc1[:, None]) + sh1[:, None]
        proj = h @ w_in[l]
        rq, rk, rv, rg, dq, dk, dv, gu, gv = jnp.split(proj, IN_SPLITS, axis=-1)
        y_ret = _retention(rq, rk, rv, rg, ret_norm_w[l])
        lam_init = 0.8 - 0.6 * math.exp(-0.3 * l)
        y_diff = _diff_attention(dq, dk, dv, diff_q_norm_w[l], diff_k_norm_w[l], diff_lambda[l],
                                 diff_subln_w[l], lam_init)
        y_gm = _gmlp(gu, gv, gmlp_norm_w[l], gmlp_norm_b[l], gmlp_ws[l], gmlp_bs[l])
        mix = jnp.concatenate([y_ret, y_diff, y_gm], axis=-1) @ w_out[l]
        x = x + g1[:, None] * mix
        h2 = _rmsnorm(x, ffn_norm_w[l]) * (1.0 + sc2[:, None]) + sh2[:, None]
        y = _hier_moe(h2.reshape(B * S, D), router_group_w[l], router_group_b[l], router_expert_w[l],
                      router_expert_b[l], expert_w_gate_up[l], expert_w_down[l]).reshape(B, S, D)
        x = x + g2[:, None] * y
    return x
```

```python
import numpy as np
from contextlib import ExitStack
import concourse.bass as bass
import concourse.mybir as mybir
from concourse.bass_utils import run_bass_kernel_spmd

F32 = mybir.dt.float32
F32R = mybir.dt.float32r
BF16 = mybir.dt.bfloat16
I32 = mybir.dt.int32
U32 = mybir.dt.uint32
AF = mybir.ActivationFunctionType
ALU = mybir.AluOpType
AX = mybir.AxisListType


class Sem:
    _n = 0

    def __init__(self, nc, name):
        self.h = nc.alloc_semaphore(name=name)
        self.cnt = 0
        Sem._n += 1
        self.key = Sem._n


class Res:
    __slots__ = ("name", "w", "r", "dsem")

    def __init__(self, name=""):
        self.name = name
        self.w = {}
        self.r = {}
        self.dsem = None


def _merge(d, ev):
    s, v = ev
    o = d.get(s.key)
    if o is None or o[1] < v:
        d[s.key] = ev


class Prog:
    ENGS = ("sp", "pe", "act", "dve", "pool")
    ATTR = {"sp": "sync", "pe": "tensor", "act": "scalar", "dve": "vector", "pool": "gpsimd"}

    def __init__(self, nc):
        self.nc = nc
        self.q = {e: [] for e in self.ENGS}
        self.esem = {e: Sem(nc, "es_" + e) for e in ("pe", "act", "dve", "pool")}
        self.waited = {}
        self.dfree = []
        self.dall = []
        self.phase_dres = []
        self.ninst = 0

    def _wait(self, E, evs, skip_own=False):
        own = self.esem.get(E)
        for k, (s, v) in evs.items():
            if skip_own and s is own:
                continue
            kk = (E, k)
            if self.waited.get(kk, 0) >= v:
                continue
            self.waited[kk] = v
            self.q[E].append(("w", s, v))

    @staticmethod
    def _deps(reads, writes, wadd=()):
        evs = {}
        for r in reads:
            for ev in r.w.values():
                _merge(evs, ev)
        for w in writes:
            for ev in w.w.values():
                _merge(evs, ev)
            for ev in w.r.values():
                _merge(evs, ev)
        for w in wadd:
            for ev in w.r.values():
                _merge(evs, ev)
        return evs

    def op(self, E, fn, reads=(), writes=(), wadd=(), skip_own=None):
        if skip_own is None:
            skip_own = (E == "pe")
        self._wait(E, self._deps(reads, writes, wadd), skip_own)
        s = self.esem[E]
        s.cnt += 1
        self.q[E].append(("i", fn, s, 1))
        ev = (s, s.cnt)
        for r in reads:
            _merge(r.r, ev)
        for w in writes:
            w.w = {s.key: ev}
            w.r = {}
        for w in wadd:
            _merge(w.w, ev)
        self.ninst += 1
        return ev

    def new_epoch(self, engines, res):
        evs = self._deps((), (res,))
        for E in engines:
            self._wait(E, evs, skip_own=False)
        res.w = {}
        res.r = {}

    def _get_dsem(self):
        if self.dfree:
            return self.dfree.pop()
        s = Sem(self.nc, "ds%d" % len(self.dall))
        self.dall.append(s)
        return s

    def dma(self, Q, out, in_, reads=(), writes=(), wadd=(), dres=None, **kw):
        assert dres is not None
        self._wait(Q, self._deps(reads, writes, wadd))
        if dres.dsem is None:
            dres.dsem = self._get_dsem()
            self.phase_dres.append(dres)
        s = dres.dsem
        s.cnt += 16
        nc = self.nc

        def fn(eng, out=out, in_=in_, kw=kw):
            return eng.dma_start(out=out, in_=in_, **kw)
        self.q[Q].append(("i", fn, s, 16))
        ev = (s, s.cnt)
        for r in reads:
            _merge(r.r, ev)
        for w in writes:
            w.w = {s.key: ev}
            w.r = {}
        for w in wadd:
            _merge(w.w, ev)
        self.ninst += 1
        return ev

    def raw(self, Q, fn, dres, reads=(), writes=(), wadd=(), inc=16):
        self._wait(Q, self._deps(reads, writes, wadd))
        if dres.dsem is None:
            dres.dsem = self._get_dsem()
            self.phase_dres.append(dres)
        s = dres.dsem
        s.cnt += inc
        self.q[Q].append(("i", fn, s, inc))
        ev = (s, s.cnt)
        for r in reads:
            _merge(r.r, ev)
        for w in writes:
            w.w = {s.key: ev}
            w.r = {}
        for w in wadd:
            _merge(w.w, ev)
        return ev

    def end_phase(self, drain_on="sp"):
        for r in self.phase_dres:
            s = r.dsem
            kk = (drain_on, s.key)
            if self.waited.get(kk, 0) < s.cnt:
                self.waited[kk] = s.cnt
                self.q[drain_on].append(("w", s, s.cnt))
        self.flush()
        for r in self.phase_dres:
            self.dfree.append(r.dsem)
            r.dsem = None
            r.w = {}
            r.r = {}
        self.phase_dres = []
        allsems = list(self.esem.values()) + self.dall
        for E in self.ENGS:
            for s in allsems:
                self.waited[(E, s.key)] = s.cnt

    def flush(self):
        with self.nc.Block() as block:
            for E in self.ENGS:
                items = self.q[E]
                if not items:
                    continue

                def body(eng, items=items):
                    for it in items:
                        if it[0] == "w":
                            eng.wait_ge(it[1].h, it[2])
                        else:
                            inst = it[1](eng)
                            inst.then_inc(it[2].h, it[3])
                getattr(block, self.ATTR[E])(body)
                self.q[E] = []


D = 2048
DIN = 6400
NMOD = 6 * D
EPS = 1e-6
NEXP = 32
EH = 1024


def host_consts(S):
    c = {}
    c["ident"] = np.eye(128, dtype=np.float32)
    PT = np.zeros((128, 128), np.float32)
    for m in range(64):
        PT[m + 64, m] = -1.0
        PT[m, m + 64] = 1.0
    c["PT"] = PT
    blk = np.zeros((128, 128), np.float32)
    blk[:64, :64] = 1.0 / 64
    blk[64:, 64:] = 1.0 / 64
    c["blk64"] = blk
    half = 64
    inv = 10000.0 ** (-np.arange(half, dtype=np.float32) / half)
    pos = np.arange(S, dtype=np.float32)
    ang = pos[None, :] * inv[:, None]
    cos = np.cos(ang).astype(np.float32)
    sin = np.sin(ang).astype(np.float32)
    cosT = np.concatenate([cos, cos], 0)
    sinT = np.concatenate([sin, sin], 0)
    ks = np.float32(128 ** -0.5)
    c["rot"] = np.stack([cosT, sinT, cosT * ks, sinT * ks], 0).astype(np.float32)
    gam = 1.0 - 2.0 ** (-5.0 - np.arange(6, dtype=np.float64))
    idx = np.arange(128, dtype=np.float64)
    rel = idx[None, :] - idx[:, None]
    dec = np.where(rel[None] >= 0, gam[:, None, None] ** np.maximum(rel, 0)[None], 0.0)
    c["decayT"] = np.ascontiguousarray(dec.transpose(1, 0, 2)).astype(np.float32)
    c["zeta"] = (gam[None, :] ** (127.0 - idx)[:, None]).astype(np.float32)
    xi = gam[:, None] ** (idx + 1.0)[None, :]
    xi512 = np.tile(xi, (1, 4))
    c["xitab"] = np.ascontiguousarray(np.broadcast_to(xi512[None], (128, 6, 512))).astype(np.float32)
    c["cdec"] = [float(g ** 128.0) for g in gam]
    c["maskT"] = (idx[None, :] >= idx[:, None]).astype(np.float32)
    c["tril"] = (idx[None, :] <= idx[:, None]).astype(np.float32)
    import ml_dtypes
    c["identb"] = np.eye(128).astype(ml_dtypes.bfloat16)
    return c


class Ctx:
    pass


def bc(ap_row, n=None):
    if n is None:
        n = 1
        for d in ap_row.shape:
            n *= d
    return bass.AP(ap_row.tensor, ap_row.offset, [[0, 128], [1, n]])


_uid = [0]


def _nm(n):
    _uid[0] += 1
    return "%s_u%d" % (n, _uid[0])


def phase_mod(P, nc, es, g):
    sb = lambda n, s, d: es.enter_context(nc.sbuf_tensor(_nm(n), s, d))
    c_sb = sb("c_sb", [128, 16], F32)
    c_act = sb("c_act", [128, 16], F32)
    ones = sb("ones_a", [128, 128], F32)
    c_rep = sb("c_rep", [128, 16, 128], F32R)
    wsl = [sb("adaw%d" % i, [128, 16, 512], F32R) for i in range(2)]
    bia = [sb("adab%d" % i, [1, 512], F32) for i in range(2)]
    row = [sb("adar%d" % i, [1, 512], F32) for i in range(2)]
    ps = [es.enter_context(nc.psum_tensor(_nm("adaps"), [128, 512], F32)) for i in range(2)]
    r_c, r_ca, r_ones, r_rep = Res("c"), Res("ca"), Res("ones"), Res("rep")
    r_w = [Res("w0"), Res("w1")]
    r_b = [Res("b0"), Res("b1")]
    r_row = [Res("r0"), Res("r1")]
    r_ps = [Res("p0"), Res("p1")]

    P.dma("sp", c_sb[:], g.c.rearrange("(p j) -> p j", j=16), writes=[r_c], dres=r_c)
    P.op("act", lambda e: e.activation(out=c_act[:], in_=c_sb[:], func=AF.Silu), reads=[r_c], writes=[r_ca])
    P.op("dve", lambda e: e.memset(ones[:], 1.0), writes=[r_ones])
    for j in range(16):
        P.op("act", lambda e, j=j: e.activation(out=c_rep[:, j, :], in_=ones[:], func=AF.Copy,
                                                scale=c_act[:, j:j + 1]),
             reads=[r_ca, r_ones], wadd=[r_rep])
    k = 0
    for l in range(2):
        awv = g.ada_w[l].rearrange("(p j) n -> p j n", j=16).bitcast(F32R)
        for nb in range(NMOD // 512):
            s = k % 2
            k += 1
            P.dma("sp", wsl[s][:], awv[:, :, nb * 512:(nb + 1) * 512], writes=[r_w[s]], dres=r_w[s])
            P.dma("sp", bia[s][:], g.ada_b[l:l + 1, nb * 512:(nb + 1) * 512], writes=[r_b[s]], dres=r_b[s])
            for j in range(16):
                P.op("pe", lambda e, j=j, s=s: e.matmul(ps[s][:], c_rep[:, j, :], wsl[s][:, j, :],
                                                        start=(j == 0), stop=(j == 15)),
                     reads=[r_rep, r_w[s]], writes=[r_ps[s]] if j == 0 else (), wadd=() if j == 0 else [r_ps[s]])
            P.op("dve", lambda e, s=s: e.tensor_tensor(out=row[s][:], in0=ps[s][0:1, :], in1=bia[s][:], op=ALU.add),
                 reads=[r_ps[s], r_b[s]], writes=[r_row[s]])
            P.dma("pool", g.mod_d[l:l + 1, nb * 512:(nb + 1) * 512], row[s][:], reads=[r_row[s]], dres=r_row[s])


def phase_proj(P, nc, es, g, l, S, x_src=None):
    if x_src is None:
        x_src = g.x
    sb = lambda n, s, d: es.enter_context(nc.sbuf_tensor(_nm(n), s, d))
    pst = lambda n: es.enter_context(nc.psum_tensor(_nm(n), [128, 512], F32))
    NG = S // 512
    ident = sb("ident", [128, 128], F32)
    PT = sb("PT", [128, 128], F32R)
    blk = sb("blk", [128, 128], F32R)
    nw = sb("nw", [128, 16], F32)
    scm = sb("scm", [128, 16], F32)
    a1 = sb("a1", [128, 16], F32)
    b1 = sb("b1", [128, 16], F32)
    wqk = sb("wqk", [128, 2], F32)
    xt = [sb("xt%d" % i, [128, D], F32) for i in range(2)]
    junk = sb("junk", [128, D], BF16)
    ss = [sb("ss%d" % i, [128, 1], F32) for i in range(2)]
    sq = [sb("sq%d" % i, [128, 1], F32) for i in range(2)]
    rstd = [sb("rstd%d" % i, [128, 1], F32) for i in range(2)]
    hT = [sb("hT%d" % i, [128, 16, 512], F32R) for i in range(2)]
    wsl = [sb("win%d" % i, [128, 16, 512], F32R) for i in range(2)]
    tabs = [sb("tab%d" % i, [128, 4, 512], F32) for i in range(2)]
    qraw = [sb("qraw%d" % i, [128, 512], F32R) for i in range(2)]
    sqf = [sb("sqf%d" % i, [128, 512], F32R) for i in range(2)]
    t1 = [sb("t1_%d" % i, [128, 512], F32) for i in range(2)]
    t2 = [sb("t2_%d" % i, [128, 512], F32) for i in range(2)]
    ofm = [sb("ofm%d" % i, [128, 512], BF16) for i in range(2)]
    otk = [sb("otk%d" % i, [128, 512], BF16) for i in range(2)]
    oxi = [sb("oxi%d" % i, [128, 512], BF16) for i in range(2)]
    xitab = sb("xitab", [128, 6, 512], F32)
    tp = [pst("tp%d" % i) for i in range(2)]
    acc = [pst("acc%d" % i) for i in range(2)]
    aux = [pst("aux%d" % i) for i in range(2)]

    R = lambda n: Res(n)
    r_const = R("const")
    r_ab = R("ab")
    r_xt = [R("xt0"), R("xt1")]
    r_ss = [R("ss0"), R("ss1")]
    r_sq = [R("sq0"), R("sq1")]
    r_rstd = [R("rs0"), R("rs1")]
    r_junk = R("junk")
    r_tp = [R("tp0"), R("tp1")]
    r_hT = [[[R("hT") for j in range(16)] for i in range(4)] for s in range(2)]
    r_w = [R("w0"), R("w1")]
    r_tab = [R("tab0"), R("tab1")]
    r_acc = [R("acc0"), R("acc1")]
    r_aux = [R("aux0"), R("aux1")]
    r_qraw = [R("q0"), R("q1")]
    r_sqf = [R("sf0"), R("sf1")]
    r_t1 = [R("t10"), R("t11")]
    r_t2 = [R("t20"), R("t21")]
    r_ofm = [R("of0"), R("of1")]
    r_otk = [R("ot0"), R("ot1")]
    r_oxi = [R("ox0"), R("ox1")]

    P.dma("sp", xitab[:], g.xitab, wadd=[r_const], dres=r_const)
    P.dma("sp", ident[:], g.ident, wadd=[r_const], dres=r_const)
    P.dma("sp", PT[:], g.PT.bitcast(F32R), wadd=[r_const], dres=r_const)
    P.dma("sp", blk[:], g.blk64.bitcast(F32R), wadd=[r_const], dres=r_const)
    P.dma("sp", nw[:], g.mix_norm_w[l].rearrange("(p j) -> p j", j=16), wadd=[r_const], dres=r_const)
    P.dma("sp", scm[:], g.mod_d[l, D:2 * D].rearrange("(p j) -> p j", j=16), wadd=[r_const], dres=r_const)
    P.dma("sp", b1[:], g.mod_d[l, 0:D].rearrange("(p j) -> p j", j=16), wadd=[r_const], dres=r_const)
    for half in range(2):
        P.dma("sp", wqk[half * 64:(half + 1) * 64, 0:1], g.diff_q_norm_w[l].rearrange("(p o) -> p o", o=1),
              wadd=[r_const], dres=r_const)
        P.dma("sp", wqk[half * 64:(half + 1) * 64, 1:2], g.diff_k_norm_w[l].rearrange("(p o) -> p o", o=1),
              wadd=[r_const], dres=r_const)
    P.op("dve", lambda e: e.scalar_tensor_tensor(out=a1[:], in0=scm[:], scalar=1.0, in1=nw[:],
                                                 op0=ALU.add, op1=ALU.mult),
         reads=[r_const], writes=[r_ab])

    if getattr(g, "dbg_stage", 99) < 1:
        return
    winv = g.w_in[l].rearrange("(p j) n -> p j n", j=16).bitcast(F32R)
    acts_tok = {3: (AF.Copy, AF.Copy), 4: (AF.Copy, AF.Silu), 5: (AF.Silu, AF.Silu),
                9: (AF.Copy, AF.Copy), 10: (AF.Copy, AF.Gelu), 11: (AF.Gelu, AF.Gelu), 12: (AF.Gelu,)}
    wk = 0
    ak = 0
    fk = 0
    tk = 0
    xk = 0
    tpk = 0
    for gi in range(NG):
        gs = gi % 2
        t0 = gi * 512
        import os
        SKIP = os.environ.get("MK_SKIP", "")
        if "rot" not in SKIP:
            P.dma("sp", tabs[gs][:], g.rot[:, :, t0:t0 + 512].rearrange("f p t -> p f t"),
                  writes=[r_tab[gs]], dres=r_tab[gs])
        for i in range(4):
            xs = xk % 2
            xk += 1
            r0 = t0 + i * 128
            P.dma("sp", xt[xs][:], x_src[r0:r0 + 128, :], writes=[r_xt[xs]], dres=r_xt[xs])
            P.op("act", lambda e, xs=xs: e.activation(out=junk[:], in_=xt[xs][:], func=AF.Square,
                                                      accum_out=ss[xs][:]),
                 reads=[r_xt[xs]], writes=[r_junk, r_ss[xs]])
            P.op("act", lambda e, xs=xs: e.activation(out=sq[xs][:], in_=ss[xs][:], func=AF.Sqrt,
                                                      scale=1.0 / D, bias=g.eps_t[:, 0:1]),
                 reads=[r_ss[xs]], writes=[r_sq[xs]])
            P.op("dve", lambda e, xs=xs: e.reciprocal(out=rstd[xs][:], in_=sq[xs][:]),
                 reads=[r_sq[xs]], writes=[r_rstd[xs]])
            P.op("dve", lambda e, xs=xs: e.tensor_scalar(out=xt[xs][:], in0=xt[xs][:], scalar1=rstd[xs][:, 0:1],
                                                         scalar2=None, op0=ALU.mult),
                 reads=[r_rstd[xs], r_xt[xs]], writes=[r_xt[xs]])
            xv = xt[xs][:].rearrange("p (q j) -> p j q", j=16)
            for rnd in range(4):
                tb = tpk % 2
                tpk += 1
                for jj in range(4):
                    j = rnd * 4 + jj
                    P.op("pe", lambda e, tb=tb, jj=jj, j=j, xv=xv: e.transpose(out=tp[tb][:, jj * 128:(jj + 1) * 128],
                                                                             in_=xv[:, j, :], identity=ident[:]),
                         reads=[r_xt[xs], r_const], writes=[r_tp[tb]] if jj == 0 else (),
                         wadd=() if jj == 0 else [r_tp[tb]])
                for jj in range(4):
                    j = rnd * 4 + jj
                    eng = "dve"
                    if eng == "dve":
                        fn = lambda e, tb=tb, jj=jj, j=j, i=i, gs=gs: e.tensor_scalar(
                            out=hT[gs][:, j, i * 128:(i + 1) * 128], in0=tp[tb][:, jj * 128:(jj + 1) * 128],
                            scalar1=a1[:, j:j + 1], scalar2=b1[:, j:j + 1], op0=ALU.mult, op1=ALU.add)
                    else:
                        fn = lambda e, tb=tb, jj=jj, j=j, i=i, gs=gs: e.activation(
                            out=hT[gs][:, j, i * 128:(i + 1) * 128], in_=tp[tb][:, jj * 128:(jj + 1) * 128],
                            func=AF.Identity, scale=a1[:, j:j + 1], bias=b1[:, j:j + 1])
                    if "evac" in SKIP or ("evacact" in SKIP and eng == "act") or ("evacdve" in SKIP and eng == "dve"):
                        continue
                    P.op(eng, fn, reads=[r_tp[tb], r_ab, r_const], writes=[r_hT[gs][i][j]])
        if getattr(g, "dbg_stage", 99) < 2:
            continue
        for b in range(13):
            if getattr(g, "dbg_stage", 99) < 3 and b not in (0,):
                continue
            if getattr(g, "dbg_stage", 99) < 4 and b not in (0, 6):
                continue
            if getattr(g, "dbg_stage", 99) < 5 and b not in (0, 6, 3):
                continue
            ncol = 512 if b < 12 else 256
            ws = wk % 2
            wk += 1
            P.dma("sp", wsl[ws][:, :, 0:ncol], winv[:, :, b * 512:b * 512 + ncol], writes=[r_w[ws]], dres=r_w[ws])
            if b in (0, 1, 2, 6, 7, 8):
                for ct in range(4):
                    a = ak % 2
                    ak += 1
                    for j in range(16):
                        P.op("pe", lambda e, a=a, ws=ws, ct=ct, j=j, gs=gs: e.matmul(
                            acc[a][:], wsl[ws][:, j, ct * 128:(ct + 1) * 128], hT[gs][:, j, :],
                            start=(j == 0), stop=(j == 15)),
                            reads=[r_w[ws]] + [r_hT[gs][i][j] for i in range(4)],
                            writes=[r_acc[a]] if j == 0 else (), wadd=() if j == 0 else [r_acc[a]])
                    f = fk % 2
                    fk += 1
                    col0 = b * 512 + ct * 128
                    if b < 3:
                        isk = col0 >= 768
                        hh = (col0 - (768 if isk else 0)) // 128
                        P.op("act", lambda e, a=a, f=f: e.activation(out=qraw[f][:], in_=acc[a][:], func=AF.Copy),
                             reads=[r_acc[a]], writes=[r_qraw[f]])
                        P.op("pe", lambda e, f=f: e.matmul(aux[f][:], PT[:], qraw[f][:], start=True, stop=True),
                             reads=[r_const, r_qraw[f]], writes=[r_aux[f]])
                        ci = 2 if isk else 0
                        P.op("dve", lambda e, f=f, ci=ci, gs=gs: e.tensor_tensor(out=t1[f][:], in0=qraw[f][:].bitcast(F32),
                                                                        in1=tabs[gs][:, ci, :], op=ALU.mult),
                             reads=[r_qraw[f], r_tab[gs]], writes=[r_t1[f]])
                        P.op("dve", lambda e, f=f, ci=ci, gs=gs: e.tensor_tensor(out=t2[f][:], in0=aux[f][:],
                                                                        in1=tabs[gs][:, ci + 1, :], op=ALU.mult),
                             reads=[r_aux[f], r_tab[gs]], writes=[r_t2[f]])
                        P.op("pool", lambda e, f=f: e.tensor_tensor(out=ofm[f][:], in0=t1[f][:], in1=t2[f][:],
                                                                   op=ALU.add),
                             reads=[r_t1[f], r_t2[f]], writes=[r_ofm[f]])
                        P.dma("pool", g.retqk_d[hh, 1 if isk else 0, :, t0:t0 + 512], ofm[f][:],
                              reads=[r_ofm[f]], dres=r_ofm[f])
                        if not isk:
                            P.op("pool", lambda e, f=f, hh=hh: e.tensor_tensor(out=oxi[f][:], in0=ofm[f][:],
                                                                              in1=xitab[:, hh, :], op=ALU.mult),
                                 reads=[r_ofm[f], r_const], writes=[r_oxi[f]])
                            P.dma("pool", g.retqk_d[hh, 2, :, t0:t0 + 512], oxi[f][:],
                                  reads=[r_oxi[f]], dres=r_oxi[f])
                    else:
                        cc = col0 - 3072
                        isk = cc >= 768
                        hh = (cc - (768 if isk else 0)) // 128
                        P.op("act", lambda e, a=a, f=f: e.activation(out=qraw[f][:], in_=acc[a][:], func=AF.Copy),
                             reads=[r_acc[a]], writes=[r_qraw[f]])
                        P.op("act", lambda e, a=a, f=f: e.activation(out=sqf[f][:], in_=acc[a][:], func=AF.Square),
                             reads=[r_acc[a]], writes=[r_sqf[f]])
                        P.op("pe", lambda e, f=f: e.matmul(aux[f][:], blk[:], sqf[f][:], start=True, stop=True),
                             reads=[r_const, r_sqf[f]], writes=[r_aux[f]])
                        P.op("act", lambda e, f=f: e.activation(out=t1[f][:], in_=aux[f][:], func=AF.Sqrt,
                                                                bias=g.eps_t[:, 0:1]),
                             reads=[r_aux[f]], writes=[r_t1[f]])
                        P.op("dve", lambda e, f=f: e.reciprocal(out=t2[f][:], in_=t1[f][:]),
                             reads=[r_t1[f]], writes=[r_t2[f]])
                        wi = 1 if isk else 0
                        P.op("dve", lambda e, f=f, wi=wi: e.scalar_tensor_tensor(
                            out=ofm[f][:], in0=qraw[f][:].bitcast(F32), scalar=wqk[:, wi:wi + 1], in1=t2[f][:],
                            op0=ALU.mult, op1=ALU.mult),
                            reads=[r_qraw[f], r_t2[f], r_const], writes=[r_ofm[f]])
                        P.dma("pool", g.diffqk_d[hh, 1 if isk else 0, :, t0:t0 + 512], ofm[f][:],
                              reads=[r_ofm[f]], dres=r_ofm[f])
            else:
                tcol0 = (b * 512 - 1536) if b <= 5 else (b * 512 - 3072)
                for i in range(4):
                    a = ak % 2
                    ak += 1
                    for j in range(16):
                        P.op("pe", lambda e, a=a, ws=ws, i=i, j=j, ncol=ncol, gs=gs: e.matmul(
                            acc[a][:, 0:ncol], hT[gs][:, j, i * 128:(i + 1) * 128], wsl[ws][:, j, 0:ncol],
                            start=(j == 0), stop=(j == 15)),
                            reads=[r_w[ws], r_hT[gs][i][j]],
                            writes=[r_acc[a]] if j == 0 else (), wadd=() if j == 0 else [r_acc[a]])
                    o = tk % 2
                    tk += 1
                    P.new_epoch(("act",), r_otk[o])
                    for hf, func in enumerate(acts_tok[b]):
                        P.op("act", lambda e, a=a, o=o, hf=hf, func=func: e.activation(
                            out=otk[o][:, hf * 256:(hf + 1) * 256], in_=acc[a][:, hf * 256:(hf + 1) * 256], func=func),
                            reads=[r_acc[a]], wadd=[r_otk[o]])
                    r0 = t0 + i * 128
                    P.dma("pool", g.tok_d[r0:r0 + 128, tcol0:tcol0 + ncol], otk[o][:, 0:ncol],
                          reads=[r_otk[o]], dres=r_otk[o])


def declare_io(nc, S, debug):
    g = Ctx()
    inp = lambda n, s, d=F32: nc.dram_tensor(n, s, d, kind="ExternalInput").ap()
    scr = lambda n, s, d=F32: nc.dram_tensor(n, s, d, kind="ExternalOutput" if debug else "Internal").ap()
    g.x = inp("x", [S, D])
    g.c = inp("c", [D])
    g.ada_w = inp("ada_w", [2, D, NMOD])
    g.ada_b = inp("ada_b", [2, NMOD])
    g.mix_norm_w = inp("mix_norm_w", [2, D])
    g.w_in = inp("w_in", [2, D, DIN])
    g.diff_q_norm_w = inp("diff_q_norm_w", [2, 64])
    g.diff_k_norm_w = inp("diff_k_norm_w", [2, 64])
    g.ident = inp("ident", [128, 128])
    g.PT = inp("PT", [128, 128])
    g.blk64 = inp("blk64", [128, 128])
    g.rot = inp("rot", [4, 128, S])
    g.xitab = inp("xitab", [128, 6, 512])
    g.decayT = inp("decayT", [128, 6, 128])
    g.zeta = inp("zeta", [128, 6])
    g.maskT = inp("maskT", [128, 128])
    g.tril = inp("tril", [128, 128])
    g.identb = inp("identb", [128, 128], BF16)
    g.ret_norm_w = inp("ret_norm_w", [2, 768])
    g.diff_lambda = inp("diff_lambda", [2, 4, 64])
    g.diff_subln_w = inp("diff_subln_w", [2, 128])
    g.gmlp_norm_w = inp("gmlp_norm_w", [2, 512])
    g.gmlp_norm_b = inp("gmlp_norm_b", [2, 512])
    g.gmlp_ws = inp("gmlp_ws", [2, 4, 128, 128])
    g.gmlp_bs = inp("gmlp_bs", [2, 4, 128])
    g.cat_d = scr("cat_d", [S, D])
    g.w_out = inp("w_out", [2, D, D])
    g.ffn_norm_w = inp("ffn_norm_w", [2, D])
    g.router_group_w = inp("router_group_w", [2, D, 4])
    g.router_group_b = inp("router_group_b", [2, 4])
    g.router_expert_w = inp("router_expert_w", [2, D, 32])
    g.router_expert_b = inp("router_expert_b", [2, 32])
    g.w_gu = inp("expert_w_gate_up", [2, NEXP, D, 2 * EH])
    g.w_dn = inp("expert_w_down", [2, NEXP, EH, D])
    NBLK = (2 * S) // BS + NEXP
    g.x1_d = scr("x1_d", [S, D])
    g.h2_d = scr("h2_d", [S + 1, D])
    g.slot_tok_d = scr("slot_tok_d", [NBLK * BS, 1], I32)
    g.yslot_d = scr("yslot_d", [NBLK * BS, D])
    g.x2_d = scr("x2_d", [S, D])
    g.out = nc.dram_tensor("out", [S, D], F32, kind="ExternalOutput").ap()
    if debug:
        g.dbg_eid = scr("dbg_eid", [128, S // 128, 2])
        g.dbg_gate = scr("dbg_gate", [128, S // 128, 2])
        g.dbg_dest = scr("dbg_dest", [128, S // 128, 2], I32)
        g.dbg_bex = scr("dbg_bex", [128, NBLK], I32)
    g.mod_d = scr("mod_d", [2, NMOD])
    g.retqk_d = scr("retqk_d", [6, 3, 128, S], BF16)
    g.diffqk_d = scr("diffqk_d", [6, 2, 128, S], BF16)
    g.tok_d = scr("tok_d", [S, 3328], BF16)
    return g


def build(S, debug=True, layers=(0,), dbg_stage=99, phases=("A", "B", "C1", "C2", "D1", "D2", "E", "F", "G")):
    nc = bass.Bass("TRN2", target_bir_lowering=False)
    nc.dge_precook = False
    g = declare_io(nc, S, debug)
    g.dbg_stage = dbg_stage
    P = Prog(nc)
    with ExitStack() as es0:
        g.eps_t = es0.enter_context(nc.sbuf_tensor(_nm("eps_t"), [128, 1], F32))
        r_eps = Res("eps")
        P.op("dve", lambda e: e.memset(g.eps_t[:], EPS), writes=[r_eps])
        if "A" in phases:
            with ExitStack() as es:
                phase_mod(P, nc, es, g)
                P.end_phase()
        for l in layers:
            if "B" in phases:
                with ExitStack() as es:
                    phase_proj(P, nc, es, g, l, S, g.x if l == 0 else g.x2_d)
                    P.end_phase()
            if "C1" in phases:
                with ExitStack() as es:
                    phase_retgmlp(P, nc, es, g, l, S)
                    P.end_phase()
            if "C2" in phases:
                with ExitStack() as es:
                    phase_diff(P, nc, es, g, l, S)
                    P.end_phase()
            x_src = g.x if l == 0 else g.x2_d
            x_dst = g.x2_d if l == 0 else g.out
            if "D1" in phases:
                with ExitStack() as es:
                    phase_wout(P, nc, es, g, l, S, x_src)
                    P.end_phase()
            with ExitStack() as esT:
                T = Ctx()
                NT = S // 128
                NBLK = (2 * S) // BS + NEXP
                T.eidA = esT.enter_context(nc.sbuf_tensor(_nm("eidA"), [128, NT, 2], F32))
                T.gateA = esT.enter_context(nc.sbuf_tensor(_nm("gateA"), [128, NT, 2], F32))
                T.destA = esT.enter_context(nc.sbuf_tensor(_nm("destA"), [128, NT, 2], I32))
                T.blkexp = esT.enter_context(nc.sbuf_tensor(_nm("blkexp"), [128, NBLK], I32))
                if "D2" in phases:
                    with ExitStack() as es:
                        phase_norm2(P, nc, es, g, l, S, T)
                        P.end_phase()
                if "E" in phases:
                    with ExitStack() as es:
                        phase_route(P, nc, es, g, l, S, T)
                        P.end_phase()
                    if debug:
                        rd = Res()
                        P.dma("sp", g.dbg_eid, T.eidA[:], dres=rd)
                        P.dma("sp", g.dbg_gate, T.gateA[:], dres=rd)
                        P.dma("sp", g.dbg_dest, T.destA[:], dres=rd)
                        P.dma("sp", g.dbg_bex, T.blkexp[:], dres=rd)
                        P.end_phase()
                if "F" in phases:
                    with ExitStack() as es:
                        phase_experts(P, nc, es, g, l, S, T)
                        P.end_phase()
                if "G" in phases:
                    with ExitStack() as es:
                        phase_combine(P, nc, es, g, l, S, T, x_dst)
                        P.end_phase()
    return nc


GAM = [1.0 - 2.0 ** (-5.0 - h) for h in range(6)]
CDEC = [g_ ** 128.0 for g_ in GAM]


def phase_retgmlp(P, nc, es, g, l, S):
    sb = lambda n, s, d: es.enter_context(nc.sbuf_tensor(_nm(n), s, d))
    pst = lambda n, d=F32, w=512: es.enter_context(nc.psum_tensor(_nm(n), [128, w], d))
    NCH = S // 128
    R = lambda n="": Res(n)
    decayT = sb("decayT", [128, 6, 128], F32)
    zeta = sb("zeta", [128, 6], F32)
    identb = sb("identb", [128, 128], BF16)
    identf = sb("identf", [128, 128], F32)
    tril = sb("tril", [128, 128], F32)
    retw = sb("retw", [128, 768], F32)
    gw = sb("gw", [128, 512], F32)
    gb = sb("gb", [128, 512], F32)
    bs = sb("bs", [128, 4], F32)
    wraw = sb("wraw", [128, 4, 128], F32)
    WT = sb("WT", [128, 4, 128], BF16)
    qk = [sb("qk%d" % i, [128, 6, 3, 512], BF16) for i in range(2)]
    tokrow = [sb("tokrow%d" % i, [128, 3328], BF16) for i in range(2)]
    st32 = sb("st32", [128, 6, 128], F32)
    st16 = sb("st16", [128, 6, 128], BF16)
    A = [sb("A%d" % i, [128, 128], BF16) for i in range(2)]
    kz = [sb("kz%d" % i, [128, 128], BF16) for i in range(2)]
    bst = sb("bst", [128, 6, 6], F32)
    mv = sb("mv", [128, 6, 2], F32)
    sd = sb("sd", [128, 6], F32)
    rs = sb("rs", [128, 6], F32)
    catr = [sb("catr%d" % i, [128, 768], F32) for i in range(2)]
    wsg = [sb("wsg%d" % i, [128, 768], F32) for i in range(2)]
    gst = sb("gst", [128, 6], F32)
    gmv = sb("gmv", [128, 2], F32)
    gsd = sb("gsd", [128, 1], F32)
    grs = sb("grs", [128, 1], F32)
    vn = sb("vn", [128, 512], F32)
    vnb = [sb("vnb%d" % i, [128, 512], BF16) for i in range(2)]
    catg = [sb("catg%d" % i, [128, 512], F32) for i in range(2)]
    yA = pst("yA")
    yB = pst("yB")
    sTp = pst("sTp")
    kvp = pst("kvp")
    ktr = pst("ktr", BF16, 1024)
    fps = pst("fps")
    wtp = pst("wtp")

    r_c = R("const")
    r_W = R("W")
    r_WT = R("WT")
    r_qk = [R(), R()]
    r_tok = [R(), R()]
    r_st32 = [R() for _ in range(6)]
    r_st16 = [R() for _ in range(6)]
    r_A = [R(), R()]
    r_kz = [R(), R()]
    r_sT = [R(), R()]
    r_kv = [R(), R()]
    r_ktr = [R(), R()]
    r_y = [R() for _ in range(6)]
    r_bst, r_mv, r_sd, r_rs = R(), R(), R(), R()
    r_catr = [R(), R()]
    r_wsg = [R(), R()]
    r_gst, r_gmv, r_gsd, r_grs, r_vn = R(), R(), R(), R(), R()
    r_vnb = [R(), R()]
    r_f = R()
    r_catg = [R(), R()]
    r_wtp = R()

    for dst, src in ((decayT, g.decayT), (zeta, g.zeta), (identb, g.identb), (identf, g.ident), (tril, g.tril)):
        P.dma("sp", dst[:], src, wadd=[r_c], dres=r_c)
    P.dma("sp", retw[:], bc(g.ret_norm_w[l]), wadd=[r_c], dres=r_c)
    P.dma("sp", gw[:], bc(g.gmlp_norm_w[l]), wadd=[r_c], dres=r_c)
    P.dma("sp", gb[:], bc(g.gmlp_norm_b[l]), wadd=[r_c], dres=r_c)
    P.dma("sp", bs[:], g.gmlp_bs[l].rearrange("g t -> t g"), wadd=[r_c], dres=r_c, allow_slow_non_contiguous=True)
    P.dma("sp", wraw[:], g.gmlp_ws[l].rearrange("g t s -> t g s"), writes=[r_W], dres=r_W)
    for gg in range(4):
        P.op("dve", lambda e, gg=gg: e.tensor_tensor(out=wraw[:, gg, :], in0=wraw[:, gg, :], in1=tril[:], op=ALU.mult),
             reads=[r_c], writes=[r_W])
        P.op("pe", lambda e, gg=gg: e.transpose(out=wtp[:, gg * 128:(gg + 1) * 128], in_=wraw[:, gg, :], identity=identf[:]),
             reads=[r_W, r_c], writes=[r_wtp])
        P.op("act", lambda e, gg=gg: e.activation(out=WT[:, gg, :], in_=wtp[:, gg * 128:(gg + 1) * 128], func=AF.Copy),
             reads=[r_wtp], wadd=[r_WT])
    P.op("dve", lambda e: e.memset(st32[:], 0.0), writes=r_st32)
    P.op("dve", lambda e: e.memset(st16[:], 0.0), writes=r_st16)

    sk = 0
    for n in range(NCH):
        ts = n % 2
        if n % 4 == 0:
            qs = (n // 4) % 2
            t0 = n * 128
            P.dma("sp", qk[qs][:], g.retqk_d[:, :, :, t0:t0 + 512].rearrange("h f p t -> p h f t"),
                  writes=[r_qk[qs]], dres=r_qk[qs])
        cc = n % 4
        P.dma("sp", tokrow[ts][:], g.tok_d[n * 128:(n + 1) * 128, :], writes=[r_tok[ts]], dres=r_tok[ts])
        P.op("pool", lambda e, ts=ts: e.tensor_tensor(out=wsg[ts][:], in0=tokrow[ts][:, 768:1536], in1=retw[:], op=ALU.mult),
             reads=[r_tok[ts], r_c], writes=[r_wsg[ts]])
        for h in range(6):
            a = sk % 2
            sk += 1
            qc = qk[qs][:, h, 0, cc * 128:(cc + 1) * 128]
            kc = qk[qs][:, h, 1, cc * 128:(cc + 1) * 128]
            qx = qk[qs][:, h, 2, cc * 128:(cc + 1) * 128]
            vh = tokrow[ts][:, h * 128:(h + 1) * 128]
            ybank = yA if h < 4 else yB
            yreg = ybank[:, (h % 4) * 128:(h % 4 + 1) * 128]
            P.op("pe", lambda e, a=a, kc=kc, qc=qc: e.matmul(sTp[:, a * 128:(a + 1) * 128], kc, qc, start=True, stop=True),
                 reads=[r_qk[qs]], writes=[r_sT[a]])
            P.op("dve", lambda e, a=a, h=h: e.tensor_tensor(out=A[a][:], in0=sTp[:, a * 128:(a + 1) * 128],
                                                          in1=decayT[:, h, :], op=ALU.mult),
                 reads=[r_sT[a], r_c], writes=[r_A[a]])
            P.op("pe", lambda e, a=a, yreg=yreg, vh=vh: e.matmul(yreg, A[a][:], vh, start=True, stop=False),
                 reads=[r_A[a], r_tok[ts]], writes=[r_y[h]])
            P.op("pe", lambda e, yreg=yreg, qx=qx, h=h: e.matmul(yreg, qx, st16[:, h, :], start=False, stop=True),
                 reads=[r_qk[qs], r_st16[h]], wadd=[r_y[h]])
            P.op("pe", lambda e, a=a, kc=kc: e.transpose(out=ktr[:, a * 128:(a + 1) * 128], in_=kc, identity=identb[:]),
                 reads=[r_qk[qs], r_c], writes=[r_ktr[a]])
            P.op("act", lambda e, a=a, h=h: e.activation(out=kz[a][:], in_=ktr[:, a * 128:(a + 1) * 128], func=AF.Copy,
                                                        scale=zeta[:, h:h + 1]),
                 reads=[r_ktr[a], r_c], writes=[r_kz[a]])
            P.op("pe", lambda e, a=a, vh=vh: e.matmul(kvp[:, a * 128:(a + 1) * 128], kz[a][:], vh, start=True, stop=True),
                 reads=[r_kz[a], r_tok[ts]], writes=[r_kv[a]])
            P.op("dve", lambda e, a=a, h=h: e.scalar_tensor_tensor(out=st32[:, h, :], in0=st32[:, h, :], scalar=float(CDEC[h]),
                                                                  in1=kvp[:, a * 128:(a + 1) * 128],
                                                                  op0=ALU.mult, op1=ALU.add),
                 reads=[r_kv[a]], writes=[r_st32[h]])
            P.op("act", lambda e, h=h: e.activation(out=st16[:, h, :], in_=st32[:, h, :], func=AF.Copy),
                 reads=[r_st32[h]], writes=[r_st16[h]])
        cs = n % 2
        for h in range(6):
            ybank = yA if h < 4 else yB
            yreg = ybank[:, (h % 4) * 128:(h % 4 + 1) * 128]
            P.op("dve", lambda e, h=h, yreg=yreg: e.bn_stats(out=bst[:, h, :], in_=yreg), reads=[r_y[h]], wadd=[r_bst])
        for h in range(6):
            P.op("dve", lambda e, h=h: e.bn_aggr(out=mv[:, h, :], in_=bst[:, h, :]), reads=[r_bst], wadd=[r_mv])
        P.op("act", lambda e: e.activation(out=sd[:], in_=mv[:, :, 1], func=AF.Sqrt, bias=g.eps_t[:, 0:1]),
             reads=[r_mv], writes=[r_sd])
        P.op("dve", lambda e: e.reciprocal(out=rs[:], in_=sd[:]), reads=[r_sd], writes=[r_rs])
        P.new_epoch(("dve",), r_catr[cs])
        for h in range(6):
            ybank = yA if h < 4 else yB
            yreg = ybank[:, (h % 4) * 128:(h % 4 + 1) * 128]
            P.op("dve", lambda e, h=h, yreg=yreg, cs=cs: e.tensor_scalar(out=catr[cs][:, h * 128:(h + 1) * 128], in0=yreg,
                                                                        scalar1=mv[:, h, 0:1], scalar2=rs[:, h:h + 1],
                                                                        op0=ALU.subtract, op1=ALU.mult),
                 reads=[r_y[h], r_mv, r_rs], wadd=[r_catr[cs]])
        P.new_epoch(("dve",), r_bst)
        P.new_epoch(("dve",), r_mv)
        P.op("pool", lambda e, cs=cs, ts=ts: e.tensor_tensor(out=catr[cs][:], in0=catr[cs][:], in1=wsg[ts][:], op=ALU.mult),
             reads=[r_wsg[ts]], writes=[r_catr[cs]])
        P.dma("pool", g.cat_d[n * 128:(n + 1) * 128, 0:768], catr[cs][:], reads=[r_catr[cs]], dres=r_catr[cs])
        gv = tokrow[ts][:, 2816:3328]
        P.op("dve", lambda e, gv=gv: e.bn_stats(out=gst[:], in_=gv), reads=[r_tok[ts]], writes=[r_gst])
        P.op("dve", lambda e: e.bn_aggr(out=gmv[:], in_=gst[:]), reads=[r_gst], writes=[r_gmv])
        P.op("act", lambda e: e.activation(out=gsd[:], in_=gmv[:, 1:2], func=AF.Sqrt, bias=g.eps_t[:, 0:1]),
             reads=[r_gmv], writes=[r_gsd])
        P.op("dve", lambda e: e.reciprocal(out=grs[:], in_=gsd[:]), reads=[r_gsd], writes=[r_grs])
        P.op("dve", lambda e, gv=gv: e.tensor_scalar(out=vn[:], in0=gv, scalar1=gmv[:, 0:1], scalar2=grs[:, 0:1],
                                                   op0=ALU.subtract, op1=ALU.mult),
             reads=[r_tok[ts], r_gmv, r_grs], writes=[r_vn])
        P.op("pool", lambda e: e.tensor_tensor(out=vn[:], in0=vn[:], in1=gw[:], op=ALU.mult), reads=[r_c], writes=[r_vn])
        P.op("pool", lambda e, cs=cs: e.tensor_tensor(out=vnb[cs][:], in0=vn[:], in1=gb[:], op=ALU.add),
             reads=[r_vn, r_c], writes=[r_vnb[cs]])
        for gg in range(4):
            P.op("pe", lambda e, gg=gg, cs=cs: e.matmul(fps[:, gg * 128:(gg + 1) * 128], WT[:, gg, :],
                                                       vnb[cs][:, gg * 128:(gg + 1) * 128], start=True, stop=True),
                 reads=[r_WT, r_vnb[cs]], writes=[r_f] if gg == 0 else (), wadd=() if gg == 0 else [r_f])
        P.new_epoch(("dve",), r_catg[cs])
        for gg in range(4):
            P.op("dve", lambda e, gg=gg, cs=cs, ts=ts: e.scalar_tensor_tensor(
                out=catg[cs][:, gg * 128:(gg + 1) * 128], in0=fps[:, gg * 128:(gg + 1) * 128], scalar=bs[:, gg:gg + 1],
                in1=tokrow[ts][:, 2304 + gg * 128:2304 + (gg + 1) * 128], op0=ALU.add, op1=ALU.mult),
                reads=[r_f, r_tok[ts], r_c], wadd=[r_catg[cs]])
        P.dma("pool", g.cat_d[n * 128:(n + 1) * 128, 1536:2048], catg[cs][:], reads=[r_catg[cs]], dres=r_catg[cs])


def phase_diff(P, nc, es, g, l, S):
    import math
    sb = lambda n, s, d: es.enter_context(nc.sbuf_tensor(_nm(n), s, d))
    pst = lambda n, d=F32, w=512: es.enter_context(nc.psum_tensor(_nm(n), [128, w], d))
    NCH = S // 128
    NQ = NCH // 4
    R = lambda n="": Res(n)
    lam_init = 0.8 - 0.6 * math.exp(-0.3 * l)
    maskT = sb("maskT", [128, 128], F32)
    wq = sb("wq", [128, 64], F32)
    wk = sb("wk", [128, 64], F32)
    lamt = sb("lamt", [128, 256], F32)
    tmp = sb("tmpd", [128, 64], F32)
    negM = sb("negM", [128, 1], F32)
    s01 = sb("s01", [128, 2], F32)
    nlam = sb("nlam", [128, 1], F32)
    subw = sb("subw", [128, 128], F32)
    qT = [sb("dqT%d" % i, [128, S], BF16) for i in range(2)]
    kT = [[sb("dkT%d_%d" % (i, j), [128, S], BF16) for j in range(2)] for i in range(2)]
    va = [sb("va%d" % i, [128, NCH, 129], BF16) for i in range(2)]
    pt = [sb("pt%d" % i, [128, 512], BF16) for i in range(3)]
    rr = sb("rr", [128, 2], F32)
    nl = sb("nl", [128, 1], F32)
    tt = [sb("tt%d" % i, [128, 128], F32) for i in range(2)]
    oo = [sb("oo%d" % i, [128, 128], F32) for i in range(2)]
    junk = sb("junkd", [128, 128], F32)
    ssq = sb("ssq", [128, 1], F32)
    sdd = sb("sdd", [128, 1], F32)
    rsd = sb("rsd", [128, 1], F32)
    catd = [sb("catd%d" % i, [128, 128], F32) for i in range(2)]
    accb = [pst("accb%d" % i) for i in range(3)]
    sTb = [pst("sTb%d" % i) for i in range(2)]

    r_c = R()
    r_q = [R(), R()]
    r_k = [R(), R()]
    r_v = [R(), R()]
    r_pt = [R(), R(), R()]
    r_sT = [R(), R()]
    r_acc = [R(), R(), R()]
    r_rr, r_nl, r_ssq, r_sdd, r_rsd, r_junk = R(), R(), R(), R(), R(), R()
    r_tt = [R(), R()]
    r_oo = [R(), R()]
    r_catd = [R(), R()]

    P.dma("sp", maskT[:], g.maskT, wadd=[r_c], dres=r_c)
    P.dma("sp", wq[:], bc(g.diff_q_norm_w[l]), wadd=[r_c], dres=r_c)
    P.dma("sp", wk[:], bc(g.diff_k_norm_w[l]), wadd=[r_c], dres=r_c)
    P.dma("sp", lamt[:], bc(g.diff_lambda[l]), wadd=[r_c], dres=r_c)
    P.dma("sp", subw[:], bc(g.diff_subln_w[l]), wadd=[r_c], dres=r_c)
    r_s = R()
    P.op("dve", lambda e: e.tensor_tensor(out=tmp[:], in0=wq[:], in1=wk[:], op=ALU.mult), reads=[r_c], writes=[r_s])
    P.op("dve", lambda e: e.tensor_reduce(out=negM[:], in_=tmp[:], axis=AX.X, op=ALU.max, apply_absolute_value=True),
         reads=[r_s], writes=[r_s])
    P.op("dve", lambda e: e.tensor_scalar(out=negM[:], in0=negM[:], scalar1=-8.0, scalar2=None, op0=ALU.mult),
         reads=[r_s], writes=[r_s])
    for i in range(2):
        P.op("dve", lambda e, i=i: e.tensor_tensor(out=tmp[:], in0=lamt[:, 128 * i:128 * i + 64], in1=lamt[:, 128 * i + 64:128 * i + 128], op=ALU.mult),
             reads=[r_c], writes=[r_s])
        P.op("dve", lambda e, i=i: e.tensor_reduce(out=s01[:, i:i + 1], in_=tmp[:], axis=AX.X, op=ALU.add),
             reads=[r_s], writes=[r_s])
    P.op("act", lambda e: e.activation(out=s01[:], in_=s01[:], func=AF.Exp), reads=[r_s], writes=[r_s])
    P.op("dve", lambda e: e.tensor_tensor(out=nlam[:], in0=s01[:, 1:2], in1=s01[:, 0:1], op=ALU.subtract),
         reads=[r_s], writes=[r_s])
    P.op("dve", lambda e: e.tensor_scalar(out=nlam[:], in0=nlam[:], scalar1=-lam_init, scalar2=None, op0=ALU.add),
         reads=[r_s], writes=[r_s])
    P.op("dve", lambda e: e.tensor_scalar(out=subw[:], in0=subw[:], scalar1=1.0 - lam_init, scalar2=None, op0=ALU.mult),
         reads=[r_c, r_s], writes=[r_s])
    r_c = r_s

    for hs_ in range(2):
        P.op("pool", lambda e, hs_=hs_: e.memset(va[hs_][:, :, 128:129], 1.0), writes=[r_v[hs_]])
        P.op("pool", lambda e, hs_=hs_: e.memset(kT[hs_][0][64:128, :], 0.0), writes=[r_k[hs_]])
        P.op("pool", lambda e, hs_=hs_: e.memset(kT[hs_][1][0:64, :], 0.0), wadd=[r_k[hs_]])
    ACO = 160
    def accreg(j, qi):
        idx = j * 4 + qi
        return idx // 3, (idx % 3) * ACO

    pk = 0
    sk = 0
    ok = 0
    for h in range(6):
        hs = h % 2
        P.dma("sp", qT[hs][:], g.diffqk_d[h, 0], writes=[r_q[hs]], dres=r_q[hs])
        P.new_epoch(("sp",), r_k[hs])
        P.dma("sp", kT[hs][0][0:64, :], g.diffqk_d[h, 1, 0:64, :], wadd=[r_k[hs]], dres=r_k[hs])
        P.dma("sp", kT[hs][1][64:128, :], g.diffqk_d[h, 1, 64:128, :], wadd=[r_k[hs]], dres=r_k[hs])
        P.new_epoch(("sp",), r_v[hs]) if h >= 2 else None
        P.dma("sp", va[hs][:, :, 0:128], g.tok_d[:, 1536 + h * 128:1536 + (h + 1) * 128].rearrange("(n c) e -> c n e", c=128),
              reads=[r_v[hs]] if h < 2 else (), wadd=[r_v[hs]], dres=r_v[hs])
        for Q in range(NQ):
            nkb = 4 * Q + 4
            for kb in range(nkb):
                c0 = max(0, kb - 4 * Q)
                ncols = (4 - c0) * 128
                for j in range(2):
                    s_ = sk % 2
                    sk += 1
                    p_ = pk % 3
                    pk += 1
                    lo, hi = j * 64, (j + 1) * 64
                    P.op("pe", lambda e, s_=s_, j=j, kb=kb, Q=Q, c0=c0, ncols=ncols, hs=hs: e.matmul(
                        sTb[s_][:, 0:ncols], kT[hs][j][:, kb * 128:(kb + 1) * 128],
                        qT[hs][:, Q * 512 + c0 * 128:Q * 512 + 512], start=True, stop=True),
                        reads=[r_k[hs], r_q[hs]], writes=[r_sT[s_]])
                    P.op("act", lambda e, s_=s_, p_=p_, ncols=ncols: e.activation(
                        out=pt[p_][:, 0:ncols], in_=sTb[s_][:, 0:ncols], func=AF.Exp, scale=0.125, bias=negM[:, 0:1]),
                        reads=[r_sT[s_], r_c], writes=[r_pt[p_]])
                    if kb >= 4 * Q:
                        P.op("pool", lambda e, p_=p_: e.tensor_tensor(out=pt[p_][:, 0:128], in0=pt[p_][:, 0:128],
                                                                      in1=maskT[:], op=ALU.mult),
                             reads=[r_c], writes=[r_pt[p_]])
                    for qi in range(c0, 4):
                        bnk, off = accreg(j, qi)
                        first = (kb == 0 and (j * 4 + qi) % 3 == 0)
                        P.op("pe", lambda e, bnk=bnk, off=off, p_=p_, qi=qi, c0=c0, kb=kb, hs=hs, first=first, Q=Q: e.matmul(
                            accb[bnk][:, off:off + 129], pt[p_][:, (qi - c0) * 128:(qi - c0 + 1) * 128],
                            va[hs][:, kb, :], start=first, stop=(kb == 4 * Q + qi), skip_group_check=True),
                            reads=[r_pt[p_], r_v[hs]],
                            writes=[r_acc[bnk]] if first else (), wadd=() if first else [r_acc[bnk]])
            for qi in range(4):
                o_ = ok % 2
                ok += 1
                b0, f0 = accreg(0, qi)
                b1, f1 = accreg(1, qi)
                P.op("dve", lambda e, b0=b0, f0=f0: e.reciprocal(out=rr[:, 0:1], in_=accb[b0][:, f0 + 128:f0 + 129]),
                     reads=[r_acc[b0]], writes=[r_rr])
                P.op("dve", lambda e, b1=b1, f1=f1: e.reciprocal(out=rr[:, 1:2], in_=accb[b1][:, f1 + 128:f1 + 129]),
                     reads=[r_acc[b1]], wadd=[r_rr])
                P.op("dve", lambda e: e.tensor_tensor(out=nl[:], in0=rr[:, 1:2], in1=nlam[:], op=ALU.mult),
                     reads=[r_rr, r_c], writes=[r_nl])
                P.op("dve", lambda e, b1=b1, f1=f1, o_=o_: e.tensor_scalar(out=tt[o_][:], in0=accb[b1][:, f1:f1 + 128],
                                                                       scalar1=nl[:, 0:1], scalar2=None, op0=ALU.mult),
                     reads=[r_acc[b1], r_nl], writes=[r_tt[o_]])
                P.op("dve", lambda e, b0=b0, f0=f0, o_=o_: e.scalar_tensor_tensor(
                    out=oo[o_][:], in0=accb[b0][:, f0:f0 + 128], scalar=rr[:, 0:1], in1=tt[o_][:],
                    op0=ALU.mult, op1=ALU.add),
                    reads=[r_acc[b0], r_rr, r_tt[o_]], writes=[r_oo[o_]])
                P.op("act", lambda e, o_=o_: e.activation(out=junk[:], in_=oo[o_][:], func=AF.Square, accum_out=ssq[:]),
                     reads=[r_oo[o_]], writes=[r_junk, r_ssq])
                P.op("act", lambda e: e.activation(out=sdd[:], in_=ssq[:], func=AF.Sqrt, scale=1.0 / 128, bias=g.eps_t[:, 0:1]),
                     reads=[r_ssq], writes=[r_sdd])
                P.op("dve", lambda e: e.reciprocal(out=rsd[:], in_=sdd[:]), reads=[r_sdd], writes=[r_rsd])
                P.op("dve", lambda e, o_=o_: e.scalar_tensor_tensor(out=catd[o_][:], in0=oo[o_][:], scalar=rsd[:, 0:1],
                                                                  in1=subw[:], op0=ALU.mult, op1=ALU.mult),
                     reads=[r_oo[o_], r_rsd, r_c], writes=[r_catd[o_]])
                r0 = (4 * Q + qi) * 128
                P.dma("pool", g.cat_d[r0:r0 + 128, 768 + h * 128:768 + (h + 1) * 128], catd[o_][:],
                      reads=[r_catd[o_]], dres=r_catd[o_])


def phase_cat_test(P, nc, es, g, l, S):
    pass


BS = 256


def dyn_dma(e, out, src, regname):
    import re
    ins = e.dma_start(out=out, in_=src)
    m = re.search(r"R\[([A-Za-z]+)_tmp_(\d+)\]", ins.concise())
    if m:
        pre, n = m.group(1), int(m.group(2))
        for cand in range(n - 3, n + 1):
            for nm in ("%s_tmp_%d" % (pre, cand), "%s_%s_snap_%d" % (pre, regname, cand)):
                try:
                    e.free_register(bass.RegisterHandle(nm, mybir.EngineType.SP))
                except Exception:
                    pass
    return ins


def phase_wout(P, nc, es, g, l, S, x_src):
    sb = lambda n, s, d: es.enter_context(nc.sbuf_tensor(_nm(n), s, d))
    pst = lambda n: es.enter_context(nc.psum_tensor(_nm(n), [128, 512], F32))
    R = lambda n="": Res(n)
    NG = S // 512
    ident = sb("identw", [128, 128], F32)
    g1bc = sb("g1bc", [128, D], F32)
    ct = [sb("ct%d" % i, [128, D], F32) for i in range(2)]
    catT = [sb("catT%d" % i, [128, 16, 512], F32R) for i in range(2)]
    wsl = [sb("wo%d" % i, [128, 16, 512], F32R) for i in range(2)]
    xb = [sb("xb%d" % i, [128, 512], F32) for i in range(3)]
    tb = [sb("tb%d" % i, [128, 512], F32) for i in range(3)]
    tp = [pst("tpw%d" % i) for i in range(2)]
    acc = [pst("accw%d" % i) for i in range(2)]
    r_c = R()
    r_ct = [R(), R()]
    r_catT = [[[R() for r in range(4)] for i in range(4)] for s in range(2)]
    r_w = [R(), R()]
    r_xb = [R(), R(), R()]
    r_tb = [R(), R(), R()]
    r_tp = [R(), R()]
    r_acc = [R(), R()]
    P.dma("sp", ident[:], g.ident, wadd=[r_c], dres=r_c)
    P.dma("sp", g1bc[:], bc(g.mod_d[l, 2 * D:3 * D]), wadd=[r_c], dres=r_c)
    wv = g.w_out[l].rearrange("(p j) n -> p j n", j=16).bitcast(F32R)
    ck = tk = wk = ak = xk = 0
    for gi in range(NG):
        gs = gi % 2
        t0 = gi * 512
        for i in range(4):
            cs = ck % 2
            ck += 1
            r0 = t0 + i * 128
            P.dma("sp", ct[cs][:], g.cat_d[r0:r0 + 128, :], writes=[r_ct[cs]], dres=r_ct[cs])
            cv = ct[cs][:].rearrange("p (q j) -> p j q", j=16)
            for rnd in range(4):
                t_ = tk % 2
                tk += 1
                for jj in range(4):
                    j = rnd * 4 + jj
                    P.op("pe", lambda e, t_=t_, jj=jj, j=j, cv=cv: e.transpose(out=tp[t_][:, jj * 128:(jj + 1) * 128],
                                                                            in_=cv[:, j, :], identity=ident[:]),
                         reads=[r_ct[cs], r_c], writes=[r_tp[t_]] if jj == 0 else (), wadd=() if jj == 0 else [r_tp[t_]])
                eng = "dve" if rnd % 2 == 0 else "act"
                outv = catT[gs][:, rnd * 4:(rnd + 1) * 4, i * 128:(i + 1) * 128]
                inv_ = tp[t_][:].rearrange("p (a b) -> p a b", a=4)
                if eng == "dve":
                    fn = lambda e, outv=outv, inv_=inv_: e.tensor_copy(out=outv, in_=inv_)
                else:
                    fn = lambda e, outv=outv, inv_=inv_: e.activation(out=outv, in_=inv_, func=AF.Copy)
                P.op(eng, fn, reads=[r_tp[t_]], writes=[r_catT[gs][i][rnd]])
        for cb in range(4):
            ws = wk % 2
            wk += 1
            P.dma("sp", wsl[ws][:], wv[:, :, cb * 512:(cb + 1) * 512], writes=[r_w[ws]], dres=r_w[ws])
            for i in range(4):
                a = ak % 2
                ak += 1
                x_ = xk % 3
                xk += 1
                r0 = t0 + i * 128
                P.dma("sp", xb[x_][:], x_src[r0:r0 + 128, cb * 512:(cb + 1) * 512], writes=[r_xb[x_]], dres=r_xb[x_])
                for j in range(16):
                    P.op("pe", lambda e, a=a, ws=ws, i=i, j=j, gs=gs: e.matmul(
                        acc[a][:], catT[gs][:, j, i * 128:(i + 1) * 128], wsl[ws][:, j, :], start=(j == 0), stop=(j == 15)),
                        reads=[r_w[ws], r_catT[gs][i][j // 4]],
                        writes=[r_acc[a]] if j == 0 else (), wadd=() if j == 0 else [r_acc[a]])
                P.op("dve", lambda e, a=a, x_=x_, cb=cb: e.tensor_tensor(out=tb[x_][:], in0=acc[a][:],
                                                                        in1=g1bc[:, cb * 512:(cb + 1) * 512], op=ALU.mult),
                     reads=[r_acc[a], r_c], writes=[r_tb[x_]])
                P.op("pool", lambda e, x_=x_: e.tensor_tensor(out=tb[x_][:], in0=tb[x_][:], in1=xb[x_][:], op=ALU.add),
                     reads=[r_xb[x_]], writes=[r_tb[x_]])
                P.dma("pool", g.x1_d[r0:r0 + 128, cb * 512:(cb + 1) * 512], tb[x_][:], reads=[r_tb[x_]], dres=r_tb[x_])


def phase_norm2(P, nc, es, g, l, S, T):
    sb = lambda n, s, d: es.enter_context(nc.sbuf_tensor(_nm(n), s, d))
    pst = lambda n: es.enter_context(nc.psum_tensor(_nm(n), [128, 512], F32))
    R = lambda n="": Res(n)
    NT = S // 128
    ident = sb("identn", [128, 128], F32)
    a2bc = sb("a2bc", [128, D], F32)
    b2bc = sb("b2bc", [128, D], F32)
    tmpbc = sb("tmpbc", [128, D], F32)
    wr = sb("wr", [128, 16, 36], F32)
    rbb = sb("rbb", [128, 36], F32)
    zrow = sb("zrow", [1, D], F32)
    xt = [sb("x1t%d" % i, [128, D], F32) for i in range(2)]
    h2 = [sb("h2t%d" % i, [128, D], F32) for i in range(2)]
    junk = sb("junkn", [128, D], BF16)
    ss = sb("ssn", [128, 1], F32)
    sq = sb("sqn", [128, 1], F32)
    rstd = sb("rstdn", [128, 1], F32)
    h2T = [sb("h2T%d" % i, [128, 16, 128], F32) for i in range(2)]
    lgsA = sb("lgsA", [128, NT, 36], F32)
    io8 = sb("io8", [128, 8], F32)
    gmax = sb("gmax", [128, NT], F32)
    oh4 = sb("oh4", [128, NT, 4], F32)
    ex4 = sb("ex4", [128, NT, 4], F32)
    sume = sb("sume", [128, NT], F32)
    pg = sb("pg", [128, NT], F32)
    gif = sb("gif", [128, NT], F32)
    tmp48 = sb("tmp48", [128, NT, 4, 8], F32)
    els = sb("els", [128, NT, 8], F32)
    els2 = sb("els2", [128, NT, 8], F32)
    top1 = sb("top1", [128, NT], F32)
    top2 = sb("top2", [128, NT], F32)
    mk1 = sb("mk1", [128, NT, 8], F32)
    mk2 = sb("mk2", [128, NT, 8], F32)
    dd = sb("dd", [128, NT], F32)
    w0 = sb("w0", [128, NT], F32)
    tp = [pst("tpn%d" % i) for i in range(2)]
    lgp = pst("lgp")
    r_c, r_x, r_h2, r_tp, r_h2T, r_lg = R(), [R(), R()], [R(), R()], [R(), R()], [R(), R()], R()
    r_s = R()
    r_lgs = R()
    r_junk = R()
    P.dma("sp", ident[:], g.ident, wadd=[r_c], dres=r_c)
    P.dma("sp", a2bc[:], bc(g.mod_d[l, 4 * D:5 * D]), wadd=[r_c], dres=r_c)
    P.dma("sp", b2bc[:], bc(g.mod_d[l, 3 * D:4 * D]), wadd=[r_c], dres=r_c)
    P.dma("sp", tmpbc[:], bc(g.ffn_norm_w[l]), wadd=[r_c], dres=r_c)
    P.dma("sp", wr[:, :, 0:4], g.router_group_w[l].rearrange("(p j) n -> p j n", j=16), wadd=[r_c], dres=r_c)
    P.dma("sp", wr[:, :, 4:36], g.router_expert_w[l].rearrange("(p j) n -> p j n", j=16), wadd=[r_c], dres=r_c)
    P.dma("sp", rbb[:, 0:4], bc(g.router_group_b[l]), wadd=[r_c], dres=r_c)
    P.dma("sp", rbb[:, 4:36], bc(g.router_expert_b[l]), wadd=[r_c], dres=r_c)
    r_c2 = R()
    P.op("dve", lambda e: e.scalar_tensor_tensor(out=a2bc[:], in0=a2bc[:], scalar=1.0, in1=tmpbc[:], op0=ALU.add, op1=ALU.mult),
         reads=[r_c], writes=[r_c2])
    P.op("dve", lambda e: e.memset(zrow[:], 0.0), writes=[r_s])
    P.dma("pool", g.h2_d[S:S + 1, :], zrow[:], reads=[r_s], dres=r_s)
    tk = 0
    for i in range(NT):
        s = i % 2
        r0 = i * 128
        P.dma("sp", xt[s][:], g.x1_d[r0:r0 + 128, :], writes=[r_x[s]], dres=r_x[s])
        P.op("act", lambda e, s=s: e.activation(out=junk[:], in_=xt[s][:], func=AF.Square, accum_out=ss[:]),
             reads=[r_x[s]], writes=[r_junk, r_s])
        P.op("act", lambda e: e.activation(out=sq[:], in_=ss[:], func=AF.Sqrt, scale=1.0 / D, bias=g.eps_t[:, 0:1]),
             reads=[r_s], writes=[r_s])
        P.op("dve", lambda e: e.reciprocal(out=rstd[:], in_=sq[:]), reads=[r_s], writes=[r_s])
        P.op("dve", lambda e, s=s: e.scalar_tensor_tensor(out=h2[s][:], in0=xt[s][:], scalar=rstd[:, 0:1], in1=a2bc[:],
                                                        op0=ALU.mult, op1=ALU.mult),
             reads=[r_x[s], r_s, r_c2], writes=[r_h2[s]])
        P.op("pool", lambda e, s=s: e.tensor_tensor(out=h2[s][:], in0=h2[s][:], in1=b2bc[:], op=ALU.add),
             reads=[r_c], writes=[r_h2[s]])
        P.dma("pool", g.h2_d[r0:r0 + 128, :], h2[s][:], reads=[r_h2[s]], dres=r_h2[s])
        hv = h2[s][:].rearrange("p (q j) -> p j q", j=16)
        for rnd in range(4):
            t_ = tk % 2
            tk += 1
            for jj in range(4):
                j = rnd * 4 + jj
                P.op("pe", lambda e, t_=t_, jj=jj, j=j, hv=hv: e.transpose(out=tp[t_][:, jj * 128:(jj + 1) * 128],
                                                                        in_=hv[:, j, :], identity=ident[:]),
                     reads=[r_h2[s], r_c], writes=[r_tp[t_]] if jj == 0 else (), wadd=() if jj == 0 else [r_tp[t_]])
            outv = h2T[s][:, rnd * 4:(rnd + 1) * 4, :]
            inv_ = tp[t_][:].rearrange("p (a b) -> p a b", a=4)
            if rnd == 0:
                P.new_epoch(("dve", "act"), r_h2T[s])
            if rnd % 2 == 0:
                P.op("dve", lambda e, outv=outv, inv_=inv_: e.tensor_copy(out=outv, in_=inv_), reads=[r_tp[t_]], wadd=[r_h2T[s]])
            else:
                P.op("act", lambda e, outv=outv, inv_=inv_: e.activation(out=outv, in_=inv_, func=AF.Copy),
                     reads=[r_tp[t_]], wadd=[r_h2T[s]])
        for j in range(16):
            P.op("pe", lambda e, s=s, j=j: e.matmul(lgp[:, 0:36], h2T[s][:, j, :], wr[:, j, :], start=(j == 0), stop=(j == 15)),
                 reads=[r_h2T[s], r_c], writes=[r_lg] if j == 0 else (), wadd=() if j == 0 else [r_lg])
        P.op("dve", lambda e, i=i: e.tensor_tensor(out=lgsA[:, i, :], in0=lgp[:, 0:36], in1=rbb[:], op=ALU.add),
             reads=[r_lg, r_c], wadd=[r_lgs])
    X = lambda eng, fn: P.op(eng, fn, reads=[r_s, r_lgs], writes=[r_s])
    glv = lgsA[:, :, 0:4]
    elv = lgsA[:, :, 4:36].rearrange("p n (g e) -> p n g e", g=4)
    bcn = lambda t, k: t[:, :].unsqueeze(2).to_broadcast([128, NT, k])
    X("pool", lambda e: e.iota(io8[:], [[1, 8]], base=0, channel_multiplier=0, allow_small_or_imprecise_dtypes=True))
    io4b = bass.AP(io8, 0, [[8, 128], [0, NT], [1, 4]])
    io8b = bass.AP(io8, 0, [[8, 128], [0, NT], [1, 8]])
    X("dve", lambda e: e.tensor_reduce(out=gmax[:], in_=glv, axis=AX.X, op=ALU.max))
    X("dve", lambda e: e.tensor_tensor(out=oh4[:], in0=glv, in1=bcn(gmax, 4), op=ALU.is_equal))
    X("dve", lambda e: e.tensor_tensor(out=ex4[:], in0=glv, in1=bcn(gmax, 4), op=ALU.subtract))
    X("act", lambda e: e.activation(out=ex4[:], in_=ex4[:], func=AF.Exp))
    X("dve", lambda e: e.tensor_reduce(out=sume[:], in_=ex4[:], axis=AX.X, op=ALU.add))
    X("dve", lambda e: e.reciprocal(out=pg[:], in_=sume[:]))
    X("dve", lambda e: e.tensor_tensor(out=ex4[:], in0=oh4[:], in1=io4b, op=ALU.mult))
    X("dve", lambda e: e.tensor_reduce(out=gif[:], in_=ex4[:], axis=AX.X, op=ALU.add))
    X("dve", lambda e: e.tensor_tensor(out=tmp48[:], in0=elv, in1=oh4[:, :, :].unsqueeze(3).to_broadcast([128, NT, 4, 8]),
                                       op=ALU.mult))
    X("dve", lambda e: e.tensor_reduce(out=els[:], in_=tmp48[:].rearrange("p n g e -> p n e g"), axis=AX.X, op=ALU.add))
    X("dve", lambda e: e.tensor_reduce(out=top1[:], in_=els[:], axis=AX.X, op=ALU.max))
    X("dve", lambda e: e.tensor_tensor(out=mk1[:], in0=els[:], in1=bcn(top1, 8), op=ALU.is_equal))
    X("dve", lambda e: e.scalar_tensor_tensor(out=els2[:], in0=mk1[:], scalar=-1e30, in1=els[:], op0=ALU.mult, op1=ALU.add))
    X("dve", lambda e: e.tensor_reduce(out=top2[:], in_=els2[:], axis=AX.X, op=ALU.max))
    X("dve", lambda e: e.tensor_tensor(out=mk2[:], in0=els2[:], in1=bcn(top2, 8), op=ALU.is_equal))
    X("dve", lambda e: e.tensor_tensor(out=mk1[:], in0=mk1[:], in1=io8b, op=ALU.mult))
    X("dve", lambda e: e.tensor_tensor(out=mk2[:], in0=mk2[:], in1=io8b, op=ALU.mult))
    X("dve", lambda e: e.tensor_reduce(out=T.eidA[:, :, 0], in_=mk1[:], axis=AX.X, op=ALU.add))
    X("dve", lambda e: e.tensor_reduce(out=T.eidA[:, :, 1], in_=mk2[:], axis=AX.X, op=ALU.add))
    X("dve", lambda e: e.tensor_scalar(out=gif[:], in0=gif[:], scalar1=8.0, scalar2=None, op0=ALU.mult))
    X("dve", lambda e: e.tensor_tensor(out=T.eidA[:], in0=T.eidA[:], in1=bcn(gif, 2), op=ALU.add))
    X("dve", lambda e: e.tensor_tensor(out=dd[:], in0=top2[:], in1=top1[:], op=ALU.subtract))
    X("act", lambda e: e.activation(out=dd[:], in_=dd[:], func=AF.Exp))
    X("dve", lambda e: e.tensor_scalar(out=w0[:], in0=dd[:], scalar1=1.0, scalar2=None, op0=ALU.add))
    X("dve", lambda e: e.reciprocal(out=w0[:], in_=w0[:]))
    X("dve", lambda e: e.tensor_tensor(out=dd[:], in0=dd[:], in1=w0[:], op=ALU.mult))
    X("dve", lambda e: e.tensor_tensor(out=T.gateA[:, :, 0], in0=w0[:], in1=pg[:], op=ALU.mult))
    X("dve", lambda e: e.tensor_tensor(out=T.gateA[:, :, 1], in0=dd[:], in1=pg[:], op=ALU.mult))

def phase_route(P, nc, es, g, l, S, T):
    sb = lambda n, s, d: es.enter_context(nc.sbuf_tensor(_nm(n), s, d))
    pst = lambda n: es.enter_context(nc.psum_tensor(_nm(n), [128, 512], F32))
    R = lambda n="": Res(n)
    NT = S // 128
    NBLK = (2 * S) // BS + NEXP
    NSLOT = NBLK * BS
    JM = S // BS
    iota32 = sb("iota32", [128, 32], F32)
    thr = sb("thr", [128, JM], F32)
    biota = sb("biota", [128, NBLK], F32)
    oh = sb("oh", [128, NT, 2, 32], F32)
    M = sb("M", [128, NT, 32], F32)
    Mb = sb("Mb", [128, NT, 32], BF16)
    U = sb("U", [128, 128], BF16)
    ones = sb("onesr", [128, 128], BF16)
    onesf = sb("onesf", [128, 32], F32)
    rank = sb("rank", [128, NT, 32], F32)
    Rtot = sb("Rtot", [128, 32], F32)
    cmp_ = sb("cmp", [128, 32, JM], F32)
    nbk = sb("nbk", [128, 32], F32)
    pendb = sb("pendb", [128, 32], F32)
    pstart = sb("pstart", [128, 32], F32)
    tmp4 = sb("tmp4", [128, NT, 2, 32], F32)
    destf = sb("destf", [128, NT, 2], F32)
    cmpb = sb("cmpb", [128, NBLK, 32], F32)
    bexf = sb("bexf", [128, NBLK], F32)
    tokid = sb("tokid", [128, NT], I32)
    initt = sb("initt", [128, NSLOT // 128], I32)
    wps = [pst("wps%d" % i) for i in range(2)]
    r_s = R()
    r_M = R()
    r_w = [R(), R()]
    r_rank = R()
    X = lambda eng, fn, extra=(), wr=None: P.op(eng, fn, reads=[r_s] + list(extra), writes=[r_s] if wr is None else wr)
    X("pool", lambda e: e.iota(iota32[:], [[1, 32]], base=0, channel_multiplier=0, allow_small_or_imprecise_dtypes=True))
    X("pool", lambda e: e.iota(thr[:], [[BS, JM]], base=0, channel_multiplier=0, allow_small_or_imprecise_dtypes=True))
    X("pool", lambda e: e.iota(biota[:], [[1, NBLK]], base=0, channel_multiplier=0, allow_small_or_imprecise_dtypes=True))
    X("pool", lambda e: e.iota(tokid[:], [[128, NT]], base=0, channel_multiplier=1))
    X("pool", lambda e: e.memset(initt[:], S))
    X("pool", lambda e: e.memset(ones[:], 1.0))
    X("pool", lambda e: e.memset(onesf[:], 1.0))
    X("pool", lambda e: e.memset(Rtot[:], 0.0))
    X("pool", lambda e: e.affine_select(out=U[:], in_=ones[:], pattern=[[1, 128]], compare_op=ALU.is_ge, fill=0.0,
                                        base=-1, channel_multiplier=-1))
    P.dma("sp", g.slot_tok_d.rearrange("(p j) o -> p (j o)", p=128), initt[:], reads=[r_s], dres=r_s)
    iob = bass.AP(iota32, 0, [[32, 128], [0, NT], [0, 2], [1, 32]])
    X("dve", lambda e: e.tensor_tensor(out=oh[:], in0=T.eidA[:, :, :].unsqueeze(3).to_broadcast([128, NT, 2, 32]),
                                       in1=iob, op=ALU.is_equal))
    X("dve", lambda e: e.tensor_tensor(out=M[:], in0=oh[:, :, 0, :], in1=oh[:, :, 1, :], op=ALU.add))
    X("dve", lambda e: e.tensor_copy(out=Mb[:], in_=M[:]), wr=[r_s, r_M])
    for i in range(NT):
        w_ = i % 2
        P.op("pe", lambda e, w_=w_, i=i: e.matmul(wps[w_][:, 0:32], U[:], Mb[:, i, :], start=True, stop=True),
             reads=[r_M, r_s], writes=[r_w[w_]])
        P.op("pe", lambda e, w_=w_, i=i: e.matmul(wps[w_][:, 32:64], ones[:], Mb[:, i, :], start=True, stop=True),
             reads=[r_M, r_s], wadd=[r_w[w_]])
        X("dve", lambda e, w_=w_, i=i: e.tensor_tensor(out=rank[:, i, :], in0=wps[w_][:, 0:32], in1=Rtot[:], op=ALU.add),
          extra=[r_w[w_]])
        X("dve", lambda e, w_=w_: e.tensor_tensor(out=Rtot[:], in0=wps[w_][:, 32:64], in1=Rtot[:], op=ALU.add),
          extra=[r_w[w_]])
    X("dve", lambda e: e.tensor_tensor(out=cmp_[:], in0=Rtot[:, :].unsqueeze(2).to_broadcast([128, 32, JM]),
                                       in1=bass.AP(thr, 0, [[JM, 128], [0, 32], [1, JM]]), op=ALU.is_gt))
    X("dve", lambda e: e.tensor_reduce(out=nbk[:], in_=cmp_[:], axis=AX.X, op=ALU.add))
    X("dve", lambda e: e.tensor_tensor_scan(out=pendb[:], data0=onesf[:], data1=nbk[:], initial=0.0, op0=ALU.mult, op1=ALU.add))
    X("dve", lambda e: e.tensor_tensor(out=pstart[:], in0=pendb[:], in1=nbk[:], op=ALU.subtract))
    X("dve", lambda e: e.tensor_scalar(out=pstart[:], in0=pstart[:], scalar1=float(BS), scalar2=None, op0=ALU.mult))
    X("dve", lambda e: e.tensor_tensor(out=rank[:], in0=rank[:], in1=bass.AP(pstart, 0, [[32, 128], [0, NT], [1, 32]]),
                                       op=ALU.add))
    X("dve", lambda e: e.tensor_tensor(out=tmp4[:], in0=oh[:],
                                       in1=bass.AP(rank, 0, [[NT * 32, 128], [32, NT], [0, 2], [1, 32]]), op=ALU.mult))
    X("dve", lambda e: e.tensor_reduce(out=destf[:], in_=tmp4[:], axis=AX.X, op=ALU.add))
    X("dve", lambda e: e.tensor_copy(out=T.destA[:], in_=destf[:]))
    X("dve", lambda e: e.tensor_tensor(out=cmpb[:], in0=bass.AP(pendb, 0, [[32, 128], [0, NBLK], [1, 32]]),
                                       in1=biota[:, :].unsqueeze(2).to_broadcast([128, NBLK, 32]), op=ALU.is_le))
    X("dve", lambda e: e.tensor_reduce(out=bexf[:], in_=cmpb[:], axis=AX.X, op=ALU.add))
    X("dve", lambda e: e.tensor_scalar(out=bexf[:], in0=bexf[:], scalar1=31.0, scalar2=None, op0=ALU.min))
    X("dve", lambda e: e.tensor_copy(out=T.blkexp[:], in_=bexf[:]))
    r_sc = R()
    for i in range(NT):
        for k in range(2):
            P.raw("pool", lambda e, i=i, k=k: e.indirect_dma_start(
                out=g.slot_tok_d, out_offset=bass.IndirectOffsetOnAxis(ap=T.destA[:, i, k:k + 1], axis=0),
                in_=tokid[:, i:i + 1], in_offset=None),
                dres=r_sc, reads=[r_s])


def phase_experts(P, nc, es, g, l, S, T):
    sb = lambda n, s, d: es.enter_context(nc.sbuf_tensor(_nm(n), s, d))
    pst = lambda n: es.enter_context(nc.psum_tensor(_nm(n), [128, 512], F32))
    R = lambda n="": Res(n)
    NBLK = (2 * S) // BS + NEXP
    ident = sb("idente", [128, 128], F32)
    idx = [sb("idx%d" % i, [128, 2], I32) for i in range(2)]
    xg = [sb("xg%d" % i, [128, D], F32) for i in range(2)]
    xbT = [sb("xbT%d" % i, [128, 16, BS], F32R) for i in range(2)]
    wgu = [sb("wgu%d" % i, [128, 16, 2, 256], F32R) for i in range(2)]
    wdn = [sb("wdn%d" % i, [128, 8, 512], F32R) for i in range(2)]
    sa = [sb("sa%d" % i, [128, BS], F32) for i in range(2)]
    hid = [sb("hid%d" % i, [128, EH], F32) for i in range(2)]
    hT = [sb("hTe%d" % i, [128, 8, BS], F32R) for i in range(2)]
    yb = [sb("yb%d" % i, [128, D], F32) for i in range(2)]
    tp = [pst("tpe%d" % i) for i in range(2)]
    gu = [pst("gu%d" % i) for i in range(2)]
    dn = [pst("dn%d" % i) for i in range(2)]
    r_c = R()
    r_idx = [R(), R()]
    r_xg = [R(), R()]
    r_xbT = [[[R() for r in range(4)] for s in range(2)] for b in range(2)]
    r_wgu = [R(), R()]
    r_wdn = [R(), R()]
    r_sa = [R(), R()]
    r_hid = [[R() for q in range(4)] for s in range(2)]
    r_hT2 = [[[R() for r in range(2)] for s in range(2)] for b in range(2)]
    r_yb = [R(), R()]
    r_tp = [R(), R()]
    r_gu = [R(), R()]
    r_dn = [R(), R()]
    P.dma("sp", ident[:], g.ident, writes=[r_c], dres=r_c)
    GU_E = D * D
    DN_E = EH * D
    if not hasattr(g, "regs"):
        g.regs = {}
    regs = g.regs

    def getreg(e, name):
        if name not in regs:
            regs[name] = e.alloc_register(name)
        return regs[name]
    gk = wk = dk = tk = uk = nk = 0
    for b in range(NBLK):
        bs_ = b % 2
        P.dma("sp", idx[bs_][:], g.slot_tok_d[b * BS:(b + 1) * BS, :].rearrange("(s p) o -> p (s o)", p=128),
              writes=[r_idx[bs_]], dres=r_idx[bs_], allow_slow_non_contiguous=True)
        for s in range(2):
            x_ = gk % 2
            gk += 1
            P.raw("pool", lambda e, x_=x_, bs_=bs_, s=s: e.indirect_dma_start(
                out=xg[x_][:], out_offset=None, in_=g.h2_d,
                in_offset=bass.IndirectOffsetOnAxis(ap=idx[bs_][:, s:s + 1], axis=0)),
                dres=r_xg[x_], reads=[r_idx[bs_]], writes=[r_xg[x_]])
            xv = xg[x_][:].rearrange("p (q j) -> p j q", j=16)
            for rnd in range(4):
                t_ = tk % 2
                tk += 1
                for jj in range(4):
                    j = rnd * 4 + jj
                    P.op("pe", lambda e, t_=t_, jj=jj, j=j, xv=xv: e.transpose(out=tp[t_][:, jj * 128:(jj + 1) * 128],
                                                                            in_=xv[:, j, :], identity=ident[:]),
                         reads=[r_xg[x_], r_c], writes=[r_tp[t_]] if jj == 0 else (), wadd=() if jj == 0 else [r_tp[t_]])
                outv = xbT[bs_][:, rnd * 4:(rnd + 1) * 4, s * 128:(s + 1) * 128]
                inv_ = tp[t_][:].rearrange("p (a b) -> p a b", a=4)
                if rnd % 2 == 0:
                    P.op("dve", lambda e, outv=outv, inv_=inv_: e.tensor_copy(out=outv, in_=inv_),
                         reads=[r_tp[t_]], writes=[r_xbT[bs_][s][rnd]])
                else:
                    P.op("act", lambda e, outv=outv, inv_=inv_: e.activation(out=outv, in_=inv_, func=AF.Copy),
                         reads=[r_tp[t_]], writes=[r_xbT[bs_][s][rnd]])
        for q in range(4):
            w_ = wk % 2
            wk += 1

            def ld_gu(e, b=b, q=q, w_=w_):
                rb = getreg(e, "r_eb")
                ro = getreg(e, "r_off")
                e.reg_load(rb, T.blkexp[0:1, b:b + 1])
                e.reg_mul(ro, rb, GU_E)
                e.reg_add(ro, ro, l * NEXP * GU_E + q * 256)
                src = bass.AP(g.w_gu.tensor, ro, [[16 * D, 128], [D, 16], [1024, 2], [1, 256]]).bitcast(F32R)
                return dyn_dma(e, wgu[w_][:], src, ro.name)
            P.raw("sp", ld_gu, dres=r_wgu[w_], writes=[r_wgu[w_]])
            for s in range(2):
                u_ = uk % 2
                uk += 1
                for j in range(16):
                    P.op("pe", lambda e, u_=u_, w_=w_, s=s, j=j, bs_=bs_: e.matmul(
                        gu[u_][:], xbT[bs_][:, j, s * 128:(s + 1) * 128],
                        wgu[w_][:, j, :, :].rearrange("p a c -> p (a c)"), start=(j == 0), stop=(j == 15)),
                        reads=[r_wgu[w_], r_xbT[bs_][s][j // 4]],
                        writes=[r_gu[u_]] if j == 0 else (), wadd=() if j == 0 else [r_gu[u_]])
                a_ = nk % 2
                nk += 1
                P.op("act", lambda e, a_=a_, u_=u_: e.activation(out=sa[a_][:], in_=gu[u_][:, 0:256], func=AF.Silu),
                     reads=[r_gu[u_]], writes=[r_sa[a_]])
                P.op("dve", lambda e, a_=a_, u_=u_, s=s, q=q: e.tensor_tensor(
                    out=hid[s][:, q * 256:(q + 1) * 256], in0=sa[a_][:], in1=gu[u_][:, 256:512], op=ALU.mult),
                    reads=[r_sa[a_], r_gu[u_]], writes=[r_hid[s][q]])
        for s in range(2):
            for rnd in range(2):
                t_ = tk % 2
                tk += 1
                for jj in range(4):
                    c_ = rnd * 4 + jj
                    P.op("pe", lambda e, t_=t_, jj=jj, c_=c_, s=s: e.transpose(
                        out=tp[t_][:, jj * 128:(jj + 1) * 128], in_=hid[s][:, c_ * 128:(c_ + 1) * 128], identity=ident[:]),
                        reads=[r_hid[s][c_ // 2], r_c], writes=[r_tp[t_]] if jj == 0 else (), wadd=() if jj == 0 else [r_tp[t_]])
                outv = hT[bs_][:, rnd * 4:(rnd + 1) * 4, s * 128:(s + 1) * 128]
                inv_ = tp[t_][:].rearrange("p (a b) -> p a b", a=4)
                if rnd % 2 == 0:
                    P.op("dve", lambda e, outv=outv, inv_=inv_: e.tensor_copy(out=outv, in_=inv_),
                         reads=[r_tp[t_]], writes=[r_hT2[bs_][s][rnd]])
                else:
                    P.op("act", lambda e, outv=outv, inv_=inv_: e.activation(out=outv, in_=inv_, func=AF.Copy),
                         reads=[r_tp[t_]], writes=[r_hT2[bs_][s][rnd]])
        for q in range(4):
            d_ = dk % 2
            dk += 1

            def ld_dn(e, b=b, q=q, d_=d_):
                rb = getreg(e, "r_eb")
                ro = getreg(e, "r_off")
                e.reg_load(rb, T.blkexp[0:1, b:b + 1])
                e.reg_mul(ro, rb, DN_E)
                e.reg_add(ro, ro, l * NEXP * DN_E + q * 512)
                src = bass.AP(g.w_dn.tensor, ro, [[D, 128], [128 * D, 8], [1, 512]]).bitcast(F32R)
                return dyn_dma(e, wdn[d_][:], src, ro.name)
            P.raw("sp", ld_dn, dres=r_wdn[d_], writes=[r_wdn[d_]])
            for s in range(2):
                o_ = (dk + s) % 2
                if q == 0:
                    P.new_epoch(("act", "dve"), r_yb[s])
                for c_ in range(8):
                    P.op("pe", lambda e, o_=o_, c_=c_, s=s, d_=d_, bs_=bs_: e.matmul(
                        dn[o_][:], hT[bs_][:, c_, s * 128:(s + 1) * 128], wdn[d_][:, c_, :], start=(c_ == 0), stop=(c_ == 7)),
                        reads=[r_wdn[d_], r_hT2[bs_][s][c_ // 4]],
                        writes=[r_dn[o_]] if c_ == 0 else (), wadd=() if c_ == 0 else [r_dn[o_]])
                if s == 0:
                    P.op("act", lambda e, o_=o_, s=s, q=q: e.activation(out=yb[s][:, q * 512:(q + 1) * 512], in_=dn[o_][:], func=AF.Copy),
                         reads=[r_dn[o_]], wadd=[r_yb[s]])
                else:
                    P.op("dve", lambda e, o_=o_, s=s, q=q: e.tensor_copy(out=yb[s][:, q * 512:(q + 1) * 512], in_=dn[o_][:]),
                         reads=[r_dn[o_]], wadd=[r_yb[s]])
        for s in range(2):
            r0 = b * BS + s * 128
            P.dma("pool", g.yslot_d[r0:r0 + 128, :], yb[s][:], reads=[r_yb[s]], dres=r_yb[s])


def phase_combine(P, nc, es, g, l, S, T, x_dst):
    sb = lambda n, s, d: es.enter_context(nc.sbuf_tensor(_nm(n), s, d))
    R = lambda n="": Res(n)
    NT = S // 128
    NB = 3
    g2bc = sb("g2bc", [128, D], F32)
    y0 = [sb("y0_%d" % i, [128, D], F32) for i in range(NB)]
    y1 = [sb("y1_%d" % i, [128, D], F32) for i in range(NB)]
    xt = [sb("xc%d" % i, [128, D], F32) for i in range(NB)]
    ac = [sb("ac%d" % i, [128, D], F32) for i in range(2)]
    r_c = R()
    r_y0, r_y1, r_x = [R() for _ in range(NB)], [R() for _ in range(NB)], [R() for _ in range(NB)]
    r_ac = [R(), R()]
    P.dma("sp", g2bc[:], bc(g.mod_d[l, 5 * D:6 * D]), writes=[r_c], dres=r_c)

    def loads(i):
        s = i % NB
        r0 = i * 128
        P.raw("pool", lambda e, s=s, i=i: e.indirect_dma_start(
            out=y0[s][:], out_offset=None, in_=g.yslot_d,
            in_offset=bass.IndirectOffsetOnAxis(ap=T.destA[:, i, 0:1], axis=0)),
            dres=r_y0[s], writes=[r_y0[s]])
        P.raw("pool", lambda e, s=s, i=i: e.indirect_dma_start(
            out=y1[s][:], out_offset=None, in_=g.yslot_d,
            in_offset=bass.IndirectOffsetOnAxis(ap=T.destA[:, i, 1:2], axis=0)),
            dres=r_y1[s], writes=[r_y1[s]])
        P.dma("sp", xt[s][:], g.x1_d[r0:r0 + 128, :], writes=[r_x[s]], dres=r_x[s])
    loads(0)
    if NT > 1:
        loads(1)
    for i in range(NT):
        if i + 2 < NT:
            loads(i + 2)
        s = i % NB
        a = i % 2
        r0 = i * 128
        P.op("act", lambda e, s=s, i=i: e.activation(out=y0[s][:], in_=y0[s][:], func=AF.Copy, scale=T.gateA[:, i, 0:1]),
             reads=[r_y0[s]], writes=[r_y0[s]])
        P.op("dve", lambda e, s=s, a=a, i=i: e.scalar_tensor_tensor(out=ac[a][:], in0=y1[s][:], scalar=T.gateA[:, i, 1:2], in1=y0[s][:],
                                                                   op0=ALU.mult, op1=ALU.add),
             reads=[r_y1[s], r_y0[s]], writes=[r_ac[a]])
        P.op("dve", lambda e, a=a: e.tensor_tensor(out=ac[a][:], in0=ac[a][:], in1=g2bc[:], op=ALU.mult),
             reads=[r_c], writes=[r_ac[a]])
        P.op("dve", lambda e, s=s, a=a: e.tensor_tensor(out=ac[a][:], in0=ac[a][:], in1=xt[s][:], op=ALU.add),
             reads=[r_x[s]], writes=[r_ac[a]])
        P.dma("sp", x_dst[r0:r0 + 128, :], ac[a][:], reads=[r_ac[a]], dres=r_ac[a])


_CONST_KEYS = ["ident", "PT", "blk64", "rot", "xitab", "decayT", "zeta", "maskT", "tril", "identb"]
_S = 4096
_NCORES = 8


def kernel(**inputs):
    S = _S
    nc = build(S, debug=False, layers=(0, 1))
    cst = host_consts(S)
    x = np.ascontiguousarray(np.asarray(inputs["x"], dtype=np.float32))
    c = np.ascontiguousarray(np.asarray(inputs["c"], dtype=np.float32))
    shared = {}
    for k, v in inputs.items():
        if k in ("x", "c"):
            continue
        shared[k] = np.ascontiguousarray(np.asarray(v, dtype=np.float32))
    for k in _CONST_KEYS:
        shared[k] = cst[k]
    in_maps = []
    for b in range(_NCORES):
        m = dict(shared)
        m["x"] = x[b]
        m["c"] = c[b]
        in_maps.append(m)
    res = run_bass_kernel_spmd(nc, in_maps, core_ids=list(range(_NCORES)))
    out = np.stack([np.asarray(r["out"]) for r in res.results], axis=0)
    return out.astype(np.float32)
```

```python
import numpy as np
from contextlib import ExitStack
import concourse.bass as bass
import concourse.mybir as mybir
from concourse.bass_utils import run_bass_kernel_spmd

F32 = mybir.dt.float32
F32R = mybir.dt.float32r
BF16 = mybir.dt.bfloat16
I32 = mybir.dt.int32
U32 = mybir.dt.uint32
AF = mybir.ActivationFunctionType
ALU = mybir.AluOpType
AX = mybir.AxisListType


class Sem:
    _n = 0

    def __init__(self, nc, name):
        self.h = nc.alloc_semaphore(name=name)
        self.cnt = 0
        Sem._n += 1
        self.key = Sem._n


class Res:
    __slots__ = ("name", "w", "r", "dsem")

    def __init__(self, name=""):
        self.name = name
        self.w = {}
        self.r = {}
        self.dsem = None


def _merge(d, ev):
    s, v = ev
    o = d.get(s.key)
    if o is None or o[1] < v:
        d[s.key] = ev


class Prog:
    ENGS = ("sp", "pe", "act", "dve", "pool")
    ATTR = {"sp": "sync", "pe": "tensor", "act": "scalar", "dve": "vector", "pool": "gpsimd"}

    def __init__(self, nc):
        self.nc = nc
        self.q = {e: [] for e in self.ENGS}
        self.esem = {e: Sem(nc, "es_" + e) for e in ("pe", "act", "dve", "pool")}
        self.waited = {}
        self.dfree = []
        self.dall = []
        self.phase_dres = []
        self.ninst = 0
        self.cond = None
        self.cregs = {}

    def _wait(self, E, evs, skip_own=False):
        own = self.esem.get(E)
        for k, (s, v) in evs.items():
            if skip_own and s is own:
                continue
            kk = (E, k)
            if self.waited.get(kk, 0) >= v:
                continue
            self.waited[kk] = v
            self.q[E].append(("w", s, v))

    @staticmethod
    def _deps(reads, writes, wadd=()):
        evs = {}
        for r in reads:
            for ev in r.w.values():
                _merge(evs, ev)
        for w in writes:
            for ev in w.w.values():
                _merge(evs, ev)
            for ev in w.r.values():
                _merge(evs, ev)
        for w in wadd:
            for ev in w.r.values():
                _merge(evs, ev)
        return evs

    def op(self, E, fn, reads=(), writes=(), wadd=(), skip_own=None):
        if skip_own is None:
            skip_own = (E == "pe")
        self._wait(E, self._deps(reads, writes, wadd), skip_own)
        s = self.esem[E]
        s.cnt += 1
        self.q[E].append(("i", fn, s, 1))
        ev = (s, s.cnt)
        for r in reads:
            _merge(r.r, ev)
        for w in writes:
            w.w = {s.key: ev}
            w.r = {}
        for w in wadd:
            _merge(w.w, ev)
        self.ninst += 1
        return ev

    def new_epoch(self, engines, res):
        evs = self._deps((), (res,))
        for E in engines:
            self._wait(E, evs, skip_own=False)
        res.w = {}
        res.r = {}

    def _get_dsem(self):
        if self.dfree:
            return self.dfree.pop()
        s = Sem(self.nc, "ds%d" % len(self.dall))
        self.dall.append(s)
        return s

    def dma(self, Q, out, in_, reads=(), writes=(), wadd=(), dres=None, **kw):
        assert dres is not None
        self._wait(Q, self._deps(reads, writes, wadd))
        if dres.dsem is None:
            dres.dsem = self._get_dsem()
            self.phase_dres.append(dres)
        s = dres.dsem
        s.cnt += 16
        if self.cond is not None:
            self.cond["dma"][(s.key, Q)] = self.cond["dma"].get((s.key, Q), 0) + 16
            self.cond["sems"][s.key] = s
        nc = self.nc

        def fn(eng, out=out, in_=in_, kw=kw):
            return eng.dma_start(out=out, in_=in_, **kw)
        self.q[Q].append(("i", fn, s, 16))
        ev = (s, s.cnt)
        for r in reads:
            _merge(r.r, ev)
        for w in writes:
            w.w = {s.key: ev}
            w.r = {}
        for w in wadd:
            _merge(w.w, ev)
        self.ninst += 1
        return ev

    def raw(self, Q, fn, dres, reads=(), writes=(), wadd=(), inc=16):
        self._wait(Q, self._deps(reads, writes, wadd))
        if dres.dsem is None:
            dres.dsem = self._get_dsem()
            self.phase_dres.append(dres)
        s = dres.dsem
        s.cnt += inc
        if self.cond is not None:
            self.cond["dma"][(s.key, Q)] = self.cond["dma"].get((s.key, Q), 0) + inc
            self.cond["sems"][s.key] = s
        self.q[Q].append(("i", fn, s, inc))
        ev = (s, s.cnt)
        for r in reads:
            _merge(r.r, ev)
        for w in writes:
            w.w = {s.key: ev}
            w.r = {}
        for w in wadd:
            _merge(w.w, ev)
        return ev

    def begin_cond(self, flag_ap):
        assert self.cond is None
        allsems = list(self.esem.values()) + self.dall
        self.cond = {"snap": {s_.key: s_.cnt for s_ in allsems}, "dma": {}, "sems": {},
                     "waited": dict(self.waited)}
        for E in self.ENGS:
            self.q[E].append(("cb", flag_ap))

    def end_cond(self):
        c = self.cond
        self.cond = None
        snap = c["snap"]
        for E in self.ENGS:
            tops = []
            if E in self.esem:
                s_ = self.esem[E]
                d = s_.cnt - snap.get(s_.key, 0)
                if d:
                    tops.append((s_, snap.get(s_.key, 0), d))
            for (k, Q), d in c["dma"].items():
                if Q == E:
                    s_ = c["sems"][k]
                    tops.append((s_, s_.cnt - sum(v for (kk, _q), v in c["dma"].items() if kk == k), d))
            self.q[E].append(("ce", tops))
        self.waited = c["waited"]

    def end_phase(self, drain_on="sp"):
        for r in self.phase_dres:
            s = r.dsem
            kk = (drain_on, s.key)
            if self.waited.get(kk, 0) < s.cnt:
                self.waited[kk] = s.cnt
                self.q[drain_on].append(("w", s, s.cnt))
        self.flush()
        for r in self.phase_dres:
            self.dfree.append(r.dsem)
            r.dsem = None
            r.w = {}
            r.r = {}
        self.phase_dres = []
        allsems = list(self.esem.values()) + self.dall
        for E in self.ENGS:
            for s in allsems:
                self.waited[(E, s.key)] = s.cnt

    def flush(self):
        with self.nc.Block() as block:
            for E in self.ENGS:
                items = self.q[E]
                if not items:
                    continue

                def body(eng, items=items, E=E):
                    ctx = None
                    for it in items:
                        if it[0] == "w":
                            eng.wait_ge(it[1].h, it[2])
                        elif it[0] == "cb":
                            if E not in self.cregs:
                                self.cregs[E] = eng.alloc_register("cflag_" + E)
                            reg = self.cregs[E]
                            eng.reg_load(reg, it[1])
                            ctx = eng.If_ne(reg, 0)
                            ctx.__enter__()
                        elif it[0] == "ce":
                            ctx.__exit__(None, None, None)
                            c2 = eng.Else()
                            c2.__enter__()
                            for s_, pre, d in it[1]:
                                if pre > 0:
                                    eng.wait_ge(s_.h, pre)
                                eng.sem_inc(s_.h, d)
                            c2.__exit__(None, None, None)
                            ctx = None
                        else:
                            inst = it[1](eng)
                            inst.then_inc(it[2].h, it[3])
                getattr(block, self.ATTR[E])(body)
                self.q[E] = []


D = 2048
DIN = 6400
NMOD = 6 * D
EPS = 1e-6
NEXP = 32
EH = 1024


def host_consts(S):
    c = {}
    c["ident"] = np.eye(128, dtype=np.float32)
    PT = np.zeros((128, 128), np.float32)
    for m in range(64):
        PT[m + 64, m] = -1.0
        PT[m, m + 64] = 1.0
    c["PT"] = PT
    blk = np.zeros((128, 128), np.float32)
    blk[:64, :64] = 1.0 / 64
    blk[64:, 64:] = 1.0 / 64
    c["blk64"] = blk
    half = 64
    inv = 10000.0 ** (-np.arange(half, dtype=np.float32) / half)
    pos = np.arange(S, dtype=np.float32)
    ang = pos[None, :] * inv[:, None]
    cos = np.cos(ang).astype(np.float32)
    sin = np.sin(ang).astype(np.float32)
    cosT = np.concatenate([cos, cos], 0)
    sinT = np.concatenate([sin, sin], 0)
    ks = np.float32(128 ** -0.5)
    c["rot"] = np.stack([cosT, sinT, cosT * ks, sinT * ks], 0).astype(np.float32)
    gam = 1.0 - 2.0 ** (-5.0 - np.arange(6, dtype=np.float64))
    idx = np.arange(128, dtype=np.float64)
    rel = idx[None, :] - idx[:, None]
    dec = np.where(rel[None] >= 0, gam[:, None, None] ** np.maximum(rel, 0)[None], 0.0)
    c["decayT"] = np.ascontiguousarray(dec.transpose(1, 0, 2)).astype(np.float32)
    c["zeta"] = (gam[None, :] ** (127.0 - idx)[:, None]).astype(np.float32)
    xi = gam[:, None] ** (idx + 1.0)[None, :]
    xi512 = np.tile(xi, (1, 4))
    c["xitab"] = np.ascontiguousarray(np.broadcast_to(xi512[None], (128, 6, 512))).astype(np.float32)
    c["cdec"] = [float(g ** 128.0) for g in gam]
    c["maskT"] = (idx[None, :] >= idx[:, None]).astype(np.float32)
    c["tril"] = (idx[None, :] <= idx[:, None]).astype(np.float32)
    import ml_dtypes
    c["identb"] = np.eye(128).astype(ml_dtypes.bfloat16)
    return c


class Ctx:
    pass


def bc(ap_row, n=None):
    if n is None:
        n = 1
        for d in ap_row.shape:
            n *= d
    return bass.AP(ap_row.tensor, ap_row.offset, [[0, 128], [1, n]])


_uid = [0]


def _nm(n):
    _uid[0] += 1
    return "%s_u%d" % (n, _uid[0])


def phase_mod(P, nc, es, g):
    sb = lambda n, s, d: es.enter_context(nc.sbuf_tensor(_nm(n), s, d))
    c_sb = sb("c_sb", [128, 16], F32)
    c_act = sb("c_act", [128, 16], F32)
    ones = sb("ones_a", [128, 128], F32)
    c_rep = sb("c_rep", [128, 16, 128], F32R)
    wsl = [sb("adaw%d" % i, [128, 16, 512], F32R) for i in range(2)]
    bia = [sb("adab%d" % i, [1, 512], F32) for i in range(2)]
    row = [sb("adar%d" % i, [1, 512], F32) for i in range(2)]
    ps = [es.enter_context(nc.psum_tensor(_nm("adaps"), [128, 512], F32)) for i in range(2)]
    r_c, r_ca, r_ones, r_rep = Res("c"), Res("ca"), Res("ones"), Res("rep")
    r_w = [Res("w0"), Res("w1")]
    r_b = [Res("b0"), Res("b1")]
    r_row = [Res("r0"), Res("r1")]
    r_ps = [Res("p0"), Res("p1")]

    P.dma("sp", c_sb[:], g.c.rearrange("(p j) -> p j", j=16), writes=[r_c], dres=r_c)
    P.op("act", lambda e: e.activation(out=c_act[:], in_=c_sb[:], func=AF.Silu), reads=[r_c], writes=[r_ca])
    P.op("dve", lambda e: e.memset(ones[:], 1.0), writes=[r_ones])
    for j in range(16):
        P.op("act", lambda e, j=j: e.activation(out=c_rep[:, j, :], in_=ones[:], func=AF.Copy,
                                                scale=c_act[:, j:j + 1]),
             reads=[r_ca, r_ones], wadd=[r_rep])
    k = 0
    for l in range(2):
        awv = g.ada_w[l].rearrange("(p j) n -> p j n", j=16).bitcast(F32R)
        for nb in range(NMOD // 512):
            s = k % 2
            k += 1
            P.dma("sp", wsl[s][:], awv[:, :, nb * 512:(nb + 1) * 512], writes=[r_w[s]], dres=r_w[s])
            P.dma("sp", bia[s][:], g.ada_b[l:l + 1, nb * 512:(nb + 1) * 512], writes=[r_b[s]], dres=r_b[s])
            for j in range(16):
                P.op("pe", lambda e, j=j, s=s: e.matmul(ps[s][:], c_rep[:, j, :], wsl[s][:, j, :],
                                                        start=(j == 0), stop=(j == 15)),
                     reads=[r_rep, r_w[s]], writes=[r_ps[s]] if j == 0 else (), wadd=() if j == 0 else [r_ps[s]])
            P.op("dve", lambda e, s=s: e.tensor_tensor(out=row[s][:], in0=ps[s][0:1, :], in1=bia[s][:], op=ALU.add),
                 reads=[r_ps[s], r_b[s]], writes=[r_row[s]])
            P.dma("pool", g.mod_d[l:l + 1, nb * 512:(nb + 1) * 512], row[s][:], reads=[r_row[s]], dres=r_row[s])


def phase_proj(P, nc, es, g, l, S, x_src=None):
    if x_src is None:
        x_src = g.x
    sb = lambda n, s, d: es.enter_context(nc.sbuf_tensor(_nm(n), s, d))
    pst = lambda n: es.enter_context(nc.psum_tensor(_nm(n), [128, 512], F32))
    NG = S // 512
    ident = sb("ident", [128, 128], F32)
    PT = sb("PT", [128, 128], F32R)
    blk = sb("blk", [128, 128], F32R)
    nw = sb("nw", [128, 16], F32)
    scm = sb("scm", [128, 16], F32)
    a1 = sb("a1", [128, 16], F32)
    b1 = sb("b1", [128, 16], F32)
    wqk = sb("wqk", [128, 2], F32)
    xt = [sb("xt%d" % i, [128, D], F32) for i in range(2)]
    junk = sb("junk", [128, D], BF16)
    ss = [sb("ss%d" % i, [128, 1], F32) for i in range(2)]
    sq = [sb("sq%d" % i, [128, 1], F32) for i in range(2)]
    rstd = [sb("rstd%d" % i, [128, 1], F32) for i in range(2)]
    hT = [sb("hT%d" % i, [128, 16, 512], F32R) for i in range(2)]
    wsl = [sb("win%d" % i, [128, 16, 512], F32R) for i in range(2)]
    tabs = [sb("tab%d" % i, [128, 4, 512], F32) for i in range(2)]
    qraw = [sb("qraw%d" % i, [128, 512], F32R) for i in range(2)]
    sqf = [sb("sqf%d" % i, [128, 512], F32R) for i in range(2)]
    t1 = [sb("t1_%d" % i, [128, 512], F32) for i in range(2)]
    t2 = [sb("t2_%d" % i, [128, 512], F32) for i in range(2)]
    ofm = [sb("ofm%d" % i, [128, 512], BF16) for i in range(2)]
    otk = [sb("otk%d" % i, [128, 512], BF16) for i in range(2)]
    oxi = [sb("oxi%d" % i, [128, 512], BF16) for i in range(2)]
    xitab = sb("xitab", [128, 6, 512], F32)
    tp = [pst("tp%d" % i) for i in range(2)]
    acc = [pst("acc%d" % i) for i in range(2)]
    aux = [pst("aux%d" % i) for i in range(2)]

    R = lambda n: Res(n)
    r_const = R("const")
    r_ab = R("ab")
    r_xt = [R("xt0"), R("xt1")]
    r_ss = [R("ss0"), R("ss1")]
    r_sq = [R("sq0"), R("sq1")]
    r_rstd = [R("rs0"), R("rs1")]
    r_junk = R("junk")
    r_tp = [R("tp0"), R("tp1")]
    r_hT = [[[R("hT") for j in range(16)] for i in range(4)] for s in range(2)]
    r_w = [R("w0"), R("w1")]
    r_tab = [R("tab0"), R("tab1")]
    r_acc = [R("acc0"), R("acc1")]
    r_aux = [R("aux0"), R("aux1")]
    r_qraw = [R("q0"), R("q1")]
    r_sqf = [R("sf0"), R("sf1")]
    r_t1 = [R("t10"), R("t11")]
    r_t2 = [R("t20"), R("t21")]
    r_ofm = [R("of0"), R("of1")]
    r_otk = [R("ot0"), R("ot1")]
    r_oxi = [R("ox0"), R("ox1")]

    P.dma("sp", xitab[:], g.xitab, wadd=[r_const], dres=r_const)
    P.dma("sp", ident[:], g.ident, wadd=[r_const], dres=r_const)
    P.dma("sp", PT[:], g.PT.bitcast(F32R), wadd=[r_const], dres=r_const)
    P.dma("sp", blk[:], g.blk64.bitcast(F32R), wadd=[r_const], dres=r_const)
    P.dma("sp", nw[:], g.mix_norm_w[l].rearrange("(p j) -> p j", j=16), wadd=[r_const], dres=r_const)
    P.dma("sp", scm[:], g.mod_d[l, D:2 * D].rearrange("(p j) -> p j", j=16), wadd=[r_const], dres=r_const)
    P.dma("sp", b1[:], g.mod_d[l, 0:D].rearrange("(p j) -> p j", j=16), wadd=[r_const], dres=r_const)
    for half in range(2):
        P.dma("sp", wqk[half * 64:(half + 1) * 64, 0:1], g.diff_q_norm_w[l].rearrange("(p o) -> p o", o=1),
              wadd=[r_const], dres=r_const)
        P.dma("sp", wqk[half * 64:(half + 1) * 64, 1:2], g.diff_k_norm_w[l].rearrange("(p o) -> p o", o=1),
              wadd=[r_const], dres=r_const)
    P.op("dve", lambda e: e.scalar_tensor_tensor(out=a1[:], in0=scm[:], scalar=1.0, in1=nw[:],
                                                 op0=ALU.add, op1=ALU.mult),
         reads=[r_const], writes=[r_ab])

    if getattr(g, "dbg_stage", 99) < 1:
        return
    winv = g.w_in[l].rearrange("(p j) n -> p j n", j=16).bitcast(F32R)
    acts_tok = {3: (AF.Copy, AF.Copy), 4: (AF.Copy, AF.Silu), 5: (AF.Silu, AF.Silu),
                9: (AF.Copy, AF.Copy), 10: (AF.Copy, AF.Gelu), 11: (AF.Gelu, AF.Gelu), 12: (AF.Gelu,)}
    wk = 0
    ak = 0
    fk = 0
    tk = 0
    xk = 0
    tpk = 0
    for gi in range(NG):
        gs = gi % 2
        t0 = gi * 512
        import os
        SKIP = os.environ.get("MK_SKIP", "")
        if "rot" not in SKIP:
            P.dma("sp", tabs[gs][:], g.rot[:, :, t0:t0 + 512].rearrange("f p t -> p f t"),
                  writes=[r_tab[gs]], dres=r_tab[gs])
        for i in range(4):
            xs = xk % 2
            xk += 1
            r0 = t0 + i * 128
            P.dma("sp", xt[xs][:], x_src[r0:r0 + 128, :], writes=[r_xt[xs]], dres=r_xt[xs])
            P.op("act", lambda e, xs=xs: e.activation(out=junk[:], in_=xt[xs][:], func=AF.Square,
                                                      accum_out=ss[xs][:]),
                 reads=[r_xt[xs]], writes=[r_junk, r_ss[xs]])
            P.op("act", lambda e, xs=xs: e.activation(out=sq[xs][:], in_=ss[xs][:], func=AF.Sqrt,
                                                      scale=1.0 / D, bias=g.eps_t[:, 0:1]),
                 reads=[r_ss[xs]], writes=[r_sq[xs]])
            P.op("dve", lambda e, xs=xs: e.reciprocal(out=rstd[xs][:], in_=sq[xs][:]),
                 reads=[r_sq[xs]], writes=[r_rstd[xs]])
            P.op("dve", lambda e, xs=xs: e.tensor_scalar(out=xt[xs][:], in0=xt[xs][:], scalar1=rstd[xs][:, 0:1],
                                                         scalar2=None, op0=ALU.mult),
                 reads=[r_rstd[xs], r_xt[xs]], writes=[r_xt[xs]])
            xv = xt[xs][:].rearrange("p (q j) -> p j q", j=16)
            for rnd in range(4):
                tb = tpk % 2
                tpk += 1
                for jj in range(4):
                    j = rnd * 4 + jj
                    P.op("pe", lambda e, tb=tb, jj=jj, j=j, xv=xv: e.transpose(out=tp[tb][:, jj * 128:(jj + 1) * 128],
                                                                             in_=xv[:, j, :], identity=ident[:]),
                         reads=[r_xt[xs], r_const], writes=[r_tp[tb]] if jj == 0 else (),
                         wadd=() if jj == 0 else [r_tp[tb]])
                for jj in range(4):
                    j = rnd * 4 + jj
                    eng = "dve"
                    if eng == "dve":
                        fn = lambda e, tb=tb, jj=jj, j=j, i=i, gs=gs: e.tensor_scalar(
                            out=hT[gs][:, j, i * 128:(i + 1) * 128], in0=tp[tb][:, jj * 128:(jj + 1) * 128],
                            scalar1=a1[:, j:j + 1], scalar2=b1[:, j:j + 1], op0=ALU.mult, op1=ALU.add)
                    else:
                        fn = lambda e, tb=tb, jj=jj, j=j, i=i, gs=gs: e.activation(
                            out=hT[gs][:, j, i * 128:(i + 1) * 128], in_=tp[tb][:, jj * 128:(jj + 1) * 128],
                            func=AF.Identity, scale=a1[:, j:j + 1], bias=b1[:, j:j + 1])
                    if "evac" in SKIP or ("evacact" in SKIP and eng == "act") or ("evacdve" in SKIP and eng == "dve"):
                        continue
                    P.op(eng, fn, reads=[r_tp[tb], r_ab, r_const], writes=[r_hT[gs][i][j]])
        if getattr(g, "dbg_stage", 99) < 2:
            continue
        for b in range(13):
            if getattr(g, "dbg_stage", 99) < 3 and b not in (0,):
                continue
            if getattr(g, "dbg_stage", 99) < 4 and b not in (0, 6):
                continue
            if getattr(g, "dbg_stage", 99) < 5 and b not in (0, 6, 3):
                continue
            ncol = 512 if b < 12 else 256
            ws = wk % 2
            wk += 1
            P.dma("sp", wsl[ws][:, :, 0:ncol], winv[:, :, b * 512:b * 512 + ncol], writes=[r_w[ws]], dres=r_w[ws])
            if b in (0, 1, 2, 6, 7, 8):
                for ct in range(4):
                    a = ak % 2
                    ak += 1
                    for j in range(16):
                        P.op("pe", lambda e, a=a, ws=ws, ct=ct, j=j, gs=gs: e.matmul(
                            acc[a][:], wsl[ws][:, j, ct * 128:(ct + 1) * 128], hT[gs][:, j, :],
                            start=(j == 0), stop=(j == 15)),
                            reads=[r_w[ws]] + [r_hT[gs][i][j] for i in range(4)],
                            writes=[r_acc[a]] if j == 0 else (), wadd=() if j == 0 else [r_acc[a]])
                    f = fk % 2
                    fk += 1
                    col0 = b * 512 + ct * 128
                    if b < 3:
                        isk = col0 >= 768
                        hh = (col0 - (768 if isk else 0)) // 128
                        P.op("act", lambda e, a=a, f=f: e.activation(out=qraw[f][:], in_=acc[a][:], func=AF.Copy),
                             reads=[r_acc[a]], writes=[r_qraw[f]])
                        P.op("pe", lambda e, f=f: e.matmul(aux[f][:], PT[:], qraw[f][:], start=True, stop=True),
                             reads=[r_const, r_qraw[f]], writes=[r_aux[f]])
                        ci = 2 if isk else 0
                        P.op("dve", lambda e, f=f, ci=ci, gs=gs: e.tensor_tensor(out=t1[f][:], in0=qraw[f][:].bitcast(F32),
                                                                        in1=tabs[gs][:, ci, :], op=ALU.mult),
                             reads=[r_qraw[f], r_tab[gs]], writes=[r_t1[f]])
                        P.op("dve", lambda e, f=f, ci=ci, gs=gs: e.tensor_tensor(out=t2[f][:], in0=aux[f][:],
                                                                        in1=tabs[gs][:, ci + 1, :], op=ALU.mult),
                             reads=[r_aux[f], r_tab[gs]], writes=[r_t2[f]])
                        P.op("pool", lambda e, f=f: e.tensor_tensor(out=ofm[f][:], in0=t1[f][:], in1=t2[f][:],
                                                                   op=ALU.add),
                             reads=[r_t1[f], r_t2[f]], writes=[r_ofm[f]])
                        P.dma("pool", g.retqk_d[hh, 1 if isk else 0, :, t0:t0 + 512], ofm[f][:],
                              reads=[r_ofm[f]], dres=r_ofm[f])
                        if not isk:
                            P.op("pool", lambda e, f=f, hh=hh: e.tensor_tensor(out=oxi[f][:], in0=ofm[f][:],
                                                                              in1=xitab[:, hh, :], op=ALU.mult),
                                 reads=[r_ofm[f], r_const], writes=[r_oxi[f]])
                            P.dma("pool", g.retqk_d[hh, 2, :, t0:t0 + 512], oxi[f][:],
                                  reads=[r_oxi[f]], dres=r_oxi[f])
                    else:
                        cc = col0 - 3072
                        isk = cc >= 768
                        hh = (cc - (768 if isk else 0)) // 128
                        P.op("act", lambda e, a=a, f=f: e.activation(out=qraw[f][:], in_=acc[a][:], func=AF.Copy),
                             reads=[r_acc[a]], writes=[r_qraw[f]])
                        P.op("act", lambda e, a=a, f=f: e.activation(out=sqf[f][:], in_=acc[a][:], func=AF.Square),
                             reads=[r_acc[a]], writes=[r_sqf[f]])
                        P.op("pe", lambda e, f=f: e.matmul(aux[f][:], blk[:], sqf[f][:], start=True, stop=True),
                             reads=[r_const, r_sqf[f]], writes=[r_aux[f]])
                        P.op("act", lambda e, f=f: e.activation(out=t1[f][:], in_=aux[f][:], func=AF.Sqrt,
                                                                bias=g.eps_t[:, 0:1]),
                             reads=[r_aux[f]], writes=[r_t1[f]])
                        P.op("dve", lambda e, f=f: e.reciprocal(out=t2[f][:], in_=t1[f][:]),
                             reads=[r_t1[f]], writes=[r_t2[f]])
                        wi = 1 if isk else 0
                        P.op("dve", lambda e, f=f, wi=wi: e.scalar_tensor_tensor(
                            out=ofm[f][:], in0=qraw[f][:].bitcast(F32), scalar=wqk[:, wi:wi + 1], in1=t2[f][:],
                            op0=ALU.mult, op1=ALU.mult),
                            reads=[r_qraw[f], r_t2[f], r_const], writes=[r_ofm[f]])
                        P.dma("pool", g.diffqk_d[hh, 1 if isk else 0, :, t0:t0 + 512], ofm[f][:],
                              reads=[r_ofm[f]], dres=r_ofm[f])
            else:
                tcol0 = (b * 512 - 1536) if b <= 5 else (b * 512 - 3072)
                for i in range(4):
                    a = ak % 2
                    ak += 1
                    for j in range(16):
                        P.op("pe", lambda e, a=a, ws=ws, i=i, j=j, ncol=ncol, gs=gs: e.matmul(
                            acc[a][:, 0:ncol], hT[gs][:, j, i * 128:(i + 1) * 128], wsl[ws][:, j, 0:ncol],
                            start=(j == 0), stop=(j == 15)),
                            reads=[r_w[ws], r_hT[gs][i][j]],
                            writes=[r_acc[a]] if j == 0 else (), wadd=() if j == 0 else [r_acc[a]])
                    o = tk % 2
                    tk += 1
                    P.new_epoch(("act",), r_otk[o])
                    for hf, func in enumerate(acts_tok[b]):
                        P.op("act", lambda e, a=a, o=o, hf=hf, func=func: e.activation(
                            out=otk[o][:, hf * 256:(hf + 1) * 256], in_=acc[a][:, hf * 256:(hf + 1) * 256], func=func),
                            reads=[r_acc[a]], wadd=[r_otk[o]])
                    r0 = t0 + i * 128
                    P.dma("pool", g.tok_d[r0:r0 + 128, tcol0:tcol0 + ncol], otk[o][:, 0:ncol],
                          reads=[r_otk[o]], dres=r_otk[o])


def declare_io(nc, S, debug):
    g = Ctx()
    inp = lambda n, s, d=F32: nc.dram_tensor(n, s, d, kind="ExternalInput").ap()
    scr = lambda n, s, d=F32: nc.dram_tensor(n, s, d, kind="ExternalOutput" if debug else "Internal").ap()
    g.x = inp("x", [S, D])
    g.c = inp("c", [D])
    g.ada_w = inp("ada_w", [2, D, NMOD])
    g.ada_b = inp("ada_b", [2, NMOD])
    g.mix_norm_w = inp("mix_norm_w", [2, D])
    g.w_in = inp("w_in", [2, D, DIN])
    g.diff_q_norm_w = inp("diff_q_norm_w", [2, 64])
    g.diff_k_norm_w = inp("diff_k_norm_w", [2, 64])
    g.ident = inp("ident", [128, 128])
    g.PT = inp("PT", [128, 128])
    g.blk64 = inp("blk64", [128, 128])
    g.rot = inp("rot", [4, 128, S])
    g.xitab = inp("xitab", [128, 6, 512])
    g.decayT = inp("decayT", [128, 6, 128])
    g.zeta = inp("zeta", [128, 6])
    g.maskT = inp("maskT", [128, 128])
    g.tril = inp("tril", [128, 128])
    g.identb = inp("identb", [128, 128], BF16)
    g.ret_norm_w = inp("ret_norm_w", [2, 768])
    g.diff_lambda = inp("diff_lambda", [2, 4, 64])
    g.diff_subln_w = inp("diff_subln_w", [2, 128])
    g.gmlp_norm_w = inp("gmlp_norm_w", [2, 512])
    g.gmlp_norm_b = inp("gmlp_norm_b", [2, 512])
    g.gmlp_ws = inp("gmlp_ws", [2, 4, 128, 128])
    g.gmlp_bs = inp("gmlp_bs", [2, 4, 128])
    g.cat_d = scr("cat_d", [S, D])
    g.w_out = inp("w_out", [2, D, D])
    g.ffn_norm_w = inp("ffn_norm_w", [2, D])
    g.router_group_w = inp("router_group_w", [2, D, 4])
    g.router_group_b = inp("router_group_b", [2, 4])
    g.router_expert_w = inp("router_expert_w", [2, D, 32])
    g.router_expert_b = inp("router_expert_b", [2, 32])
    g.w_gu = inp("expert_w_gate_up", [2, NEXP, D, 2 * EH])
    g.w_dn = inp("expert_w_down", [2, NEXP, EH, D])
    NBLK = (2 * S) // BS + NEXP
    g.x1_d = scr("x1_d", [S, D])
    g.h2_d = scr("h2_d", [S + 1, D])
    g.slot_tok_d = scr("slot_tok_d", [NBLK * BS, 1], I32)
    g.yslot_d = scr("yslot_d", [NBLK * BS, D])
    g.x2_d = scr("x2_d", [S, D])
    g.out = nc.dram_tensor("out", [S, D], F32, kind="ExternalOutput").ap()
    if debug:
        g.dbg_eid = scr("dbg_eid", [128, S // 128, 2])
        g.dbg_gate = scr("dbg_gate", [128, S // 128, 2])
        g.dbg_dest = scr("dbg_dest", [128, S // 128, 2], I32)
        g.dbg_bex = scr("dbg_bex", [128, NBLK], I32)
    g.mod_d = scr("mod_d", [2, NMOD])
    g.retqk_d = scr("retqk_d", [6, 3, 128, S], BF16)
    g.diffqk_d = scr("diffqk_d", [6, 2, 128, S], BF16)
    g.tok_d = scr("tok_d", [S, 3328], BF16)
    return g


def build(S, debug=True, layers=(0,), dbg_stage=99, phases=("A", "B", "C1", "C2", "D1", "D2", "E", "F", "G")):
    nc = bass.Bass("TRN2", target_bir_lowering=False)
    nc.dge_precook = False
    g = declare_io(nc, S, debug)
    g.dbg_stage = dbg_stage
    P = Prog(nc)
    with ExitStack() as es0:
        g.eps_t = es0.enter_context(nc.sbuf_tensor(_nm("eps_t"), [128, 1], F32))
        r_eps = Res("eps")
        P.op("dve", lambda e: e.memset(g.eps_t[:], EPS), writes=[r_eps])
        if "A" in phases:
            with ExitStack() as es:
                phase_mod(P, nc, es, g)
                P.end_phase()
        for l in layers:
            if "B" in phases:
                with ExitStack() as es:
                    phase_proj(P, nc, es, g, l, S, g.x if l == 0 else g.x2_d)
                    P.end_phase()
            if "C1" in phases:
                with ExitStack() as es:
                    phase_retgmlp(P, nc, es, g, l, S)
                    P.end_phase()
            if "C2" in phases:
                with ExitStack() as es:
                    phase_diff(P, nc, es, g, l, S)
                    P.end_phase()
            x_src = g.x if l == 0 else g.x2_d
            x_dst = g.x2_d if l == 0 else g.out
            if "D1" in phases:
                with ExitStack() as es:
                    phase_wout(P, nc, es, g, l, S, x_src)
                    P.end_phase()
            with ExitStack() as esT:
                T = Ctx()
                NT = S // 128
                NBLK = (2 * S) // BS + NEXP
                T.eidA = esT.enter_context(nc.sbuf_tensor(_nm("eidA"), [128, NT, 2], F32))
                T.gateA = esT.enter_context(nc.sbuf_tensor(_nm("gateA"), [128, NT, 2], F32))
                T.destA = esT.enter_context(nc.sbuf_tensor(_nm("destA"), [128, NT, 2], I32))
                T.blkexp = esT.enter_context(nc.sbuf_tensor(_nm("blkexp"), [128, NBLK], I32))
                T.blkused = esT.enter_context(nc.sbuf_tensor(_nm("blkused"), [128, NBLK], I32))
                if "D2" in phases:
                    with ExitStack() as es:
                        phase_norm2(P, nc, es, g, l, S, T)
                        P.end_phase()
                if "E" in phases:
                    with ExitStack() as es:
                        phase_route(P, nc, es, g, l, S, T)
                        P.end_phase()
                    if debug:
                        rd = Res()
                        P.dma("sp", g.dbg_eid, T.eidA[:], dres=rd)
                        P.dma("sp", g.dbg_gate, T.gateA[:], dres=rd)
                        P.dma("sp", g.dbg_dest, T.destA[:], dres=rd)
                        P.dma("sp", g.dbg_bex, T.blkexp[:], dres=rd)
                        P.end_phase()
                if "F" in phases:
                    with ExitStack() as es:
                        phase_experts(P, nc, es, g, l, S, T)
                        P.end_phase()
                if "G" in phases:
                    with ExitStack() as es:
                        phase_combine(P, nc, es, g, l, S, T, x_dst)
                        P.end_phase()
    return nc


GAM = [1.0 - 2.0 ** (-5.0 - h) for h in range(6)]
CDEC = [g_ ** 128.0 for g_ in GAM]


def phase_retgmlp(P, nc, es, g, l, S):
    sb = lambda n, s, d: es.enter_context(nc.sbuf_tensor(_nm(n), s, d))
    pst = lambda n, d=F32, w=512: es.enter_context(nc.psum_tensor(_nm(n), [128, w], d))
    NCH = S // 128
    R = lambda n="": Res(n)
    decayT = sb("decayT", [128, 6, 128], F32)
    zeta = sb("zeta", [128, 6], F32)
    identb = sb("identb", [128, 128], BF16)
    identf = sb("identf", [128, 128], F32)
    tril = sb("tril", [128, 128], F32)
    retw = sb("retw", [128, 768], F32)
    gw = sb("gw", [128, 512], F32)
    gb = sb("gb", [128, 512], F32)
    bs = sb("bs", [128, 4], F32)
    wraw = sb("wraw", [128, 4, 128], F32)
    WT = sb("WT", [128, 4, 128], BF16)
    qk = [sb("qk%d" % i, [128, 6, 3, 512], BF16) for i in range(2)]
    tokrow = [sb("tokrow%d" % i, [128, 3328], BF16) for i in range(2)]
    st32 = sb("st32", [128, 6, 128], F32)
    st16 = sb("st16", [128, 6, 128], BF16)
    A = [sb("A%d" % i, [128, 128], BF16) for i in range(2)]
    kz = [sb("kz%d" % i, [128, 128], BF16) for i in range(2)]
    bst = sb("bst", [128, 6, 6], F32)
    mv = sb("mv", [128, 6, 2], F32)
    sd = sb("sd", [128, 6], F32)
    rs = sb("rs", [128, 6], F32)
    catr = [sb("catr%d" % i, [128, 768], F32) for i in range(2)]
    wsg = [sb("wsg%d" % i, [128, 768], F32) for i in range(2)]
    gst = sb("gst", [128, 6], F32)
    gmv = sb("gmv", [128, 2], F32)
    gsd = sb("gsd", [128, 1], F32)
    grs = sb("grs", [128, 1], F32)
    vn = sb("vn", [128, 512], F32)
    vnb = [sb("vnb%d" % i, [128, 512], BF16) for i in range(2)]
    catg = [sb("catg%d" % i, [128, 512], F32) for i in range(2)]
    yA = pst("yA")
    yB = pst("yB")
    sTp = pst("sTp")
    kvp = pst("kvp")
    ktr = pst("ktr", BF16, 1024)
    fps = pst("fps")
    wtp = pst("wtp")

    r_c = R("const")
    r_W = R("W")
    r_WT = R("WT")
    r_qk = [R(), R()]
    r_tok = [R(), R()]
    r_st32 = [R() for _ in range(6)]
    r_st16 = [R() for _ in range(6)]
    r_A = [R(), R()]
    r_kz = [R(), R()]
    r_sT = [R(), R()]
    r_kv = [R(), R()]
    r_ktr = [R(), R()]
    r_y = [R() for _ in range(6)]
    r_bst, r_mv, r_sd, r_rs = R(), R(), R(), R()
    r_catr = [R(), R()]
    r_wsg = [R(), R()]
    r_gst, r_gmv, r_gsd, r_grs, r_vn = R(), R(), R(), R(), R()
    r_vnb = [R(), R()]
    r_f = R()
    r_catg = [R(), R()]
    r_wtp = R()

    for dst, src in ((decayT, g.decayT), (zeta, g.zeta), (identb, g.identb), (identf, g.ident), (tril, g.tril)):
        P.dma("sp", dst[:], src, wadd=[r_c], dres=r_c)
    P.dma("sp", retw[:], bc(g.ret_norm_w[l]), wadd=[r_c], dres=r_c)
    P.dma("sp", gw[:], bc(g.gmlp_norm_w[l]), wadd=[r_c], dres=r_c)
    P.dma("sp", gb[:], bc(g.gmlp_norm_b[l]), wadd=[r_c], dres=r_c)
    P.dma("sp", bs[:], g.gmlp_bs[l].rearrange("g t -> t g"), wadd=[r_c], dres=r_c, allow_slow_non_contiguous=True)
    P.dma("sp", wraw[:], g.gmlp_ws[l].rearrange("g t s -> t g s"), writes=[r_W], dres=r_W)
    for gg in range(4):
        P.op("dve", lambda e, gg=gg: e.tensor_tensor(out=wraw[:, gg, :], in0=wraw[:, gg, :], in1=tril[:], op=ALU.mult),
             reads=[r_c], writes=[r_W])
        P.op("pe", lambda e, gg=gg: e.transpose(out=wtp[:, gg * 128:(gg + 1) * 128], in_=wraw[:, gg, :], identity=identf[:]),
             reads=[r_W, r_c], writes=[r_wtp])
        P.op("act", lambda e, gg=gg: e.activation(out=WT[:, gg, :], in_=wtp[:, gg * 128:(gg + 1) * 128], func=AF.Copy),
             reads=[r_wtp], wadd=[r_WT])
    P.op("dve", lambda e: e.memset(st32[:], 0.0), writes=r_st32)
    P.op("dve", lambda e: e.memset(st16[:], 0.0), writes=r_st16)

    sk = 0
    for n in range(NCH):
        ts = n % 2
        if n % 4 == 0:
            qs = (n // 4) % 2
            t0 = n * 128
            P.dma("sp", qk[qs][:], g.retqk_d[:, :, :, t0:t0 + 512].rearrange("h f p t -> p h f t"),
                  writes=[r_qk[qs]], dres=r_qk[qs])
        cc = n % 4
        P.dma("sp", tokrow[ts][:], g.tok_d[n * 128:(n + 1) * 128, :], writes=[r_tok[ts]], dres=r_tok[ts])
        P.op("pool", lambda e, ts=ts: e.tensor_tensor(out=wsg[ts][:], in0=tokrow[ts][:, 768:1536], in1=retw[:], op=ALU.mult),
             reads=[r_tok[ts], r_c], writes=[r_wsg[ts]])
        for h in range(6):
            a = sk % 2
            sk += 1
            qc = qk[qs][:, h, 0, cc * 128:(cc + 1) * 128]
            kc = qk[qs][:, h, 1, cc * 128:(cc + 1) * 128]
            qx = qk[qs][:, h, 2, cc * 128:(cc + 1) * 128]
            vh = tokrow[ts][:, h * 128:(h + 1) * 128]
            ybank = yA if h < 4 else yB
            yreg = ybank[:, (h % 4) * 128:(h % 4 + 1) * 128]
            P.op("pe", lambda e, a=a, kc=kc, qc=qc: e.matmul(sTp[:, a * 128:(a + 1) * 128], kc, qc, start=True, stop=True),
                 reads=[r_qk[qs]], writes=[r_sT[a]])
            P.op("dve", lambda e, a=a, h=h: e.tensor_tensor(out=A[a][:], in0=sTp[:, a * 128:(a + 1) * 128],
                                                          in1=decayT[:, h, :], op=ALU.mult),
                 reads=[r_sT[a], r_c], writes=[r_A[a]])
            P.op("pe", lambda e, a=a, yreg=yreg, vh=vh: e.matmul(yreg, A[a][:], vh, start=True, stop=False),
                 reads=[r_A[a], r_tok[ts]], writes=[r_y[h]])
            P.op("pe", lambda e, yreg=yreg, qx=qx, h=h: e.matmul(yreg, qx, st16[:, h, :], start=False, stop=True),
                 reads=[r_qk[qs], r_st16[h]], wadd=[r_y[h]])
            P.op("pe", lambda e, a=a, kc=kc: e.transpose(out=ktr[:, a * 128:(a + 1) * 128], in_=kc, identity=identb[:]),
                 reads=[r_qk[qs], r_c], writes=[r_ktr[a]])
            P.op("act", lambda e, a=a, h=h: e.activation(out=kz[a][:], in_=ktr[:, a * 128:(a + 1) * 128], func=AF.Copy,
                                                        scale=zeta[:, h:h + 1]),
                 reads=[r_ktr[a], r_c], writes=[r_kz[a]])
            P.op("pe", lambda e, a=a, vh=vh: e.matmul(kvp[:, a * 128:(a + 1) * 128], kz[a][:], vh, start=True, stop=True),
                 reads=[r_kz[a], r_tok[ts]], writes=[r_kv[a]])
            P.op("dve", lambda e, a=a, h=h: e.scalar_tensor_tensor(out=st32[:, h, :], in0=st32[:, h, :], scalar=float(CDEC[h]),
                                                                  in1=kvp[:, a * 128:(a + 1) * 128],
                                                                  op0=ALU.mult, op1=ALU.add),
                 reads=[r_kv[a]], writes=[r_st32[h]])
            P.op("act", lambda e, h=h: e.activation(out=st16[:, h, :], in_=st32[:, h, :], func=AF.Copy),
                 reads=[r_st32[h]], writes=[r_st16[h]])
        cs = n % 2
        for h in range(6):
            ybank = yA if h < 4 else yB
            yreg = ybank[:, (h % 4) * 128:(h % 4 + 1) * 128]
            P.op("dve", lambda e, h=h, yreg=yreg: e.bn_stats(out=bst[:, h, :], in_=yreg), reads=[r_y[h]], wadd=[r_bst])
        for h in range(6):
            P.op("dve", lambda e, h=h: e.bn_aggr(out=mv[:, h, :], in_=bst[:, h, :]), reads=[r_bst], wadd=[r_mv])
        P.op("act", lambda e: e.activation(out=sd[:], in_=mv[:, :, 1], func=AF.Sqrt, bias=g.eps_t[:, 0:1]),
             reads=[r_mv], writes=[r_sd])
        P.op("dve", lambda e: e.reciprocal(out=rs[:], in_=sd[:]), reads=[r_sd], writes=[r_rs])
        P.new_epoch(("dve",), r_catr[cs])
        for h in range(6):
            ybank = yA if h < 4 else yB
            yreg = ybank[:, (h % 4) * 128:(h % 4 + 1) * 128]
            P.op("dve", lambda e, h=h, yreg=yreg, cs=cs: e.tensor_scalar(out=catr[cs][:, h * 128:(h + 1) * 128], in0=yreg,
                                                                        scalar1=mv[:, h, 0:1], scalar2=rs[:, h:h + 1],
                                                                        op0=ALU.subtract, op1=ALU.mult),
                 reads=[r_y[h], r_mv, r_rs], wadd=[r_catr[cs]])
        P.new_epoch(("dve",), r_bst)
        P.new_epoch(("dve",), r_mv)
        P.op("pool", lambda e, cs=cs, ts=ts: e.tensor_tensor(out=catr[cs][:], in0=catr[cs][:], in1=wsg[ts][:], op=ALU.mult),
             reads=[r_wsg[ts]], writes=[r_catr[cs]])
        P.dma("pool", g.cat_d[n * 128:(n + 1) * 128, 0:768], catr[cs][:], reads=[r_catr[cs]], dres=r_catr[cs])
        gv = tokrow[ts][:, 2816:3328]
        P.op("dve", lambda e, gv=gv: e.bn_stats(out=gst[:], in_=gv), reads=[r_tok[ts]], writes=[r_gst])
        P.op("dve", lambda e: e.bn_aggr(out=gmv[:], in_=gst[:]), reads=[r_gst], writes=[r_gmv])
        P.op("act", lambda e: e.activation(out=gsd[:], in_=gmv[:, 1:2], func=AF.Sqrt, bias=g.eps_t[:, 0:1]),
             reads=[r_gmv], writes=[r_gsd])
        P.op("dve", lambda e: e.reciprocal(out=grs[:], in_=gsd[:]), reads=[r_gsd], writes=[r_grs])
        P.op("dve", lambda e, gv=gv: e.tensor_scalar(out=vn[:], in0=gv, scalar1=gmv[:, 0:1], scalar2=grs[:, 0:1],
                                                   op0=ALU.subtract, op1=ALU.mult),
             reads=[r_tok[ts], r_gmv, r_grs], writes=[r_vn])
        P.op("pool", lambda e: e.tensor_tensor(out=vn[:], in0=vn[:], in1=gw[:], op=ALU.mult), reads=[r_c], writes=[r_vn])
        P.op("pool", lambda e, cs=cs: e.tensor_tensor(out=vnb[cs][:], in0=vn[:], in1=gb[:], op=ALU.add),
             reads=[r_vn, r_c], writes=[r_vnb[cs]])
        for gg in range(4):
            P.op("pe", lambda e, gg=gg, cs=cs: e.matmul(fps[:, gg * 128:(gg + 1) * 128], WT[:, gg, :],
                                                       vnb[cs][:, gg * 128:(gg + 1) * 128], start=True, stop=True),
                 reads=[r_WT, r_vnb[cs]], writes=[r_f] if gg == 0 else (), wadd=() if gg == 0 else [r_f])
        P.new_epoch(("dve",), r_catg[cs])
        for gg in range(4):
            P.op("dve", lambda e, gg=gg, cs=cs, ts=ts: e.scalar_tensor_tensor(
                out=catg[cs][:, gg * 128:(gg + 1) * 128], in0=fps[:, gg * 128:(gg + 1) * 128], scalar=bs[:, gg:gg + 1],
                in1=tokrow[ts][:, 2304 + gg * 128:2304 + (gg + 1) * 128], op0=ALU.add, op1=ALU.mult),
                reads=[r_f, r_tok[ts], r_c], wadd=[r_catg[cs]])
        P.dma("pool", g.cat_d[n * 128:(n + 1) * 128, 1536:2048], catg[cs][:], reads=[r_catg[cs]], dres=r_catg[cs])


def phase_diff(P, nc, es, g, l, S):
    import math
    sb = lambda n, s, d: es.enter_context(nc.sbuf_tensor(_nm(n), s, d))
    pst = lambda n, d=F32, w=512: es.enter_context(nc.psum_tensor(_nm(n), [128, w], d))
    NCH = S // 128
    NQ = NCH // 4
    R = lambda n="": Res(n)
    lam_init = 0.8 - 0.6 * math.exp(-0.3 * l)
    maskT = sb("maskT", [128, 128], F32)
    wq = sb("wq", [128, 64], F32)
    wk = sb("wk", [128, 64], F32)
    lamt = sb("lamt", [128, 256], F32)
    tmp = sb("tmpd", [128, 64], F32)
    negM = sb("negM", [128, 1], F32)
    s01 = sb("s01", [128, 2], F32)
    nlam = sb("nlam", [128, 1], F32)
    subw = sb("subw", [128, 128], F32)
    qT = [sb("dqT%d" % i, [128, S], BF16) for i in range(2)]
    kT = [[sb("dkT%d_%d" % (i, j), [128, S], BF16) for j in range(2)] for i in range(2)]
    va = [sb("va%d" % i, [128, NCH, 129], BF16) for i in range(2)]
    pt = [sb("pt%d" % i, [128, 512], BF16) for i in range(3)]
    rr = sb("rr", [128, 2], F32)
    nl = sb("nl", [128, 1], F32)
    tt = [sb("tt%d" % i, [128, 128], F32) for i in range(2)]
    oo = [sb("oo%d" % i, [128, 128], F32) for i in range(2)]
    junk = sb("junkd", [128, 128], F32)
    ssq = sb("ssq", [128, 1], F32)
    sdd = sb("sdd", [128, 1], F32)
    rsd = sb("rsd", [128, 1], F32)
    catd = [sb("catd%d" % i, [128, 128], F32) for i in range(2)]
    accb = [pst("accb%d" % i) for i in range(3)]
    sTb = [pst("sTb%d" % i) for i in range(2)]

    r_c = R()
    r_q = [R(), R()]
    r_k = [R(), R()]
    r_v = [R(), R()]
    r_pt = [R(), R(), R()]
    r_sT = [R(), R()]
    r_acc = [R(), R(), R()]
    r_rr, r_nl, r_ssq, r_sdd, r_rsd, r_junk = R(), R(), R(), R(), R(), R()
    r_tt = [R(), R()]
    r_oo = [R(), R()]
    r_catd = [R(), R()]

    P.dma("sp", maskT[:], g.maskT, wadd=[r_c], dres=r_c)
    P.dma("sp", wq[:], bc(g.diff_q_norm_w[l]), wadd=[r_c], dres=r_c)
    P.dma("sp", wk[:], bc(g.diff_k_norm_w[l]), wadd=[r_c], dres=r_c)
    P.dma("sp", lamt[:], bc(g.diff_lambda[l]), wadd=[r_c], dres=r_c)
    P.dma("sp", subw[:], bc(g.diff_subln_w[l]), wadd=[r_c], dres=r_c)
    r_s = R()
    P.op("dve", lambda e: e.tensor_tensor(out=tmp[:], in0=wq[:], in1=wk[:], op=ALU.mult), reads=[r_c], writes=[r_s])
    P.op("dve", lambda e: e.tensor_reduce(out=negM[:], in_=tmp[:], axis=AX.X, op=ALU.max, apply_absolute_value=True),
         reads=[r_s], writes=[r_s])
    P.op("dve", lambda e: e.tensor_scalar(out=negM[:], in0=negM[:], scalar1=-8.0, scalar2=None, op0=ALU.mult),
         reads=[r_s], writes=[r_s])
    for i in range(2):
        P.op("dve", lambda e, i=i: e.tensor_tensor(out=tmp[:], in0=lamt[:, 128 * i:128 * i + 64], in1=lamt[:, 128 * i + 64:128 * i + 128], op=ALU.mult),
             reads=[r_c], writes=[r_s])
        P.op("dve", lambda e, i=i: e.tensor_reduce(out=s01[:, i:i + 1], in_=tmp[:], axis=AX.X, op=ALU.add),
             reads=[r_s], writes=[r_s])
    P.op("act", lambda e: e.activation(out=s01[:], in_=s01[:], func=AF.Exp), reads=[r_s], writes=[r_s])
    P.op("dve", lambda e: e.tensor_tensor(out=nlam[:], in0=s01[:, 1:2], in1=s01[:, 0:1], op=ALU.subtract),
         reads=[r_s], writes=[r_s])
    P.op("dve", lambda e: e.tensor_scalar(out=nlam[:], in0=nlam[:], scalar1=-lam_init, scalar2=None, op0=ALU.add),
         reads=[r_s], writes=[r_s])
    P.op("dve", lambda e: e.tensor_scalar(out=subw[:], in0=subw[:], scalar1=1.0 - lam_init, scalar2=None, op0=ALU.mult),
         reads=[r_c, r_s], writes=[r_s])
    r_c = r_s

    for hs_ in range(2):
        P.op("pool", lambda e, hs_=hs_: e.memset(va[hs_][:, :, 128:129], 1.0), writes=[r_v[hs_]])
        P.op("pool", lambda e, hs_=hs_: e.memset(kT[hs_][0][64:128, :], 0.0), writes=[r_k[hs_]])
        P.op("pool", lambda e, hs_=hs_: e.memset(kT[hs_][1][0:64, :], 0.0), wadd=[r_k[hs_]])
    ACO = 160
    def accreg(j, qi):
        idx = j * 4 + qi
        return idx // 3, (idx % 3) * ACO

    pk = 0
    sk = 0
    ok = 0
    for h in range(6):
        hs = h % 2
        P.dma("sp", qT[hs][:], g.diffqk_d[h, 0], writes=[r_q[hs]], dres=r_q[hs])
        P.new_epoch(("sp",), r_k[hs])
        P.dma("sp", kT[hs][0][0:64, :], g.diffqk_d[h, 1, 0:64, :], wadd=[r_k[hs]], dres=r_k[hs])
        P.dma("sp", kT[hs][1][64:128, :], g.diffqk_d[h, 1, 64:128, :], wadd=[r_k[hs]], dres=r_k[hs])
        P.new_epoch(("sp",), r_v[hs]) if h >= 2 else None
        P.dma("sp", va[hs][:, :, 0:128], g.tok_d[:, 1536 + h * 128:1536 + (h + 1) * 128].rearrange("(n c) e -> c n e", c=128),
              reads=[r_v[hs]] if h < 2 else (), wadd=[r_v[hs]], dres=r_v[hs])
        for Q in range(NQ):
            nkb = 4 * Q + 4
            for kb in range(nkb):
                c0 = max(0, kb - 4 * Q)
                ncols = (4 - c0) * 128
                for j in range(2):
                    s_ = sk % 2
                    sk += 1
                    p_ = pk % 3
                    pk += 1
                    lo, hi = j * 64, (j + 1) * 64
                    P.op("pe", lambda e, s_=s_, j=j, kb=kb, Q=Q, c0=c0, ncols=ncols, hs=hs: e.matmul(
                        sTb[s_][:, 0:ncols], kT[hs][j][:, kb * 128:(kb + 1) * 128],
                        qT[hs][:, Q * 512 + c0 * 128:Q * 512 + 512], start=True, stop=True),
                        reads=[r_k[hs], r_q[hs]], writes=[r_sT[s_]])
                    P.op("act", lambda e, s_=s_, p_=p_, ncols=ncols: e.activation(
                        out=pt[p_][:, 0:ncols], in_=sTb[s_][:, 0:ncols], func=AF.Exp, scale=0.125, bias=negM[:, 0:1]),
                        reads=[r_sT[s_], r_c], writes=[r_pt[p_]])
                    if kb >= 4 * Q:
                        P.op("pool", lambda e, p_=p_: e.tensor_tensor(out=pt[p_][:, 0:128], in0=pt[p_][:, 0:128],
                                                                      in1=maskT[:], op=ALU.mult),
                             reads=[r_c], writes=[r_pt[p_]])
                    for qi in range(c0, 4):
                        bnk, off = accreg(j, qi)
                        first = (kb == 0 and (j * 4 + qi) % 3 == 0)
                        P.op("pe", lambda e, bnk=bnk, off=off, p_=p_, qi=qi, c0=c0, kb=kb, hs=hs, first=first, Q=Q: e.matmul(
                            accb[bnk][:, off:off + 129], pt[p_][:, (qi - c0) * 128:(qi - c0 + 1) * 128],
                            va[hs][:, kb, :], start=first, stop=(kb == 4 * Q + qi), skip_group_check=True),
                            reads=[r_pt[p_], r_v[hs]],
                            writes=[r_acc[bnk]] if first else (), wadd=() if first else [r_acc[bnk]])
            for qi in range(4):
                o_ = ok % 2
                ok += 1
                b0, f0 = accreg(0, qi)
                b1, f1 = accreg(1, qi)
                P.op("dve", lambda e, b0=b0, f0=f0: e.reciprocal(out=rr[:, 0:1], in_=accb[b0][:, f0 + 128:f0 + 129]),
                     reads=[r_acc[b0]], writes=[r_rr])
                P.op("dve", lambda e, b1=b1, f1=f1: e.reciprocal(out=rr[:, 1:2], in_=accb[b1][:, f1 + 128:f1 + 129]),
                     reads=[r_acc[b1]], wadd=[r_rr])
                P.op("dve", lambda e: e.tensor_tensor(out=nl[:], in0=rr[:, 1:2], in1=nlam[:], op=ALU.mult),
                     reads=[r_rr, r_c], writes=[r_nl])
                P.op("dve", lambda e, b1=b1, f1=f1, o_=o_: e.tensor_scalar(out=tt[o_][:], in0=accb[b1][:, f1:f1 + 128],
                                                                       scalar1=nl[:, 0:1], scalar2=None, op0=ALU.mult),
                     reads=[r_acc[b1], r_nl], writes=[r_tt[o_]])
                P.op("dve", lambda e, b0=b0, f0=f0, o_=o_: e.scalar_tensor_tensor(
                    out=oo[o_][:], in0=accb[b0][:, f0:f0 + 128], scalar=rr[:, 0:1], in1=tt[o_][:],
                    op0=ALU.mult, op1=ALU.add),
                    reads=[r_acc[b0], r_rr, r_tt[o_]], writes=[r_oo[o_]])
                P.op("act", lambda e, o_=o_: e.activation(out=junk[:], in_=oo[o_][:], func=AF.Square, accum_out=ssq[:]),
                     reads=[r_oo[o_]], writes=[r_junk, r_ssq])
                P.op("act", lambda e: e.activation(out=sdd[:], in_=ssq[:], func=AF.Sqrt, scale=1.0 / 128, bias=g.eps_t[:, 0:1]),
                     reads=[r_ssq], writes=[r_sdd])
                P.op("dve", lambda e: e.reciprocal(out=rsd[:], in_=sdd[:]), reads=[r_sdd], writes=[r_rsd])
                P.op("dve", lambda e, o_=o_: e.scalar_tensor_tensor(out=catd[o_][:], in0=oo[o_][:], scalar=rsd[:, 0:1],
                                                                  in1=subw[:], op0=ALU.mult, op1=ALU.mult),
                     reads=[r_oo[o_], r_rsd, r_c], writes=[r_catd[o_]])
                r0 = (4 * Q + qi) * 128
                P.dma("pool", g.cat_d[r0:r0 + 128, 768 + h * 128:768 + (h + 1) * 128], catd[o_][:],
                      reads=[r_catd[o_]], dres=r_catd[o_])


def phase_cat_test(P, nc, es, g, l, S):
    pass


BS = 256


def dyn_dma(e, out, src, regname):
    import re
    ins = e.dma_start(out=out, in_=src)
    m = re.search(r"R\[([A-Za-z]+)_tmp_(\d+)\]", ins.concise())
    if m:
        pre, n = m.group(1), int(m.group(2))
        for cand in range(n - 3, n + 1):
            for nm in ("%s_tmp_%d" % (pre, cand), "%s_%s_snap_%d" % (pre, regname, cand)):
                try:
                    e.free_register(bass.RegisterHandle(nm, mybir.EngineType.SP))
                except Exception:
                    pass
    return ins


def phase_wout(P, nc, es, g, l, S, x_src):
    sb = lambda n, s, d: es.enter_context(nc.sbuf_tensor(_nm(n), s, d))
    pst = lambda n: es.enter_context(nc.psum_tensor(_nm(n), [128, 512], F32))
    R = lambda n="": Res(n)
    NG = S // 512
    ident = sb("identw", [128, 128], F32)
    g1bc = sb("g1bc", [128, D], F32)
    ct = [sb("ct%d" % i, [128, D], F32) for i in range(2)]
    catT = [sb("catT%d" % i, [128, 16, 512], F32R) for i in range(2)]
    wsl = [sb("wo%d" % i, [128, 16, 512], F32R) for i in range(2)]
    xb = [sb("xb%d" % i, [128, 512], F32) for i in range(3)]
    tb = [sb("tb%d" % i, [128, 512], F32) for i in range(3)]
    tp = [pst("tpw%d" % i) for i in range(2)]
    acc = [pst("accw%d" % i) for i in range(2)]
    r_c = R()
    r_ct = [R(), R()]
    r_catT = [[[R() for r in range(4)] for i in range(4)] for s in range(2)]
    r_w = [R(), R()]
    r_xb = [R(), R(), R()]
    r_tb = [R(), R(), R()]
    r_tp = [R(), R()]
    r_acc = [R(), R()]
    P.dma("sp", ident[:], g.ident, wadd=[r_c], dres=r_c)
    P.dma("sp", g1bc[:], bc(g.mod_d[l, 2 * D:3 * D]), wadd=[r_c], dres=r_c)
    wv = g.w_out[l].rearrange("(p j) n -> p j n", j=16).bitcast(F32R)
    ck = tk = wk = ak = xk = 0
    for gi in range(NG):
        gs = gi % 2
        t0 = gi * 512
        for i in range(4):
            cs = ck % 2
            ck += 1
            r0 = t0 + i * 128
            P.dma("sp", ct[cs][:], g.cat_d[r0:r0 + 128, :], writes=[r_ct[cs]], dres=r_ct[cs])
            cv = ct[cs][:].rearrange("p (q j) -> p j q", j=16)
            for rnd in range(4):
                t_ = tk % 2
                tk += 1
                for jj in range(4):
                    j = rnd * 4 + jj
                    P.op("pe", lambda e, t_=t_, jj=jj, j=j, cv=cv: e.transpose(out=tp[t_][:, jj * 128:(jj + 1) * 128],
                                                                            in_=cv[:, j, :], identity=ident[:]),
                         reads=[r_ct[cs], r_c], writes=[r_tp[t_]] if jj == 0 else (), wadd=() if jj == 0 else [r_tp[t_]])
                eng = "dve" if rnd % 2 == 0 else "act"
                outv = catT[gs][:, rnd * 4:(rnd + 1) * 4, i * 128:(i + 1) * 128]
                inv_ = tp[t_][:].rearrange("p (a b) -> p a b", a=4)
                if eng == "dve":
                    fn = lambda e, outv=outv, inv_=inv_: e.tensor_copy(out=outv, in_=inv_)
                else:
                    fn = lambda e, outv=outv, inv_=inv_: e.activation(out=outv, in_=inv_, func=AF.Copy)
                P.op(eng, fn, reads=[r_tp[t_]], writes=[r_catT[gs][i][rnd]])
        for cb in range(4):
            ws = wk % 2
            wk += 1
            P.dma("sp", wsl[ws][:], wv[:, :, cb * 512:(cb + 1) * 512], writes=[r_w[ws]], dres=r_w[ws])
            for i in range(4):
                a = ak % 2
                ak += 1
                x_ = xk % 3
                xk += 1
                r0 = t0 + i * 128
                P.dma("sp", xb[x_][:], x_src[r0:r0 + 128, cb * 512:(cb + 1) * 512], writes=[r_xb[x_]], dres=r_xb[x_])
                for j in range(16):
                    P.op("pe", lambda e, a=a, ws=ws, i=i, j=j, gs=gs: e.matmul(
                        acc[a][:], catT[gs][:, j, i * 128:(i + 1) * 128], wsl[ws][:, j, :], start=(j == 0), stop=(j == 15)),
                        reads=[r_w[ws], r_catT[gs][i][j // 4]],
                        writes=[r_acc[a]] if j == 0 else (), wadd=() if j == 0 else [r_acc[a]])
                P.op("dve", lambda e, a=a, x_=x_, cb=cb: e.tensor_tensor(out=tb[x_][:], in0=acc[a][:],
                                                                        in1=g1bc[:, cb * 512:(cb + 1) * 512], op=ALU.mult),
                     reads=[r_acc[a], r_c], writes=[r_tb[x_]])
                P.op("pool", lambda e, x_=x_: e.tensor_tensor(out=tb[x_][:], in0=tb[x_][:], in1=xb[x_][:], op=ALU.add),
                     reads=[r_xb[x_]], writes=[r_tb[x_]])
                P.dma("pool", g.x1_d[r0:r0 + 128, cb * 512:(cb + 1) * 512], tb[x_][:], reads=[r_tb[x_]], dres=r_tb[x_])


def phase_norm2(P, nc, es, g, l, S, T):
    sb = lambda n, s, d: es.enter_context(nc.sbuf_tensor(_nm(n), s, d))
    pst = lambda n: es.enter_context(nc.psum_tensor(_nm(n), [128, 512], F32))
    R = lambda n="": Res(n)
    NT = S // 128
    ident = sb("identn", [128, 128], F32)
    a2bc = sb("a2bc", [128, D], F32)
    b2bc = sb("b2bc", [128, D], F32)
    tmpbc = sb("tmpbc", [128, D], F32)
    wr = sb("wr", [128, 16, 36], F32)
    rbb = sb("rbb", [128, 36], F32)
    zrow = sb("zrow", [1, D], F32)
    xt = [sb("x1t%d" % i, [128, D], F32) for i in range(2)]
    h2 = [sb("h2t%d" % i, [128, D], F32) for i in range(2)]
    junk = sb("junkn", [128, D], BF16)
    ss = sb("ssn", [128, 1], F32)
    sq = sb("sqn", [128, 1], F32)
    rstd = sb("rstdn", [128, 1], F32)
    h2T = [sb("h2T%d" % i, [128, 16, 128], F32) for i in range(2)]
    lgsA = sb("lgsA", [128, NT, 36], F32)
    io8 = sb("io8", [128, 8], F32)
    gmax = sb("gmax", [128, NT], F32)
    oh4 = sb("oh4", [128, NT, 4], F32)
    ex4 = sb("ex4", [128, NT, 4], F32)
    sume = sb("sume", [128, NT], F32)
    pg = sb("pg", [128, NT], F32)
    gif = sb("gif", [128, NT], F32)
    tmp48 = sb("tmp48", [128, NT, 4, 8], F32)
    els = sb("els", [128, NT, 8], F32)
    els2 = sb("els2", [128, NT, 8], F32)
    top1 = sb("top1", [128, NT], F32)
    top2 = sb("top2", [128, NT], F32)
    mk1 = sb("mk1", [128, NT, 8], F32)
    mk2 = sb("mk2", [128, NT, 8], F32)
    dd = sb("dd", [128, NT], F32)
    w0 = sb("w0", [128, NT], F32)
    tp = [pst("tpn%d" % i) for i in range(2)]
    lgp = pst("lgp")
    r_c, r_x, r_h2, r_tp, r_h2T, r_lg = R(), [R(), R()], [R(), R()], [R(), R()], [R(), R()], R()
    r_s = R()
    r_lgs = R()
    r_junk = R()
    P.dma("sp", ident[:], g.ident, wadd=[r_c], dres=r_c)
    P.dma("sp", a2bc[:], bc(g.mod_d[l, 4 * D:5 * D]), wadd=[r_c], dres=r_c)
    P.dma("sp", b2bc[:], bc(g.mod_d[l, 3 * D:4 * D]), wadd=[r_c], dres=r_c)
    P.dma("sp", tmpbc[:], bc(g.ffn_norm_w[l]), wadd=[r_c], dres=r_c)
    P.dma("sp", wr[:, :, 0:4], g.router_group_w[l].rearrange("(p j) n -> p j n", j=16), wadd=[r_c], dres=r_c)
    P.dma("sp", wr[:, :, 4:36], g.router_expert_w[l].rearrange("(p j) n -> p j n", j=16), wadd=[r_c], dres=r_c)
    P.dma("sp", rbb[:, 0:4], bc(g.router_group_b[l]), wadd=[r_c], dres=r_c)
    P.dma("sp", rbb[:, 4:36], bc(g.router_expert_b[l]), wadd=[r_c], dres=r_c)
    r_c2 = R()
    P.op("dve", lambda e: e.scalar_tensor_tensor(out=a2bc[:], in0=a2bc[:], scalar=1.0, in1=tmpbc[:], op0=ALU.add, op1=ALU.mult),
         reads=[r_c], writes=[r_c2])
    P.op("dve", lambda e: e.memset(zrow[:], 0.0), writes=[r_s])
    P.dma("pool", g.h2_d[S:S + 1, :], zrow[:], reads=[r_s], dres=r_s)
    tk = 0
    for i in range(NT):
        s = i % 2
        r0 = i * 128
        P.dma("sp", xt[s][:], g.x1_d[r0:r0 + 128, :], writes=[r_x[s]], dres=r_x[s])
        P.op("act", lambda e, s=s: e.activation(out=junk[:], in_=xt[s][:], func=AF.Square, accum_out=ss[:]),
             reads=[r_x[s]], writes=[r_junk, r_s])
        P.op("act", lambda e: e.activation(out=sq[:], in_=ss[:], func=AF.Sqrt, scale=1.0 / D, bias=g.eps_t[:, 0:1]),
             reads=[r_s], writes=[r_s])
        P.op("dve", lambda e: e.reciprocal(out=rstd[:], in_=sq[:]), reads=[r_s], writes=[r_s])
        P.op("dve", lambda e, s=s: e.scalar_tensor_tensor(out=h2[s][:], in0=xt[s][:], scalar=rstd[:, 0:1], in1=a2bc[:],
                                                        op0=ALU.mult, op1=ALU.mult),
             reads=[r_x[s], r_s, r_c2], writes=[r_h2[s]])
        P.op("pool", lambda e, s=s: e.tensor_tensor(out=h2[s][:], in0=h2[s][:], in1=b2bc[:], op=ALU.add),
             reads=[r_c], writes=[r_h2[s]])
        P.dma("pool", g.h2_d[r0:r0 + 128, :], h2[s][:], reads=[r_h2[s]], dres=r_h2[s])
        hv = h2[s][:].rearrange("p (q j) -> p j q", j=16)
        for rnd in range(4):
            t_ = tk % 2
            tk += 1
            for jj in range(4):
                j = rnd * 4 + jj
                P.op("pe", lambda e, t_=t_, jj=jj, j=j, hv=hv: e.transpose(out=tp[t_][:, jj * 128:(jj + 1) * 128],
                                                                        in_=hv[:, j, :], identity=ident[:]),
                     reads=[r_h2[s], r_c], writes=[r_tp[t_]] if jj == 0 else (), wadd=() if jj == 0 else [r_tp[t_]])
            outv = h2T[s][:, rnd * 4:(rnd + 1) * 4, :]
            inv_ = tp[t_][:].rearrange("p (a b) -> p a b", a=4)
            if rnd == 0:
                P.new_epoch(("dve", "act"), r_h2T[s])
            if rnd % 2 == 0:
                P.op("dve", lambda e, outv=outv, inv_=inv_: e.tensor_copy(out=outv, in_=inv_), reads=[r_tp[t_]], wadd=[r_h2T[s]])
            else:
                P.op("act", lambda e, outv=outv, inv_=inv_: e.activation(out=outv, in_=inv_, func=AF.Copy),
                     reads=[r_tp[t_]], wadd=[r_h2T[s]])
        for j in range(16):
            P.op("pe", lambda e, s=s, j=j: e.matmul(lgp[:, 0:36], h2T[s][:, j, :], wr[:, j, :], start=(j == 0), stop=(j == 15)),
                 reads=[r_h2T[s], r_c], writes=[r_lg] if j == 0 else (), wadd=() if j == 0 else [r_lg])
        P.op("dve", lambda e, i=i: e.tensor_tensor(out=lgsA[:, i, :], in0=lgp[:, 0:36], in1=rbb[:], op=ALU.add),
             reads=[r_lg, r_c], wadd=[r_lgs])
    X = lambda eng, fn: P.op(eng, fn, reads=[r_s, r_lgs], writes=[r_s])
    glv = lgsA[:, :, 0:4]
    elv = lgsA[:, :, 4:36].rearrange("p n (g e) -> p n g e", g=4)
    bcn = lambda t, k: t[:, :].unsqueeze(2).to_broadcast([128, NT, k])
    X("pool", lambda e: e.iota(io8[:], [[1, 8]], base=0, channel_multiplier=0, allow_small_or_imprecise_dtypes=True))
    io4b = bass.AP(io8, 0, [[8, 128], [0, NT], [1, 4]])
    io8b = bass.AP(io8, 0, [[8, 128], [0, NT], [1, 8]])
    X("dve", lambda e: e.tensor_reduce(out=gmax[:], in_=glv, axis=AX.X, op=ALU.max))
    X("dve", lambda e: e.tensor_tensor(out=oh4[:], in0=glv, in1=bcn(gmax, 4), op=ALU.is_equal))
    X("dve", lambda e: e.tensor_tensor(out=ex4[:], in0=glv, in1=bcn(gmax, 4), op=ALU.subtract))
    X("act", lambda e: e.activation(out=ex4[:], in_=ex4[:], func=AF.Exp))
    X("dve", lambda e: e.tensor_reduce(out=sume[:], in_=ex4[:], axis=AX.X, op=ALU.add))
    X("dve", lambda e: e.reciprocal(out=pg[:], in_=sume[:]))
    X("dve", lambda e: e.tensor_tensor(out=ex4[:], in0=oh4[:], in1=io4b, op=ALU.mult))
    X("dve", lambda e: e.tensor_reduce(out=gif[:], in_=ex4[:], axis=AX.X, op=ALU.add))
    X("dve", lambda e: e.tensor_tensor(out=tmp48[:], in0=elv, in1=oh4[:, :, :].unsqueeze(3).to_broadcast([128, NT, 4, 8]),
                                       op=ALU.mult))
    X("dve", lambda e: e.tensor_reduce(out=els[:], in_=tmp48[:].rearrange("p n g e -> p n e g"), axis=AX.X, op=ALU.add))
    X("dve", lambda e: e.tensor_reduce(out=top1[:], in_=els[:], axis=AX.X, op=ALU.max))
    X("dve", lambda e: e.tensor_tensor(out=mk1[:], in0=els[:], in1=bcn(top1, 8), op=ALU.is_equal))
    X("dve", lambda e: e.scalar_tensor_tensor(out=els2[:], in0=mk1[:], scalar=-1e30, in1=els[:], op0=ALU.mult, op1=ALU.add))
    X("dve", lambda e: e.tensor_reduce(out=top2[:], in_=els2[:], axis=AX.X, op=ALU.max))
    X("dve", lambda e: e.tensor_tensor(out=mk2[:], in0=els2[:], in1=bcn(top2, 8), op=ALU.is_equal))
    X("dve", lambda e: e.tensor_tensor(out=mk1[:], in0=mk1[:], in1=io8b, op=ALU.mult))
    X("dve", lambda e: e.tensor_tensor(out=mk2[:], in0=mk2[:], in1=io8b, op=ALU.mult))
    X("dve", lambda e: e.tensor_reduce(out=T.eidA[:, :, 0], in_=mk1[:], axis=AX.X, op=ALU.add))
    X("dve", lambda e: e.tensor_reduce(out=T.eidA[:, :, 1], in_=mk2[:], axis=AX.X, op=ALU.add))
    X("dve", lambda e: e.tensor_scalar(out=gif[:], in0=gif[:], scalar1=8.0, scalar2=None, op0=ALU.mult))
    X("dve", lambda e: e.tensor_tensor(out=T.eidA[:], in0=T.eidA[:], in1=bcn(gif, 2), op=ALU.add))
    X("dve", lambda e: e.tensor_tensor(out=dd[:], in0=top2[:], in1=top1[:], op=ALU.subtract))
    X("act", lambda e: e.activation(out=dd[:], in_=dd[:], func=AF.Exp))
    X("dve", lambda e: e.tensor_scalar(out=w0[:], in0=dd[:], scalar1=1.0, scalar2=None, op0=ALU.add))
    X("dve", lambda e: e.reciprocal(out=w0[:], in_=w0[:]))
    X("dve", lambda e: e.tensor_tensor(out=dd[:], in0=dd[:], in1=w0[:], op=ALU.mult))
    X("dve", lambda e: e.tensor_tensor(out=T.gateA[:, :, 0], in0=w0[:], in1=pg[:], op=ALU.mult))
    X("dve", lambda e: e.tensor_tensor(out=T.gateA[:, :, 1], in0=dd[:], in1=pg[:], op=ALU.mult))

def phase_route(P, nc, es, g, l, S, T):
    sb = lambda n, s, d: es.enter_context(nc.sbuf_tensor(_nm(n), s, d))
    pst = lambda n: es.enter_context(nc.psum_tensor(_nm(n), [128, 512], F32))
    R = lambda n="": Res(n)
    NT = S // 128
    NBLK = (2 * S) // BS + NEXP
    NSLOT = NBLK * BS
    JM = S // BS
    iota32 = sb("iota32", [128, 32], F32)
    thr = sb("thr", [128, JM], F32)
    biota = sb("biota", [128, NBLK], F32)
    oh = sb("oh", [128, NT, 2, 32], F32)
    M = sb("M", [128, NT, 32], F32)
    Mb = sb("Mb", [128, NT, 32], BF16)
    U = sb("U", [128, 128], BF16)
    ones = sb("onesr", [128, 128], BF16)
    onesf = sb("onesf", [128, 32], F32)
    rank = sb("rank", [128, NT, 32], F32)
    Rtot = sb("Rtot", [128, 32], F32)
    cmp_ = sb("cmp", [128, 32, JM], F32)
    nbk = sb("nbk", [128, 32], F32)
    pendb = sb("pendb", [128, 32], F32)
    pstart = sb("pstart", [128, 32], F32)
    tmp4 = sb("tmp4", [128, NT, 2, 32], F32)
    destf = sb("destf", [128, NT, 2], F32)
    cmpb = sb("cmpb", [128, NBLK, 32], F32)
    bexf = sb("bexf", [128, NBLK], F32)
    tokid = sb("tokid", [128, NT], I32)
    initt = sb("initt", [128, NSLOT // 128], I32)
    wps = [pst("wps%d" % i) for i in range(2)]
    r_s = R()
    r_M = R()
    r_w = [R(), R()]
    r_rank = R()
    X = lambda eng, fn, extra=(), wr=None: P.op(eng, fn, reads=[r_s] + list(extra), writes=[r_s] if wr is None else wr)
    X("pool", lambda e: e.iota(iota32[:], [[1, 32]], base=0, channel_multiplier=0, allow_small_or_imprecise_dtypes=True))
    X("pool", lambda e: e.iota(thr[:], [[BS, JM]], base=0, channel_multiplier=0, allow_small_or_imprecise_dtypes=True))
    X("pool", lambda e: e.iota(biota[:], [[1, NBLK]], base=0, channel_multiplier=0, allow_small_or_imprecise_dtypes=True))
    X("pool", lambda e: e.iota(tokid[:], [[128, NT]], base=0, channel_multiplier=1))
    X("pool", lambda e: e.memset(initt[:], S))
    X("pool", lambda e: e.memset(ones[:], 1.0))
    X("pool", lambda e: e.memset(onesf[:], 1.0))
    X("pool", lambda e: e.memset(Rtot[:], 0.0))
    X("pool", lambda e: e.affine_select(out=U[:], in_=ones[:], pattern=[[1, 128]], compare_op=ALU.is_ge, fill=0.0,
                                        base=-1, channel_multiplier=-1))
    P.dma("sp", g.slot_tok_d.rearrange("(p j) o -> p (j o)", p=128), initt[:], reads=[r_s], dres=r_s)
    iob = bass.AP(iota32, 0, [[32, 128], [0, NT], [0, 2], [1, 32]])
    X("dve", lambda e: e.tensor_tensor(out=oh[:], in0=T.eidA[:, :, :].unsqueeze(3).to_broadcast([128, NT, 2, 32]),
                                       in1=iob, op=ALU.is_equal))
    X("dve", lambda e: e.tensor_tensor(out=M[:], in0=oh[:, :, 0, :], in1=oh[:, :, 1, :], op=ALU.add))
    X("dve", lambda e: e.tensor_copy(out=Mb[:], in_=M[:]), wr=[r_s, r_M])
    for i in range(NT):
        w_ = i % 2
        P.op("pe", lambda e, w_=w_, i=i: e.matmul(wps[w_][:, 0:32], U[:], Mb[:, i, :], start=True, stop=True),
             reads=[r_M, r_s], writes=[r_w[w_]])
        P.op("pe", lambda e, w_=w_, i=i: e.matmul(wps[w_][:, 32:64], ones[:], Mb[:, i, :], start=True, stop=True),
             reads=[r_M, r_s], wadd=[r_w[w_]])
        X("dve", lambda e, w_=w_, i=i: e.tensor_tensor(out=rank[:, i, :], in0=wps[w_][:, 0:32], in1=Rtot[:], op=ALU.add),
          extra=[r_w[w_]])
        X("dve", lambda e, w_=w_: e.tensor_tensor(out=Rtot[:], in0=wps[w_][:, 32:64], in1=Rtot[:], op=ALU.add),
          extra=[r_w[w_]])
    X("dve", lambda e: e.tensor_tensor(out=cmp_[:], in0=Rtot[:, :].unsqueeze(2).to_broadcast([128, 32, JM]),
                                       in1=bass.AP(thr, 0, [[JM, 128], [0, 32], [1, JM]]), op=ALU.is_gt))
    X("dve", lambda e: e.tensor_reduce(out=nbk[:], in_=cmp_[:], axis=AX.X, op=ALU.add))
    X("dve", lambda e: e.tensor_tensor_scan(out=pendb[:], data0=onesf[:], data1=nbk[:], initial=0.0, op0=ALU.mult, op1=ALU.add))
    X("dve", lambda e: e.tensor_tensor(out=pstart[:], in0=pendb[:], in1=nbk[:], op=ALU.subtract))
    X("dve", lambda e: e.tensor_scalar(out=pstart[:], in0=pstart[:], scalar1=float(BS), scalar2=None, op0=ALU.mult))
    X("dve", lambda e: e.tensor_tensor(out=rank[:], in0=rank[:], in1=bass.AP(pstart, 0, [[32, 128], [0, NT], [1, 32]]),
                                       op=ALU.add))
    X("dve", lambda e: e.tensor_tensor(out=tmp4[:], in0=oh[:],
                                       in1=bass.AP(rank, 0, [[NT * 32, 128], [32, NT], [0, 2], [1, 32]]), op=ALU.mult))
    X("dve", lambda e: e.tensor_reduce(out=destf[:], in_=tmp4[:], axis=AX.X, op=ALU.add))
    X("dve", lambda e: e.tensor_copy(out=T.destA[:], in_=destf[:]))
    X("dve", lambda e: e.tensor_tensor(out=cmpb[:], in0=bass.AP(pendb, 0, [[32, 128], [0, NBLK], [1, 32]]),
                                       in1=biota[:, :].unsqueeze(2).to_broadcast([128, NBLK, 32]), op=ALU.is_le))
    X("dve", lambda e: e.tensor_reduce(out=bexf[:], in_=cmpb[:], axis=AX.X, op=ALU.add))
    X("dve", lambda e: e.tensor_scalar(out=bexf[:], in0=bexf[:], scalar1=31.0, scalar2=None, op0=ALU.min))
    X("dve", lambda e: e.tensor_copy(out=T.blkexp[:], in_=bexf[:]))
    X("dve", lambda e: e.tensor_tensor(out=bexf[:], in0=biota[:], in1=pendb[:, 31:32].to_broadcast([128, NBLK]), op=ALU.is_lt))
    X("dve", lambda e: e.tensor_copy(out=T.blkused[:], in_=bexf[:]))
    r_sc = R()
    for i in range(NT):
        for k in range(2):
            P.raw("pool", lambda e, i=i, k=k: e.indirect_dma_start(
                out=g.slot_tok_d, out_offset=bass.IndirectOffsetOnAxis(ap=T.destA[:, i, k:k + 1], axis=0),
                in_=tokid[:, i:i + 1], in_offset=None),
                dres=r_sc, reads=[r_s])


def phase_experts(P, nc, es, g, l, S, T):
    sb = lambda n, s, d: es.enter_context(nc.sbuf_tensor(_nm(n), s, d))
    pst = lambda n: es.enter_context(nc.psum_tensor(_nm(n), [128, 512], F32))
    R = lambda n="": Res(n)
    NBLK = (2 * S) // BS + NEXP
    ident = sb("idente", [128, 128], F32)
    idx = [sb("idx%d" % i, [128, 2], I32) for i in range(2)]
    xg = [sb("xg%d" % i, [128, D], F32) for i in range(2)]
    xbT = [sb("xbT%d" % i, [128, 16, BS], F32R) for i in range(2)]
    wgu = [sb("wgu%d" % i, [128, 16, 2, 256], F32R) for i in range(2)]
    wdn = [sb("wdn%d" % i, [128, 8, 512], F32R) for i in range(2)]
    sa = [sb("sa%d" % i, [128, BS], F32) for i in range(2)]
    hid = [sb("hid%d" % i, [128, EH], F32) for i in range(2)]
    hT = [sb("hTe%d" % i, [128, 8, BS], F32R) for i in range(2)]
    yb = [sb("yb%d" % i, [128, D], F32) for i in range(2)]
    tp = [pst("tpe%d" % i) for i in range(2)]
    gu = [pst("gu%d" % i) for i in range(2)]
    dn = [pst("dn%d" % i) for i in range(2)]
    r_c = R()
    r_idx = [R(), R()]
    r_xg = [R(), R()]
    r_xbT = [[[R() for r in range(4)] for s in range(2)] for b in range(2)]
    r_wgu = [R(), R()]
    r_wdn = [R(), R()]
    r_sa = [R(), R()]
    r_hid = [[R() for q in range(4)] for s in range(2)]
    r_hT2 = [[[R() for r in range(2)] for s in range(2)] for b in range(2)]
    r_yb = [R(), R()]
    r_tp = [R(), R()]
    r_gu = [R(), R()]
    r_dn = [R(), R()]
    P.dma("sp", ident[:], g.ident, writes=[r_c], dres=r_c)
    GU_E = D * D
    DN_E = EH * D
    if not hasattr(g, "regs"):
        g.regs = {}
    regs = g.regs

    def getreg(e, name):
        if name not in regs:
            regs[name] = e.alloc_register(name)
        return regs[name]
    gk = wk = dk = tk = uk = nk = 0
    NMIN = (2 * S) // BS
    for b in range(NBLK):
        bs_ = b % 2
        if b >= NMIN:
            P.begin_cond(T.blkused[0:1, b:b + 1])
        P.dma("sp", idx[bs_][:], g.slot_tok_d[b * BS:(b + 1) * BS, :].rearrange("(s p) o -> p (s o)", p=128),
              writes=[r_idx[bs_]], dres=r_idx[bs_], allow_slow_non_contiguous=True)
        for s in range(2):
            x_ = gk % 2
            gk += 1
            P.raw("pool", lambda e, x_=x_, bs_=bs_, s=s: e.indirect_dma_start(
                out=xg[x_][:], out_offset=None, in_=g.h2_d,
                in_offset=bass.IndirectOffsetOnAxis(ap=idx[bs_][:, s:s + 1], axis=0)),
                dres=r_xg[x_], reads=[r_idx[bs_]], writes=[r_xg[x_]])
            xv = xg[x_][:].rearrange("p (q j) -> p j q", j=16)
            for rnd in range(4):
                t_ = tk % 2
                tk += 1
                for jj in range(4):
                    j = rnd * 4 + jj
                    P.op("pe", lambda e, t_=t_, jj=jj, j=j, xv=xv: e.transpose(out=tp[t_][:, jj * 128:(jj + 1) * 128],
                                                                            in_=xv[:, j, :], identity=ident[:]),
                         reads=[r_xg[x_], r_c], writes=[r_tp[t_]] if jj == 0 else (), wadd=() if jj == 0 else [r_tp[t_]])
                outv = xbT[bs_][:, rnd * 4:(rnd + 1) * 4, s * 128:(s + 1) * 128]
                inv_ = tp[t_][:].rearrange("p (a b) -> p a b", a=4)
                if rnd % 2 == 0:
                    P.op("dve", lambda e, outv=outv, inv_=inv_: e.tensor_copy(out=outv, in_=inv_),
                         reads=[r_tp[t_]], writes=[r_xbT[bs_][s][rnd]])
                else:
                    P.op("act", lambda e, outv=outv, inv_=inv_: e.activation(out=outv, in_=inv_, func=AF.Copy),
                         reads=[r_tp[t_]], writes=[r_xbT[bs_][s][rnd]])
        for q in range(4):
            w_ = wk % 2
            wk += 1

            def ld_gu(e, b=b, q=q, w_=w_):
                rb = getreg(e, "r_eb")
                ro = getreg(e, "r_off")
                e.reg_load(rb, T.blkexp[0:1, b:b + 1])
                e.reg_mul(ro, rb, GU_E)
                e.reg_add(ro, ro, l * NEXP * GU_E + q * 256)
                src = bass.AP(g.w_gu.tensor, ro, [[16 * D, 128], [D, 16], [1024, 2], [1, 256]]).bitcast(F32R)
                return dyn_dma(e, wgu[w_][:], src, ro.name)
            P.raw("sp", ld_gu, dres=r_wgu[w_], writes=[r_wgu[w_]])
            for s in range(2):
                u_ = uk % 2
                uk += 1
                for j in range(16):
                    P.op("pe", lambda e, u_=u_, w_=w_, s=s, j=j, bs_=bs_: e.matmul(
                        gu[u_][:], xbT[bs_][:, j, s * 128:(s + 1) * 128],
                        wgu[w_][:, j, :, :].rearrange("p a c -> p (a c)"), start=(j == 0), stop=(j == 15)),
                        reads=[r_wgu[w_], r_xbT[bs_][s][j // 4]],
                        writes=[r_gu[u_]] if j == 0 else (), wadd=() if j == 0 else [r_gu[u_]])
                a_ = nk % 2
                nk += 1
                P.op("act", lambda e, a_=a_, u_=u_: e.activation(out=sa[a_][:], in_=gu[u_][:, 0:256], func=AF.Silu),
                     reads=[r_gu[u_]], writes=[r_sa[a_]])
                P.op("dve", lambda e, a_=a_, u_=u_, s=s, q=q: e.tensor_tensor(
                    out=hid[s][:, q * 256:(q + 1) * 256], in0=sa[a_][:], in1=gu[u_][:, 256:512], op=ALU.mult),
                    reads=[r_sa[a_], r_gu[u_]], writes=[r_hid[s][q]])
        for s in range(2):
            for rnd in range(2):
                t_ = tk % 2
                tk += 1
                for jj in range(4):
                    c_ = rnd * 4 + jj
                    P.op("pe", lambda e, t_=t_, jj=jj, c_=c_, s=s: e.transpose(
                        out=tp[t_][:, jj * 128:(jj + 1) * 128], in_=hid[s][:, c_ * 128:(c_ + 1) * 128], identity=ident[:]),
                        reads=[r_hid[s][c_ // 2], r_c], writes=[r_tp[t_]] if jj == 0 else (), wadd=() if jj == 0 else [r_tp[t_]])
                outv = hT[bs_][:, rnd * 4:(rnd + 1) * 4, s * 128:(s + 1) * 128]
                inv_ = tp[t_][:].rearrange("p (a b) -> p a b", a=4)
                if rnd % 2 == 0:
                    P.op("dve", lambda e, outv=outv, inv_=inv_: e.tensor_copy(out=outv, in_=inv_),
                         reads=[r_tp[t_]], writes=[r_hT2[bs_][s][rnd]])
                else:
                    P.op("act", lambda e, outv=outv, inv_=inv_: e.activation(out=outv, in_=inv_, func=AF.Copy),
                         reads=[r_tp[t_]], writes=[r_hT2[bs_][s][rnd]])
        for q in range(4):
            d_ = dk % 2
            dk += 1

            def ld_dn(e, b=b, q=q, d_=d_):
                rb = getreg(e, "r_eb")
                ro = getreg(e, "r_off")
                e.reg_load(rb, T.blkexp[0:1, b:b + 1])
                e.reg_mul(ro, rb, DN_E)
                e.reg_add(ro, ro, l * NEXP * DN_E + q * 512)
                src = bass.AP(g.w_dn.tensor, ro, [[D, 128], [128 * D, 8], [1, 512]]).bitcast(F32R)
                return dyn_dma(e, wdn[d_][:], src, ro.name)
            P.raw("sp", ld_dn, dres=r_wdn[d_], writes=[r_wdn[d_]])
            for s in range(2):
                o_ = (dk + s) % 2
                if q == 0:
                    P.new_epoch(("act", "dve"), r_yb[s])
                for c_ in range(8):
                    P.op("pe", lambda e, o_=o_, c_=c_, s=s, d_=d_, bs_=bs_: e.matmul(
                        dn[o_][:], hT[bs_][:, c_, s * 128:(s + 1) * 128], wdn[d_][:, c_, :], start=(c_ == 0), stop=(c_ == 7)),
                        reads=[r_wdn[d_], r_hT2[bs_][s][c_ // 4]],
                        writes=[r_dn[o_]] if c_ == 0 else (), wadd=() if c_ == 0 else [r_dn[o_]])
                if s == 0:
                    P.op("act", lambda e, o_=o_, s=s, q=q: e.activation(out=yb[s][:, q * 512:(q + 1) * 512], in_=dn[o_][:], func=AF.Copy),
                         reads=[r_dn[o_]], wadd=[r_yb[s]])
                else:
                    P.op("dve", lambda e, o_=o_, s=s, q=q: e.tensor_copy(out=yb[s][:, q * 512:(q + 1) * 512], in_=dn[o_][:]),
                         reads=[r_dn[o_]], wadd=[r_yb[s]])
        for s in range(2):
            r0 = b * BS + s * 128
            P.dma("pool", g.yslot_d[r0:r0 + 128, :], yb[s][:], reads=[r_yb[s]], dres=r_yb[s])
        if b >= NMIN:
            P.end_cond()


def phase_combine(P, nc, es, g, l, S, T, x_dst):
    sb = lambda n, s, d: es.enter_context(nc.sbuf_tensor(_nm(n), s, d))
    R = lambda n="": Res(n)
    NT = S // 128
    NB = 3
    g2bc = sb("g2bc", [128, D], F32)
    y0 = [sb("y0_%d" % i, [128, D], F32) for i in range(NB)]
    y1 = [sb("y1_%d" % i, [128, D], F32) for i in range(NB)]
    xt = [sb("xc%d" % i, [128, D], F32) for i in range(NB)]
    ac = [sb("ac%d" % i, [128, D], F32) for i in range(2)]
    r_c = R()
    r_y0, r_y1, r_x = [R() for _ in range(NB)], [R() for _ in range(NB)], [R() for _ in range(NB)]
    r_ac = [R(), R()]
    P.dma("sp", g2bc[:], bc(g.mod_d[l, 5 * D:6 * D]), writes=[r_c], dres=r_c)

    def loads(i):
        s = i % NB
        r0 = i * 128
        P.raw("pool", lambda e, s=s, i=i: e.indirect_dma_start(
            out=y0[s][:], out_offset=None, in_=g.yslot_d,
            in_offset=bass.IndirectOffsetOnAxis(ap=T.destA[:, i, 0:1], axis=0)),
            dres=r_y0[s], writes=[r_y0[s]])
        P.raw("pool", lambda e, s=s, i=i: e.indirect_dma_start(
            out=y1[s][:], out_offset=None, in_=g.yslot_d,
            in_offset=bass.IndirectOffsetOnAxis(ap=T.destA[:, i, 1:2], axis=0)),
            dres=r_y1[s], writes=[r_y1[s]])
        P.dma("sp", xt[s][:], g.x1_d[r0:r0 + 128, :], writes=[r_x[s]], dres=r_x[s])
    loads(0)
    if NT > 1:
        loads(1)
    for i in range(NT):
        if i + 2 < NT:
            loads(i + 2)
        s = i % NB
        a = i % 2
        r0 = i * 128
        P.op("act", lambda e, s=s, i=i: e.activation(out=y0[s][:], in_=y0[s][:], func=AF.Copy, scale=T.gateA[:, i, 0:1]),
             reads=[r_y0[s]], writes=[r_y0[s]])
        P.op("dve", lambda e, s=s, a=a, i=i: e.scalar_tensor_tensor(out=ac[a][:], in0=y1[s][:], scalar=T.gateA[:, i, 1:2], in1=y0[s][:],
                                                                   op0=ALU.mult, op1=ALU.add),
             reads=[r_y1[s], r_y0[s]], writes=[r_ac[a]])
        P.op("dve", lambda e, a=a: e.tensor_tensor(out=ac[a][:], in0=ac[a][:], in1=g2bc[:], op=ALU.mult),
             reads=[r_c], writes=[r_ac[a]])
        P.op("dve", lambda e, s=s, a=a: e.tensor_tensor(out=ac[a][:], in0=ac[a][:], in1=xt[s][:], op=ALU.add),
             reads=[r_x[s]], writes=[r_ac[a]])
        P.dma("sp", x_dst[r0:r0 + 128, :], ac[a][:], reads=[r_ac[a]], dres=r_ac[a])


_CONST_KEYS = ["ident", "PT", "blk64", "rot", "xitab", "decayT", "zeta", "maskT", "tril", "identb"]
_S = 4096
_NCORES = 8


def kernel(**inputs):
    S = _S
    nc = build(S, debug=False, layers=(0, 1))
    cst = host_consts(S)
    x = np.ascontiguousarray(np.asarray(inputs["x"], dtype=np.float32))
    c = np.ascontiguousarray(np.asarray(inputs["c"], dtype=np.float32))
    shared = {}
    for k, v in inputs.items():
        if k in ("x", "c"):
            continue
        shared[k] = np.ascontiguousarray(np.asarray(v, dtype=np.float32))
    for k in _CONST_KEYS:
        shared[k] = cst[k]
    in_maps = []
    for b in range(_NCORES):
        m = dict(shared)
        m["x"] = x[b]
        m["c"] = c[b]
        in_maps.append(m)
    res = run_bass_kernel_spmd(nc, in_maps, core_ids=list(range(_NCORES)))
    out = np.stack([np.asarray(r["out"]) for r in res.results], axis=0)
    return out.astype(np.float32)
```

```python
import numpy as np
from contextlib import ExitStack
import concourse.bass as bass
import concourse.mybir as mybir
from concourse.bass_utils import run_bass_kernel_spmd

F32 = mybir.dt.float32
F32R = mybir.dt.float32r
BF16 = mybir.dt.bfloat16
I32 = mybir.dt.int32
U32 = mybir.dt.uint32
AF = mybir.ActivationFunctionType
ALU = mybir.AluOpType
AX = mybir.AxisListType


class Sem:
    _n = 0

    def __init__(self, nc, name):
        self.h = nc.alloc_semaphore(name=name)
        self.cnt = 0
        Sem._n += 1
        self.key = Sem._n


class Res:
    __slots__ = ("name", "w", "r", "dsem")

    def __init__(self, name=""):
        self.name = name
        self.w = {}
        self.r = {}
        self.dsem = None


def _merge(d, ev):
    s, v = ev
    o = d.get(s.key)
    if o is None or o[1] < v:
        d[s.key] = ev


class Prog:
    ENGS = ("sp", "pe", "act", "dve", "pool")
    ATTR = {"sp": "sync", "pe": "tensor", "act": "scalar", "dve": "vector", "pool": "gpsimd"}

    def __init__(self, nc):
        self.nc = nc
        self.q = {e: [] for e in self.ENGS}
        self.esem = {e: Sem(nc, "es_" + e) for e in ("pe", "act", "dve", "pool")}
        self.waited = {}
        self.dfree = []
        self.dall = []
        self.phase_dres = []
        self.ninst = 0

    def _wait(self, E, evs, skip_own=False):
        own = self.esem.get(E)
        for k, (s, v) in evs.items():
            if skip_own and s is own:
                continue
            kk = (E, k)
            if self.waited.get(kk, 0) >= v:
                continue
            self.waited[kk] = v
            self.q[E].append(("w", s, v))

    @staticmethod
    def _deps(reads, writes, wadd=()):
        evs = {}
        for r in reads:
            for ev in r.w.values():
                _merge(evs, ev)
        for w in writes:
            for ev in w.w.values():
                _merge(evs, ev)
            for ev in w.r.values():
                _merge(evs, ev)
        for w in wadd:
            for ev in w.r.values():
                _merge(evs, ev)
        return evs

    def op(self, E, fn, reads=(), writes=(), wadd=(), skip_own=None):
        if skip_own is None:
            skip_own = (E == "pe")
        self._wait(E, self._deps(reads, writes, wadd), skip_own)
        s = self.esem[E]
        s.cnt += 1
        self.q[E].append(("i", fn, s, 1))
        ev = (s, s.cnt)
        for r in reads:
            _merge(r.r, ev)
        for w in writes:
            w.w = {s.key: ev}
            w.r = {}
        for w in wadd:
            _merge(w.w, ev)
        self.ninst += 1
        return ev

    def new_epoch(self, engines, res):
        evs = self._deps((), (res,))
        for E in engines:
            self._wait(E, evs, skip_own=False)
        res.w = {}
        res.r = {}

    def _get_dsem(self):
        if self.dfree:
            return self.dfree.pop()
        s = Sem(self.nc, "ds%d" % len(self.dall))
        self.dall.append(s)
        return s

    def dma(self, Q, out, in_, reads=(), writes=(), wadd=(), dres=None, **kw):
        assert dres is not None
        self._wait(Q, self._deps(reads, writes, wadd))
        if dres.dsem is None:
            dres.dsem = self._get_dsem()
            self.phase_dres.append(dres)
        s = dres.dsem
        s.cnt += 16
        nc = self.nc

        def fn(eng, out=out, in_=in_, kw=kw):
            return eng.dma_start(out=out, in_=in_, **kw)
        self.q[Q].append(("i", fn, s, 16))
        ev = (s, s.cnt)
        for r in reads:
            _merge(r.r, ev)
        for w in writes:
            w.w = {s.key: ev}
            w.r = {}
        for w in wadd:
            _merge(w.w, ev)
        self.ninst += 1
        return ev

    def raw(self, Q, fn, dres, reads=(), writes=(), wadd=(), inc=16):
        self._wait(Q, self._deps(reads, writes, wadd))
        if dres.dsem is None:
            dres.dsem = self._get_dsem()
            self.phase_dres.append(dres)
        s = dres.dsem
        s.cnt += inc
        self.q[Q].append(("i", fn, s, inc))
        ev = (s, s.cnt)
        for r in reads:
            _merge(r.r, ev)
        for w in writes:
            w.w = {s.key: ev}
            w.r = {}
        for w in wadd:
            _merge(w.w, ev)
        return ev

    def end_phase(self, drain_on="sp"):
        for r in self.phase_dres:
            s = r.dsem
            kk = (drain_on, s.key)
            if self.waited.get(kk, 0) < s.cnt:
                self.waited[kk] = s.cnt
                self.q[drain_on].append(("w", s, s.cnt))
        self.flush()
        for r in self.phase_dres:
            self.dfree.append(r.dsem)
            r.dsem = None
            r.w = {}
            r.r = {}
        self.phase_dres = []
        allsems = list(self.esem.values()) + self.dall
        for E in self.ENGS:
            for s in allsems:
                self.waited[(E, s.key)] = s.cnt

    def flush(self):
        with self.nc.Block() as block:
            for E in self.ENGS:
                items = self.q[E]
                if not items:
                    continue

                def body(eng, items=items):
                    for it in items:
                        if it[0] == "w":
                            eng.wait_ge(it[1].h, it[2])
                        else:
                            inst = it[1](eng)
                            inst.then_inc(it[2].h, it[3])
                getattr(block, self.ATTR[E])(body)
                self.q[E] = []


D = 2048
DIN = 6400
NMOD = 6 * D
EPS = 1e-6
NEXP = 32
EH = 1024


def host_consts(S):
    c = {}
    c["ident"] = np.eye(128, dtype=np.float32)
    PT = np.zeros((128, 128), np.float32)
    for m in range(64):
        PT[m + 64, m] = -1.0
        PT[m, m + 64] = 1.0
    c["PT"] = PT
    blk = np.zeros((128, 128), np.float32)
    blk[:64, :64] = 1.0 / 64
    blk[64:, 64:] = 1.0 / 64
    c["blk64"] = blk
    half = 64
    inv = 10000.0 ** (-np.arange(half, dtype=np.float32) / half)
    pos = np.arange(S, dtype=np.float32)
    ang = pos[None, :] * inv[:, None]
    cos = np.cos(ang).astype(np.float32)
    sin = np.sin(ang).astype(np.float32)
    cosT = np.concatenate([cos, cos], 0)
    sinT = np.concatenate([sin, sin], 0)
    ks = np.float32(128 ** -0.5)
    c["rot"] = np.stack([cosT, sinT, cosT * ks, sinT * ks], 0).astype(np.float32)
    gam = 1.0 - 2.0 ** (-5.0 - np.arange(6, dtype=np.float64))
    idx = np.arange(128, dtype=np.float64)
    rel = idx[None, :] - idx[:, None]
    dec = np.where(rel[None] >= 0, gam[:, None, None] ** np.maximum(rel, 0)[None], 0.0)
    c["decayT"] = np.ascontiguousarray(dec.transpose(1, 0, 2)).astype(np.float32)
    c["zeta"] = (gam[None, :] ** (127.0 - idx)[:, None]).astype(np.float32)
    xi = gam[:, None] ** (idx + 1.0)[None, :]
    xi512 = np.tile(xi, (1, 4))
    c["xitab"] = np.ascontiguousarray(np.broadcast_to(xi512[None], (128, 6, 512))).astype(np.float32)
    c["cdec"] = [float(g ** 128.0) for g in gam]
    c["maskT"] = (idx[None, :] >= idx[:, None]).astype(np.float32)
    c["tril"] = (idx[None, :] <= idx[:, None]).astype(np.float32)
    import ml_dtypes
    c["identb"] = np.eye(128).astype(ml_dtypes.bfloat16)
    return c


class Ctx:
    pass


def bc(ap_row, n=None):
    if n is None:
        n = 1
        for d in ap_row.shape:
            n *= d
    return bass.AP(ap_row.tensor, ap_row.offset, [[0, 128], [1, n]])


_uid = [0]


def _nm(n):
    _uid[0] += 1
    return "%s_u%d" % (n, _uid[0])


def phase_mod(P, nc, es, g):
    sb = lambda n, s, d: es.enter_context(nc.sbuf_tensor(_nm(n), s, d))
    c_sb = sb("c_sb", [128, 16], F32)
    c_act = sb("c_act", [128, 16], F32)
    ones = sb("ones_a", [128, 128], F32)
    c_rep = sb("c_rep", [128, 16, 128], F32R)
    wsl = [sb("adaw%d" % i, [128, 16, 512], F32R) for i in range(2)]
    bia = [sb("adab%d" % i, [1, 512], F32) for i in range(2)]
    row = [sb("adar%d" % i, [1, 512], F32) for i in range(2)]
    ps = [es.enter_context(nc.psum_tensor(_nm("adaps"), [128, 512], F32)) for i in range(2)]
    r_c, r_ca, r_ones, r_rep = Res("c"), Res("ca"), Res("ones"), Res("rep")
    r_w = [Res("w0"), Res("w1")]
    r_b = [Res("b0"), Res("b1")]
    r_row = [Res("r0"), Res("r1")]
    r_ps = [Res("p0"), Res("p1")]

    P.dma("sp", c_sb[:], g.c.rearrange("(p j) -> p j", j=16), writes=[r_c], dres=r_c)
    P.op("act", lambda e: e.activation(out=c_act[:], in_=c_sb[:], func=AF.Silu), reads=[r_c], writes=[r_ca])
    P.op("dve", lambda e: e.memset(ones[:], 1.0), writes=[r_ones])
    for j in range(16):
        P.op("act", lambda e, j=j: e.activation(out=c_rep[:, j, :], in_=ones[:], func=AF.Copy,
                                                scale=c_act[:, j:j + 1]),
             reads=[r_ca, r_ones], wadd=[r_rep])
    k = 0
    for l in range(2):
        awv = g.ada_w[l].rearrange("(p j) n -> p j n", j=16).bitcast(F32R)
        for nb in range(NMOD // 512):
            s = k % 2
            k += 1
            P.dma("sp", wsl[s][:], awv[:, :, nb * 512:(nb + 1) * 512], writes=[r_w[s]], dres=r_w[s])
            P.dma("sp", bia[s][:], g.ada_b[l:l + 1, nb * 512:(nb + 1) * 512], writes=[r_b[s]], dres=r_b[s])
            for j in range(16):
                P.op("pe", lambda e, j=j, s=s: e.matmul(ps[s][:], c_rep[:, j, :], wsl[s][:, j, :],
                                                        start=(j == 0), stop=(j == 15)),
                     reads=[r_rep, r_w[s]], writes=[r_ps[s]] if j == 0 else (), wadd=() if j == 0 else [r_ps[s]])
            P.op("dve", lambda e, s=s: e.tensor_tensor(out=row[s][:], in0=ps[s][0:1, :], in1=bia[s][:], op=ALU.add),
                 reads=[r_ps[s], r_b[s]], writes=[r_row[s]])
            P.dma("pool", g.mod_d[l:l + 1, nb * 512:(nb + 1) * 512], row[s][:], reads=[r_row[s]], dres=r_row[s])


def phase_proj(P, nc, es, g, l, S, x_src=None):
    if x_src is None:
        x_src = g.x
    sb = lambda n, s, d: es.enter_context(nc.sbuf_tensor(_nm(n), s, d))
    pst = lambda n: es.enter_context(nc.psum_tensor(_nm(n), [128, 512], F32))
    NG = S // 512
    ident = sb("ident", [128, 128], F32)
    PT = sb("PT", [128, 128], F32R)
    blk = sb("blk", [128, 128], F32R)
    nw = sb("nw", [128, 16], F32)
    scm = sb("scm", [128, 16], F32)
    a1 = sb("a1", [128, 16], F32)
    b1 = sb("b1", [128, 16], F32)
    wqk = sb("wqk", [128, 2], F32)
    xt = [sb("xt%d" % i, [128, D], F32) for i in range(2)]
    junk = sb("junk", [128, D], BF16)
    ss = [sb("ss%d" % i, [128, 1], F32) for i in range(2)]
    sq = [sb("sq%d" % i, [128, 1], F32) for i in range(2)]
    rstd = [sb("rstd%d" % i, [128, 1], F32) for i in range(2)]
    hT = [sb("hT%d" % i, [128, 16, 512], F32R) for i in range(2)]
    wsl = [sb("win%d" % i, [128, 16, 512], F32R) for i in range(2)]
    tabs = [sb("tab%d" % i, [128, 4, 512], F32) for i in range(2)]
    qraw = [sb("qraw%d" % i, [128, 512], F32R) for i in range(2)]
    sqf = [sb("sqf%d" % i, [128, 512], F32R) for i in range(2)]
    t1 = [sb("t1_%d" % i, [128, 512], F32) for i in range(2)]
    t2 = [sb("t2_%d" % i, [128, 512], F32) for i in range(2)]
    ofm = [sb("ofm%d" % i, [128, 512], BF16) for i in range(2)]
    otk = [sb("otk%d" % i, [128, 512], BF16) for i in range(2)]
    oxi = [sb("oxi%d" % i, [128, 512], BF16) for i in range(2)]
    xitab = sb("xitab", [128, 6, 512], F32)
    tp = [pst("tp%d" % i) for i in range(2)]
    acc = [pst("acc%d" % i) for i in range(2)]
    aux = [pst("aux%d" % i) for i in range(2)]

    R = lambda n: Res(n)
    r_const = R("const")
    r_ab = R("ab")
    r_xt = [R("xt0"), R("xt1")]
    r_ss = [R("ss0"), R("ss1")]
    r_sq = [R("sq0"), R("sq1")]
    r_rstd = [R("rs0"), R("rs1")]
    r_junk = R("junk")
    r_tp = [R("tp0"), R("tp1")]
    r_hT = [[[R("hT") for j in range(16)] for i in range(4)] for s in range(2)]
    r_w = [R("w0"), R("w1")]
    r_tab = [R("tab0"), R("tab1")]
    r_acc = [R("acc0"), R("acc1")]
    r_aux = [R("aux0"), R("aux1")]
    r_qraw = [R("q0"), R("q1")]
    r_sqf = [R("sf0"), R("sf1")]
    r_t1 = [R("t10"), R("t11")]
    r_t2 = [R("t20"), R("t21")]
    r_ofm = [R("of0"), R("of1")]
    r_otk = [R("ot0"), R("ot1")]
    r_oxi = [R("ox0"), R("ox1")]

    P.dma("sp", xitab[:], g.xitab, wadd=[r_const], dres=r_const)
    P.dma("sp", ident[:], g.ident, wadd=[r_const], dres=r_const)
    P.dma("sp", PT[:], g.PT.bitcast(F32R), wadd=[r_const], dres=r_const)
    P.dma("sp", blk[:], g.blk64.bitcast(F32R), wadd=[r_const], dres=r_const)
    P.dma("sp", nw[:], g.mix_norm_w[l].rearrange("(p j) -> p j", j=16), wadd=[r_const], dres=r_const)
    P.dma("sp", scm[:], g.mod_d[l, D:2 * D].rearrange("(p j) -> p j", j=16), wadd=[r_const], dres=r_const)
    P.dma("sp", b1[:], g.mod_d[l, 0:D].rearrange("(p j) -> p j", j=16), wadd=[r_const], dres=r_const)
    for half in range(2):
        P.dma("sp", wqk[half * 64:(half + 1) * 64, 0:1], g.diff_q_norm_w[l].rearrange("(p o) -> p o", o=1),
              wadd=[r_const], dres=r_const)
        P.dma("sp", wqk[half * 64:(half + 1) * 64, 1:2], g.diff_k_norm_w[l].rearrange("(p o) -> p o", o=1),
              wadd=[r_const], dres=r_const)
    P.op("dve", lambda e: e.scalar_tensor_tensor(out=a1[:], in0=scm[:], scalar=1.0, in1=nw[:],
                                                 op0=ALU.add, op1=ALU.mult),
         reads=[r_const], writes=[r_ab])

    if getattr(g, "dbg_stage", 99) < 1:
        return
    winv = g.w_in[l].rearrange("(p j) n -> p j n", j=16).bitcast(F32R)
    acts_tok = {3: (AF.Copy, AF.Copy), 4: (AF.Copy, AF.Silu), 5: (AF.Silu, AF.Silu),
                9: (AF.Copy, AF.Copy), 10: (AF.Copy, AF.Gelu), 11: (AF.Gelu, AF.Gelu), 12: (AF.Gelu,)}
    wk = 0
    ak = 0
    fk = 0
    tk = 0
    xk = 0
    tpk = 0
    for gi in range(NG):
        gs = gi % 2
        t0 = gi * 512
        import os
        SKIP = os.environ.get("MK_SKIP", "")
        if "rot" not in SKIP:
            P.dma("sp", tabs[gs][:], g.rot[:, :, t0:t0 + 512].rearrange("f p t -> p f t"),
                  writes=[r_tab[gs]], dres=r_tab[gs])
        for i in range(4):
            xs = xk % 2
            xk += 1
            r0 = t0 + i * 128
            P.dma("sp", xt[xs][:], x_src[r0:r0 + 128, :], writes=[r_xt[xs]], dres=r_xt[xs])
            P.op("act", lambda e, xs=xs: e.activation(out=junk[:], in_=xt[xs][:], func=AF.Square,
                                                      accum_out=ss[xs][:]),
                 reads=[r_xt[xs]], writes=[r_junk, r_ss[xs]])
            P.op("act", lambda e, xs=xs: e.activation(out=sq[xs][:], in_=ss[xs][:], func=AF.Sqrt,
                                                      scale=1.0 / D, bias=g.eps_t[:, 0:1]),
                 reads=[r_ss[xs]], writes=[r_sq[xs]])
            P.op("dve", lambda e, xs=xs: e.reciprocal(out=rstd[xs][:], in_=sq[xs][:]),
                 reads=[r_sq[xs]], writes=[r_rstd[xs]])
            P.op("dve", lambda e, xs=xs: e.tensor_scalar(out=xt[xs][:], in0=xt[xs][:], scalar1=rstd[xs][:, 0:1],
                                                         scalar2=None, op0=ALU.mult),
                 reads=[r_rstd[xs], r_xt[xs]], writes=[r_xt[xs]])
            xv = xt[xs][:].rearrange("p (q j) -> p j q", j=16)
            for rnd in range(4):
                tb = tpk % 2
                tpk += 1
                for jj in range(4):
                    j = rnd * 4 + jj
                    P.op("pe", lambda e, tb=tb, jj=jj, j=j, xv=xv: e.transpose(out=tp[tb][:, jj * 128:(jj + 1) * 128],
                                                                             in_=xv[:, j, :], identity=ident[:]),
                         reads=[r_xt[xs], r_const], writes=[r_tp[tb]] if jj == 0 else (),
                         wadd=() if jj == 0 else [r_tp[tb]])
                for jj in range(4):
                    j = rnd * 4 + jj
                    eng = "dve"
                    if eng == "dve":
                        fn = lambda e, tb=tb, jj=jj, j=j, i=i, gs=gs: e.tensor_scalar(
                            out=hT[gs][:, j, i * 128:(i + 1) * 128], in0=tp[tb][:, jj * 128:(jj + 1) * 128],
                            scalar1=a1[:, j:j + 1], scalar2=b1[:, j:j + 1], op0=ALU.mult, op1=ALU.add)
                    else:
                        fn = lambda e, tb=tb, jj=jj, j=j, i=i, gs=gs: e.activation(
                            out=hT[gs][:, j, i * 128:(i + 1) * 128], in_=tp[tb][:, jj * 128:(jj + 1) * 128],
                            func=AF.Identity, scale=a1[:, j:j + 1], bias=b1[:, j:j + 1])
                    if "evac" in SKIP or ("evacact" in SKIP and eng == "act") or ("evacdve" in SKIP and eng == "dve"):
                        continue
                    P.op(eng, fn, reads=[r_tp[tb], r_ab, r_const], writes=[r_hT[gs][i][j]])
        if getattr(g, "dbg_stage", 99) < 2:
            continue
        for b in range(13):
            if getattr(g, "dbg_stage", 99) < 3 and b not in (0,):
                continue
            if getattr(g, "dbg_stage", 99) < 4 and b not in (0, 6):
                continue
            if getattr(g, "dbg_stage", 99) < 5 and b not in (0, 6, 3):
                continue
            ncol = 512 if b < 12 else 256
            ws = wk % 2
            wk += 1
            P.dma("sp", wsl[ws][:, :, 0:ncol], winv[:, :, b * 512:b * 512 + ncol], writes=[r_w[ws]], dres=r_w[ws])
            if b in (0, 1, 2, 6, 7, 8):
                for ct in range(4):
                    a = ak % 2
                    ak += 1
                    for j in range(16):
                        P.op("pe", lambda e, a=a, ws=ws, ct=ct, j=j, gs=gs: e.matmul(
                            acc[a][:], wsl[ws][:, j, ct * 128:(ct + 1) * 128], hT[gs][:, j, :],
                            start=(j == 0), stop=(j == 15)),
                            reads=[r_w[ws]] + [r_hT[gs][i][j] for i in range(4)],
                            writes=[r_acc[a]] if j == 0 else (), wadd=() if j == 0 else [r_acc[a]])
                    f = fk % 2
                    fk += 1
                    col0 = b * 512 + ct * 128
                    if b < 3:
                        isk = col0 >= 768
                        hh = (col0 - (768 if isk else 0)) // 128
                        P.op("act", lambda e, a=a, f=f: e.activation(out=qraw[f][:], in_=acc[a][:], func=AF.Copy),
                             reads=[r_acc[a]], writes=[r_qraw[f]])
                        P.op("pe", lambda e, f=f: e.matmul(aux[f][:], PT[:], qraw[f][:], start=True, stop=True),
                             reads=[r_const, r_qraw[f]], writes=[r_aux[f]])
                        ci = 2 if isk else 0
                        P.op("dve", lambda e, f=f, ci=ci, gs=gs: e.tensor_tensor(out=t1[f][:], in0=qraw[f][:].bitcast(F32),
                                                                        in1=tabs[gs][:, ci, :], op=ALU.mult),
                             reads=[r_qraw[f], r_tab[gs]], writes=[r_t1[f]])
                        P.op("dve", lambda e, f=f, ci=ci, gs=gs: e.tensor_tensor(out=t2[f][:], in0=aux[f][:],
                                                                        in1=tabs[gs][:, ci + 1, :], op=ALU.mult),
                             reads=[r_aux[f], r_tab[gs]], writes=[r_t2[f]])
                        P.op("pool", lambda e, f=f: e.tensor_tensor(out=ofm[f][:], in0=t1[f][:], in1=t2[f][:],
                                                                   op=ALU.add),
                             reads=[r_t1[f], r_t2[f]], writes=[r_ofm[f]])
                        P.dma("pool", g.retqk_d[hh, 1 if isk else 0, :, t0:t0 + 512], ofm[f][:],
                              reads=[r_ofm[f]], dres=r_ofm[f])
                        if not isk:
                            P.op("pool", lambda e, f=f, hh=hh: e.tensor_tensor(out=oxi[f][:], in0=ofm[f][:],
                                                                              in1=xitab[:, hh, :], op=ALU.mult),
                                 reads=[r_ofm[f], r_const], writes=[r_oxi[f]])
                            P.dma("pool", g.retqk_d[hh, 2, :, t0:t0 + 512], oxi[f][:],
                                  reads=[r_oxi[f]], dres=r_oxi[f])
                    else:
                        cc = col0 - 3072
                        isk = cc >= 768
                        hh = (cc - (768 if isk else 0)) // 128
                        P.op("act", lambda e, a=a, f=f: e.activation(out=qraw[f][:], in_=acc[a][:], func=AF.Copy),
                             reads=[r_acc[a]], writes=[r_qraw[f]])
                        P.op("act", lambda e, a=a, f=f: e.activation(out=sqf[f][:], in_=acc[a][:], func=AF.Square),
                             reads=[r_acc[a]], writes=[r_sqf[f]])
                        P.op("pe", lambda e, f=f: e.matmul(aux[f][:], blk[:], sqf[f][:], start=True, stop=True),
                             reads=[r_const, r_sqf[f]], writes=[r_aux[f]])
                        P.op("act", lambda e, f=f: e.activation(out=t1[f][:], in_=aux[f][:], func=AF.Sqrt,
                                                                bias=g.eps_t[:, 0:1]),
                             reads=[r_aux[f]], writes=[r_t1[f]])
                        P.op("dve", lambda e, f=f: e.reciprocal(out=t2[f][:], in_=t1[f][:]),
                             reads=[r_t1[f]], writes=[r_t2[f]])
                        wi = 1 if isk else 0
                        P.op("dve", lambda e, f=f, wi=wi: e.scalar_tensor_tensor(
                            out=ofm[f][:], in0=qraw[f][:].bitcast(F32), scalar=wqk[:, wi:wi + 1], in1=t2[f][:],
                            op0=ALU.mult, op1=ALU.mult),
                            reads=[r_qraw[f], r_t2[f], r_const], writes=[r_ofm[f]])
                        P.dma("pool", g.diffqk_d[hh, 1 if isk else 0, :, t0:t0 + 512], ofm[f][:],
                              reads=[r_ofm[f]], dres=r_ofm[f])
            else:
                tcol0 = (b * 512 - 1536) if b <= 5 else (b * 512 - 3072)
                for i in range(4):
                    a = ak % 2
                    ak += 1
                    for j in range(16):
                        P.op("pe", lambda e, a=a, ws=ws, i=i, j=j, ncol=ncol, gs=gs: e.matmul(
                            acc[a][:, 0:ncol], hT[gs][:, j, i * 128:(i + 1) * 128], wsl[ws][:, j, 0:ncol],
                            start=(j == 0), stop=(j == 15)),
                            reads=[r_w[ws], r_hT[gs][i][j]],
                            writes=[r_acc[a]] if j == 0 else (), wadd=() if j == 0 else [r_acc[a]])
                    o = tk % 2
                    tk += 1
                    P.new_epoch(("act",), r_otk[o])
                    for hf, func in enumerate(acts_tok[b]):
                        P.op("act", lambda e, a=a, o=o, hf=hf, func=func: e.activation(
                            out=otk[o][:, hf * 256:(hf + 1) * 256], in_=acc[a][:, hf * 256:(hf + 1) * 256], func=func),
                            reads=[r_acc[a]], wadd=[r_otk[o]])
                    r0 = t0 + i * 128
                    P.dma("pool", g.tok_d[r0:r0 + 128, tcol0:tcol0 + ncol], otk[o][:, 0:ncol],
                          reads=[r_otk[o]], dres=r_otk[o])


def declare_io(nc, S, debug):
    g = Ctx()
    inp = lambda n, s, d=F32: nc.dram_tensor(n, s, d, kind="ExternalInput").ap()
    scr = lambda n, s, d=F32: nc.dram_tensor(n, s, d, kind="ExternalOutput" if debug else "Internal").ap()
    g.x = inp("x", [S, D])
    g.c = inp("c", [D])
    g.ada_w = inp("ada_w", [2, D, NMOD])
    g.ada_b = inp("ada_b", [2, NMOD])
    g.mix_norm_w = inp("mix_norm_w", [2, D])
    g.w_in = inp("w_in", [2, D, DIN])
    g.diff_q_norm_w = inp("diff_q_norm_w", [2, 64])
    g.diff_k_norm_w = inp("diff_k_norm_w", [2, 64])
    g.ident = inp("ident", [128, 128])
    g.PT = inp("PT", [128, 128])
    g.blk64 = inp("blk64", [128, 128])
    g.rot = inp("rot", [4, 128, S])
    g.xitab = inp("xitab", [128, 6, 512])
    g.decayT = inp("decayT", [128, 6, 128])
    g.zeta = inp("zeta", [128, 6])
    g.maskT = inp("maskT", [128, 128])
    g.tril = inp("tril", [128, 128])
    g.identb = inp("identb", [128, 128], BF16)
    g.ret_norm_w = inp("ret_norm_w", [2, 768])
    g.diff_lambda = inp("diff_lambda", [2, 4, 64])
    g.diff_subln_w = inp("diff_subln_w", [2, 128])
    g.gmlp_norm_w = inp("gmlp_norm_w", [2, 512])
    g.gmlp_norm_b = inp("gmlp_norm_b", [2, 512])
    g.gmlp_ws = inp("gmlp_ws", [2, 4, 128, 128])
    g.gmlp_bs = inp("gmlp_bs", [2, 4, 128])
    g.cat_d = scr("cat_d", [S, D])
    g.w_out = inp("w_out", [2, D, D])
    g.ffn_norm_w = inp("ffn_norm_w", [2, D])
    g.router_group_w = inp("router_group_w", [2, D, 4])
    g.router_group_b = inp("router_group_b", [2, 4])
    g.router_expert_w = inp("router_expert_w", [2, D, 32])
    g.router_expert_b = inp("router_expert_b", [2, 32])
    g.w_gu = inp("expert_w_gate_up", [2, NEXP, D, 2 * EH])
    g.w_dn = inp("expert_w_down", [2, NEXP, EH, D])
    NBLK = (2 * S) // BS + NEXP
    g.x1_d = scr("x1_d", [S, D])
    g.h2_d = scr("h2_d", [S + 1, D])
    g.slot_tok_d = scr("slot_tok_d", [NBLK * BS, 1], I32)
    g.yslot_d = scr("yslot_d", [NBLK * BS, D])
    g.x2_d = scr("x2_d", [S, D])
    g.out = nc.dram_tensor("out", [S, D], F32, kind="ExternalOutput").ap()
    if debug:
        g.dbg_eid = scr("dbg_eid", [128, S // 128, 2])
        g.dbg_gate = scr("dbg_gate", [128, S // 128, 2])
        g.dbg_dest = scr("dbg_dest", [128, S // 128, 2], I32)
        g.dbg_bex = scr("dbg_bex", [128, NBLK], I32)
    g.mod_d = scr("mod_d", [2, NMOD])
    g.retqk_d = scr("retqk_d", [6, 3, 128, S], BF16)
    g.diffqk_d = scr("diffqk_d", [6, 2, 128, S], BF16)
    g.tok_d = scr("tok_d", [S, 3328], BF16)
    return g


def build(S, debug=True, layers=(0,), dbg_stage=99, phases=("A", "B", "C1", "C2", "D1", "D2", "E", "F", "G")):
    nc = bass.Bass("TRN2", target_bir_lowering=False)
    nc.dge_precook = False
    g = declare_io(nc, S, debug)
    g.dbg_stage = dbg_stage
    P = Prog(nc)
    with ExitStack() as es0:
        g.eps_t = es0.enter_context(nc.sbuf_tensor(_nm("eps_t"), [128, 1], F32))
        r_eps = Res("eps")
        P.op("dve", lambda e: e.memset(g.eps_t[:], EPS), writes=[r_eps])
        if "A" in phases:
            with ExitStack() as es:
                phase_mod(P, nc, es, g)
                P.end_phase()
        for l in layers:
            if "B" in phases:
                with ExitStack() as es:
                    phase_proj(P, nc, es, g, l, S, g.x if l == 0 else g.x2_d)
                    P.end_phase()
            if "C1" in phases:
                with ExitStack() as es:
                    phase_retgmlp(P, nc, es, g, l, S)
                    P.end_phase()
            if "C2" in phases:
                with ExitStack() as es:
                    phase_diff(P, nc, es, g, l, S)
                    P.end_phase()
            x_src = g.x if l == 0 else g.x2_d
            x_dst = g.x2_d if l == 0 else g.out
            if "D1" in phases:
                with ExitStack() as es:
                    phase_wout(P, nc, es, g, l, S, x_src)
                    P.end_phase()
            with ExitStack() as esT:
                T = Ctx()
                NT = S // 128
                NBLK = (2 * S) // BS + NEXP
                T.eidA = esT.enter_context(nc.sbuf_tensor(_nm("eidA"), [128, NT, 2], F32))
                T.gateA = esT.enter_context(nc.sbuf_tensor(_nm("gateA"), [128, NT, 2], F32))
                T.destA = esT.enter_context(nc.sbuf_tensor(_nm("destA"), [128, NT, 2], I32))
                T.blkexp = esT.enter_context(nc.sbuf_tensor(_nm("blkexp"), [128, NBLK], I32))
                if "D2" in phases:
                    with ExitStack() as es:
                        phase_norm2(P, nc, es, g, l, S, T)
                        P.end_phase()
                if "E" in phases:
                    with ExitStack() as es:
                        phase_route(P, nc, es, g, l, S, T)
                        P.end_phase()
                    if debug:
                        rd = Res()
                        P.dma("sp", g.dbg_eid, T.eidA[:], dres=rd)
                        P.dma("sp", g.dbg_gate, T.gateA[:], dres=rd)
                        P.dma("sp", g.dbg_dest, T.destA[:], dres=rd)
                        P.dma("sp", g.dbg_bex, T.blkexp[:], dres=rd)
                        P.end_phase()
                if "F" in phases:
                    with ExitStack() as es:
                        phase_experts(P, nc, es, g, l, S, T)
                        P.end_phase()
                if "G" in phases:
                    with ExitStack() as es:
                        phase_combine(P, nc, es, g, l, S, T, x_dst)
                        P.end_phase()
    return nc


GAM = [1.0 - 2.0 ** (-5.0 - h) for h in range(6)]
CDEC = [g_ ** 128.0 for g_ in GAM]


def phase_retgmlp(P, nc, es, g, l, S):
    sb = lambda n, s, d: es.enter_context(nc.sbuf_tensor(_nm(n), s, d))
    pst = lambda n, d=F32, w=512: es.enter_context(nc.psum_tensor(_nm(n), [128, w], d))
    NCH = S // 128
    R = lambda n="": Res(n)
    decayT = sb("decayT", [128, 6, 128], F32)
    zeta = sb("zeta", [128, 6], F32)
    identb = sb("identb", [128, 128], BF16)
    identf = sb("identf", [128, 128], F32)
    tril = sb("tril", [128, 128], F32)
    retw = sb("retw", [128, 768], F32)
    gw = sb("gw", [128, 512], F32)
    gb = sb("gb", [128, 512], F32)
    bs = sb("bs", [128, 4], F32)
    wraw = sb("wraw", [128, 4, 128], F32)
    WT = sb("WT", [128, 4, 128], BF16)
    qk = [sb("qk%d" % i, [128, 6, 3, 512], BF16) for i in range(2)]
    tokrow = [sb("tokrow%d" % i, [128, 3328], BF16) for i in range(2)]
    st32 = sb("st32", [128, 6, 128], F32)
    st16 = sb("st16", [128, 6, 128], BF16)
    A = [sb("A%d" % i, [128, 128], BF16) for i in range(2)]
    kz = [sb("kz%d" % i, [128, 128], BF16) for i in range(2)]
    bst = sb("bst", [128, 6, 6], F32)
    mv = sb("mv", [128, 6, 2], F32)
    sd = sb("sd", [128, 6], F32)
    rs = sb("rs", [128, 6], F32)
    catr = [sb("catr%d" % i, [128, 768], F32) for i in range(2)]
    wsg = [sb("wsg%d" % i, [128, 768], F32) for i in range(2)]
    gst = sb("gst", [128, 6], F32)
    gmv = sb("gmv", [128, 2], F32)
    gsd = sb("gsd", [128, 1], F32)
    grs = sb("grs", [128, 1], F32)
    vn = sb("vn", [128, 512], F32)
    vnb = [sb("vnb%d" % i, [128, 512], BF16) for i in range(2)]
    catg = [sb("catg%d" % i, [128, 512], F32) for i in range(2)]
    yA = pst("yA")
    yB = pst("yB")
    sTp = pst("sTp")
    kvp = pst("kvp")
    ktr = pst("ktr", BF16, 1024)
    fps = pst("fps")
    wtp = pst("wtp")

    r_c = R("const")
    r_W = R("W")
    r_WT = R("WT")
    r_qk = [R(), R()]
    r_tok = [R(), R()]
    r_st32 = [R() for _ in range(6)]
    r_st16 = [R() for _ in range(6)]
    r_A = [R(), R()]
    r_kz = [R(), R()]
    r_sT = [R(), R()]
    r_kv = [R(), R()]
    r_ktr = [R(), R()]
    r_y = [R() for _ in range(6)]
    r_bst, r_mv, r_sd, r_rs = R(), R(), R(), R()
    r_catr = [R(), R()]
    r_wsg = [R(), R()]
    r_gst, r_gmv, r_gsd, r_grs, r_vn = R(), R(), R(), R(), R()
    r_vnb = [R(), R()]
    r_f = R()
    r_catg = [R(), R()]
    r_wtp = R()

    for dst, src in ((decayT, g.decayT), (zeta, g.zeta), (identb, g.identb), (identf, g.ident), (tril, g.tril)):
        P.dma("sp", dst[:], src, wadd=[r_c], dres=r_c)
    P.dma("sp", retw[:], bc(g.ret_norm_w[l]), wadd=[r_c], dres=r_c)
    P.dma("sp", gw[:], bc(g.gmlp_norm_w[l]), wadd=[r_c], dres=r_c)
    P.dma("sp", gb[:], bc(g.gmlp_norm_b[l]), wadd=[r_c], dres=r_c)
    P.dma("sp", bs[:], g.gmlp_bs[l].rearrange("g t -> t g"), wadd=[r_c], dres=r_c, allow_slow_non_contiguous=True)
    P.dma("sp", wraw[:], g.gmlp_ws[l].rearrange("g t s -> t g s"), writes=[r_W], dres=r_W)
    for gg in range(4):
        P.op("dve", lambda e, gg=gg: e.tensor_tensor(out=wraw[:, gg, :], in0=wraw[:, gg, :], in1=tril[:], op=ALU.mult),
             reads=[r_c], writes=[r_W])
        P.op("pe", lambda e, gg=gg: e.transpose(out=wtp[:, gg * 128:(gg + 1) * 128], in_=wraw[:, gg, :], identity=identf[:]),
             reads=[r_W, r_c], writes=[r_wtp])
        P.op("act", lambda e, gg=gg: e.activation(out=WT[:, gg, :], in_=wtp[:, gg * 128:(gg + 1) * 128], func=AF.Copy),
             reads=[r_wtp], wadd=[r_WT])
    P.op("dve", lambda e: e.memset(st32[:], 0.0), writes=r_st32)
    P.op("dve", lambda e: e.memset(st16[:], 0.0), writes=r_st16)

    sk = 0
    for n in range(NCH):
        ts = n % 2
        if n % 4 == 0:
            qs = (n // 4) % 2
            t0 = n * 128
            P.dma("sp", qk[qs][:], g.retqk_d[:, :, :, t0:t0 + 512].rearrange("h f p t -> p h f t"),
                  writes=[r_qk[qs]], dres=r_qk[qs])
        cc = n % 4
        P.dma("sp", tokrow[ts][:], g.tok_d[n * 128:(n + 1) * 128, :], writes=[r_tok[ts]], dres=r_tok[ts])
        P.op("pool", lambda e, ts=ts: e.tensor_tensor(out=wsg[ts][:], in0=tokrow[ts][:, 768:1536], in1=retw[:], op=ALU.mult),
             reads=[r_tok[ts], r_c], writes=[r_wsg[ts]])
        for h in range(6):
            a = sk % 2
            sk += 1
            qc = qk[qs][:, h, 0, cc * 128:(cc + 1) * 128]
            kc = qk[qs][:, h, 1, cc * 128:(cc + 1) * 128]
            qx = qk[qs][:, h, 2, cc * 128:(cc + 1) * 128]
            vh = tokrow[ts][:, h * 128:(h + 1) * 128]
            ybank = yA if h < 4 else yB
            yreg = ybank[:, (h % 4) * 128:(h % 4 + 1) * 128]
            P.op("pe", lambda e, a=a, kc=kc, qc=qc: e.matmul(sTp[:, a * 128:(a + 1) * 128], kc, qc, start=True, stop=True),
                 reads=[r_qk[qs]], writes=[r_sT[a]])
            P.op("dve", lambda e, a=a, h=h: e.tensor_tensor(out=A[a][:], in0=sTp[:, a * 128:(a + 1) * 128],
                                                          in1=decayT[:, h, :], op=ALU.mult),
                 reads=[r_sT[a], r_c], writes=[r_A[a]])
            P.op("pe", lambda e, a=a, yreg=yreg, vh=vh: e.matmul(yreg, A[a][:], vh, start=True, stop=False),
                 reads=[r_A[a], r_tok[ts]], writes=[r_y[h]])
            P.op("pe", lambda e, yreg=yreg, qx=qx, h=h: e.matmul(yreg, qx, st16[:, h, :], start=False, stop=True),
                 reads=[r_qk[qs], r_st16[h]], wadd=[r_y[h]])
            P.op("pe", lambda e, a=a, kc=kc: e.transpose(out=ktr[:, a * 128:(a + 1) * 128], in_=kc, identity=identb[:]),
                 reads=[r_qk[qs], r_c], writes=[r_ktr[a]])
            P.op("act", lambda e, a=a, h=h: e.activation(out=kz[a][:], in_=ktr[:, a * 128:(a + 1) * 128], func=AF.Copy,
                                                        scale=zeta[:, h:h + 1]),
                 reads=[r_ktr[a], r_c], writes=[r_kz[a]])
            P.op("pe", lambda e, a=a, vh=vh: e.matmul(kvp[:, a * 128:(a + 1) * 128], kz[a][:], vh, start=True, stop=True),
                 reads=[r_kz[a], r_tok[ts]], writes=[r_kv[a]])
            P.op("dve", lambda e, a=a, h=h: e.scalar_tensor_tensor(out=st32[:, h, :], in0=st32[:, h, :], scalar=float(CDEC[h]),
                                                                  in1=kvp[:, a * 128:(a + 1) * 128],
                                                                  op0=ALU.mult, op1=ALU.add),
                 reads=[r_kv[a]], writes=[r_st32[h]])
            P.op("act", lambda e, h=h: e.activation(out=st16[:, h, :], in_=st32[:, h, :], func=AF.Copy),
                 reads=[r_st32[h]], writes=[r_st16[h]])
        cs = n % 2
        for h in range(6):
            ybank = yA if h < 4 else yB
            yreg = ybank[:, (h % 4) * 128:(h % 4 + 1) * 128]
            P.op("dve", lambda e, h=h, yreg=yreg: e.bn_stats(out=bst[:, h, :], in_=yreg), reads=[r_y[h]], wadd=[r_bst])
        for h in range(6):
            P.op("dve", lambda e, h=h: e.bn_aggr(out=mv[:, h, :], in_=bst[:, h, :]), reads=[r_bst], wadd=[r_mv])
        P.op("act", lambda e: e.activation(out=sd[:], in_=mv[:, :, 1], func=AF.Sqrt, bias=g.eps_t[:, 0:1]),
             reads=[r_mv], writes=[r_sd])
        P.op("dve", lambda e: e.reciprocal(out=rs[:], in_=sd[:]), reads=[r_sd], writes=[r_rs])
        P.new_epoch(("dve",), r_catr[cs])
        for h in range(6):
            ybank = yA if h < 4 else yB
            yreg = ybank[:, (h % 4) * 128:(h % 4 + 1) * 128]
            P.op("dve", lambda e, h=h, yreg=yreg, cs=cs: e.tensor_scalar(out=catr[cs][:, h * 128:(h + 1) * 128], in0=yreg,
                                                                        scalar1=mv[:, h, 0:1], scalar2=rs[:, h:h + 1],
                                                                        op0=ALU.subtract, op1=ALU.mult),
                 reads=[r_y[h], r_mv, r_rs], wadd=[r_catr[cs]])
        P.new_epoch(("dve",), r_bst)
        P.new_epoch(("dve",), r_mv)
        P.op("pool", lambda e, cs=cs, ts=ts: e.tensor_tensor(out=catr[cs][:], in0=catr[cs][:], in1=wsg[ts][:], op=ALU.mult),
             reads=[r_wsg[ts]], writes=[r_catr[cs]])
        P.dma("pool", g.cat_d[n * 128:(n + 1) * 128, 0:768], catr[cs][:], reads=[r_catr[cs]], dres=r_catr[cs])
        gv = tokrow[ts][:, 2816:3328]
        P.op("dve", lambda e, gv=gv: e.bn_stats(out=gst[:], in_=gv), reads=[r_tok[ts]], writes=[r_gst])
        P.op("dve", lambda e: e.bn_aggr(out=gmv[:], in_=gst[:]), reads=[r_gst], writes=[r_gmv])
        P.op("act", lambda e: e.activation(out=gsd[:], in_=gmv[:, 1:2], func=AF.Sqrt, bias=g.eps_t[:, 0:1]),
             reads=[r_gmv], writes=[r_gsd])
        P.op("dve", lambda e: e.reciprocal(out=grs[:], in_=gsd[:]), reads=[r_gsd], writes=[r_grs])
        P.op("dve", lambda e, gv=gv: e.tensor_scalar(out=vn[:], in0=gv, scalar1=gmv[:, 0:1], scalar2=grs[:, 0:1],
                                                   op0=ALU.subtract, op1=ALU.mult),
             reads=[r_tok[ts], r_gmv, r_grs], writes=[r_vn])
        P.op("pool", lambda e: e.tensor_tensor(out=vn[:], in0=vn[:], in1=gw[:], op=ALU.mult), reads=[r_c], writes=[r_vn])
        P.op("pool", lambda e, cs=cs: e.tensor_tensor(out=vnb[cs][:], in0=vn[:], in1=gb[:], op=ALU.add),
             reads=[r_vn, r_c], writes=[r_vnb[cs]])
        for gg in range(4):
            P.op("pe", lambda e, gg=gg, cs=cs: e.matmul(fps[:, gg * 128:(gg + 1) * 128], WT[:, gg, :],
                                                       vnb[cs][:, gg * 128:(gg + 1) * 128], start=True, stop=True),
                 reads=[r_WT, r_vnb[cs]], writes=[r_f] if gg == 0 else (), wadd=() if gg == 0 else [r_f])
        P.new_epoch(("dve",), r_catg[cs])
        for gg in range(4):
            P.op("dve", lambda e, gg=gg, cs=cs, ts=ts: e.scalar_tensor_tensor(
                out=catg[cs][:, gg * 128:(gg + 1) * 128], in0=fps[:, gg * 128:(gg + 1) * 128], scalar=bs[:, gg:gg + 1],
                in1=tokrow[ts][:, 2304 + gg * 128:2304 + (gg + 1) * 128], op0=ALU.add, op1=ALU.mult),
                reads=[r_f, r_tok[ts], r_c], wadd=[r_catg[cs]])
        P.dma("pool", g.cat_d[n * 128:(n + 1) * 128, 1536:2048], catg[cs][:], reads=[r_catg[cs]], dres=r_catg[cs])


def phase_diff(P, nc, es, g, l, S):
    import math
    sb = lambda n, s, d: es.enter_context(nc.sbuf_tensor(_nm(n), s, d))
    pst = lambda n, d=F32, w=512: es.enter_context(nc.psum_tensor(_nm(n), [128, w], d))
    NCH = S // 128
    NQ = NCH // 4
    R = lambda n="": Res(n)
    lam_init = 0.8 - 0.6 * math.exp(-0.3 * l)
    maskT = sb("maskT", [128, 128], F32)
    wq = sb("wq", [128, 64], F32)
    wk = sb("wk", [128, 64], F32)
    lamt = sb("lamt", [128, 256], F32)
    tmp = sb("tmpd", [128, 64], F32)
    negM = sb("negM", [128, 1], F32)
    s01 = sb("s01", [128, 2], F32)
    nlam = sb("nlam", [128, 1], F32)
    subw = sb("subw", [128, 128], F32)
    qT = [sb("dqT%d" % i, [128, S], BF16) for i in range(2)]
    kT = [[sb("dkT%d_%d" % (i, j), [128, S], BF16) for j in range(2)] for i in range(2)]
    va = [sb("va%d" % i, [128, NCH, 129], BF16) for i in range(2)]
    pt = [sb("pt%d" % i, [128, 512], BF16) for i in range(3)]
    rr = sb("rr", [128, 2], F32)
    nl = sb("nl", [128, 1], F32)
    tt = [sb("tt%d" % i, [128, 128], F32) for i in range(2)]
    oo = [sb("oo%d" % i, [128, 128], F32) for i in range(2)]
    junk = sb("junkd", [128, 128], F32)
    ssq = sb("ssq", [128, 1], F32)
    sdd = sb("sdd", [128, 1], F32)
    rsd = sb("rsd", [128, 1], F32)
    catd = [sb("catd%d" % i, [128, 128], F32) for i in range(2)]
    accb = [[pst("accb%d_%d" % (p, i)) for i in range(3)] for p in range(2)]
    sTb = [pst("sTb%d" % i) for i in range(2)]

    r_c = R()
    r_q = [R(), R()]
    r_k = [R(), R()]
    r_v = [R(), R()]
    r_pt = [R(), R(), R()]
    r_sT = [R(), R()]
    r_acc = [[R(), R(), R()], [R(), R(), R()]]
    r_rr, r_nl, r_ssq, r_sdd, r_rsd, r_junk = R(), R(), R(), R(), R(), R()
    r_tt = [R(), R()]
    r_oo = [R(), R()]
    r_catd = [R(), R()]

    P.dma("sp", maskT[:], g.maskT, wadd=[r_c], dres=r_c)
    P.dma("sp", wq[:], bc(g.diff_q_norm_w[l]), wadd=[r_c], dres=r_c)
    P.dma("sp", wk[:], bc(g.diff_k_norm_w[l]), wadd=[r_c], dres=r_c)
    P.dma("sp", lamt[:], bc(g.diff_lambda[l]), wadd=[r_c], dres=r_c)
    P.dma("sp", subw[:], bc(g.diff_subln_w[l]), wadd=[r_c], dres=r_c)
    r_s = R()
    P.op("dve", lambda e: e.tensor_tensor(out=tmp[:], in0=wq[:], in1=wk[:], op=ALU.mult), reads=[r_c], writes=[r_s])
    P.op("dve", lambda e: e.tensor_reduce(out=negM[:], in_=tmp[:], axis=AX.X, op=ALU.max, apply_absolute_value=True),
         reads=[r_s], writes=[r_s])
    P.op("dve", lambda e: e.tensor_scalar(out=negM[:], in0=negM[:], scalar1=-8.0, scalar2=None, op0=ALU.mult),
         reads=[r_s], writes=[r_s])
    for i in range(2):
        P.op("dve", lambda e, i=i: e.tensor_tensor(out=tmp[:], in0=lamt[:, 128 * i:128 * i + 64], in1=lamt[:, 128 * i + 64:128 * i + 128], op=ALU.mult),
             reads=[r_c], writes=[r_s])
        P.op("dve", lambda e, i=i: e.tensor_reduce(out=s01[:, i:i + 1], in_=tmp[:], axis=AX.X, op=ALU.add),
             reads=[r_s], writes=[r_s])
    P.op("act", lambda e: e.activation(out=s01[:], in_=s01[:], func=AF.Exp), reads=[r_s], writes=[r_s])
    P.op("dve", lambda e: e.tensor_tensor(out=nlam[:], in0=s01[:, 1:2], in1=s01[:, 0:1], op=ALU.subtract),
         reads=[r_s], writes=[r_s])
    P.op("dve", lambda e: e.tensor_scalar(out=nlam[:], in0=nlam[:], scalar1=-lam_init, scalar2=None, op0=ALU.add),
         reads=[r_s], writes=[r_s])
    P.op("dve", lambda e: e.tensor_scalar(out=subw[:], in0=subw[:], scalar1=1.0 - lam_init, scalar2=None, op0=ALU.mult),
         reads=[r_c, r_s], writes=[r_s])
    r_c = r_s

    for hs_ in range(2):
        P.op("pool", lambda e, hs_=hs_: e.memset(va[hs_][:, :, 128:129], 1.0), writes=[r_v[hs_]])
        P.op("pool", lambda e, hs_=hs_: e.memset(kT[hs_][0][64:128, :], 0.0), writes=[r_k[hs_]])
        P.op("pool", lambda e, hs_=hs_: e.memset(kT[hs_][1][0:64, :], 0.0), wadd=[r_k[hs_]])
    ACO = 160
    def accreg(j, qi):
        idx = j * 4 + qi
        return idx // 3, (idx % 3) * ACO

    st_ = {"pk": 0, "sk": 0, "ok": 0, "qpar": 0}
    for h in range(6):
        hs = h % 2
        P.dma("sp", qT[hs][:], g.diffqk_d[h, 0], writes=[r_q[hs]], dres=r_q[hs])
        P.new_epoch(("sp",), r_k[hs])
        P.dma("sp", kT[hs][0][0:64, :], g.diffqk_d[h, 1, 0:64, :], wadd=[r_k[hs]], dres=r_k[hs])
        P.dma("sp", kT[hs][1][64:128, :], g.diffqk_d[h, 1, 64:128, :], wadd=[r_k[hs]], dres=r_k[hs])
        P.new_epoch(("sp",), r_v[hs]) if h >= 2 else None
        P.dma("sp", va[hs][:, :, 0:128], g.tok_d[:, 1536 + h * 128:1536 + (h + 1) * 128].rearrange("(n c) e -> c n e", c=128),
              reads=[r_v[hs]] if h < 2 else (), wadd=[r_v[hs]], dres=r_v[hs])
        steps = [(Q, kb, j) for Q in range(NQ) for kb in range(4 * Q + 4) for j in range(2)]

        def front(Q, kb, j):
            c0 = max(0, kb - 4 * Q)
            ncols = (4 - c0) * 128
            s_ = st_["sk"] % 2
            st_["sk"] += 1
            p_ = st_["pk"] % 3
            st_["pk"] += 1
            P.op("pe", lambda e, s_=s_, j=j, kb=kb, Q=Q, c0=c0, ncols=ncols, hs=hs: e.matmul(
                sTb[s_][:, 0:ncols], kT[hs][j][:, kb * 128:(kb + 1) * 128],
                qT[hs][:, Q * 512 + c0 * 128:Q * 512 + 512], start=True, stop=True),
                reads=[r_k[hs], r_q[hs]], writes=[r_sT[s_]])
            P.op("act", lambda e, s_=s_, p_=p_, ncols=ncols: e.activation(
                out=pt[p_][:, 0:ncols], in_=sTb[s_][:, 0:ncols], func=AF.Exp, scale=0.125, bias=negM[:, 0:1]),
                reads=[r_sT[s_], r_c], writes=[r_pt[p_]])
            if kb >= 4 * Q:
                P.op("pool", lambda e, p_=p_: e.tensor_tensor(out=pt[p_][:, 0:128], in0=pt[p_][:, 0:128],
                                                              in1=maskT[:], op=ALU.mult),
                     reads=[r_c], writes=[r_pt[p_]])
            return p_

        def back(Q, kb, j, p_, par):
            c0 = max(0, kb - 4 * Q)
            for qi in range(c0, 4):
                bnk, off = accreg(j, qi)
                first = (kb == 0 and (j * 4 + qi) % 3 == 0)
                P.op("pe", lambda e, bnk=bnk, off=off, p_=p_, qi=qi, c0=c0, kb=kb, hs=hs, first=first, Q=Q, par=par: e.matmul(
                    accb[par][bnk][:, off:off + 129], pt[p_][:, (qi - c0) * 128:(qi - c0 + 1) * 128],
                    va[hs][:, kb, :], start=first, stop=(kb == 4 * Q + qi), skip_group_check=True),
                    reads=[r_pt[p_], r_v[hs]],
                    writes=[r_acc[par][bnk]] if first else (), wadd=() if first else [r_acc[par][bnk]])

        def norm(Q, par):
                for qi in range(4):
                    o_ = st_["ok"] % 2
                    st_["ok"] += 1
                    b0, f0 = accreg(0, qi)
                    b1, f1 = accreg(1, qi)
                    P.op("dve", lambda e, b0=b0, f0=f0: e.reciprocal(out=rr[:, 0:1], in_=accb[par][b0][:, f0 + 128:f0 + 129]),
                         reads=[r_acc[par][b0]], writes=[r_rr])
                    P.op("dve", lambda e, b1=b1, f1=f1: e.reciprocal(out=rr[:, 1:2], in_=accb[par][b1][:, f1 + 128:f1 + 129]),
                         reads=[r_acc[par][b1]], wadd=[r_rr])
                    P.op("dve", lambda e: e.tensor_tensor(out=nl[:], in0=rr[:, 1:2], in1=nlam[:], op=ALU.mult),
                         reads=[r_rr, r_c], writes=[r_nl])
                    P.op("dve", lambda e, b1=b1, f1=f1, o_=o_: e.tensor_scalar(out=tt[o_][:], in0=accb[par][b1][:, f1:f1 + 128],
                                                                           scalar1=nl[:, 0:1], scalar2=None, op0=ALU.mult),
                         reads=[r_acc[par][b1], r_nl], writes=[r_tt[o_]])
                    P.op("dve", lambda e, b0=b0, f0=f0, o_=o_: e.scalar_tensor_tensor(
                        out=oo[o_][:], in0=accb[par][b0][:, f0:f0 + 128], scalar=rr[:, 0:1], in1=tt[o_][:],
                        op0=ALU.mult, op1=ALU.add),
                        reads=[r_acc[par][b0], r_rr, r_tt[o_]], writes=[r_oo[o_]])
                    P.op("act", lambda e, o_=o_: e.activation(out=junk[:], in_=oo[o_][:], func=AF.Square, accum_out=ssq[:]),
                         reads=[r_oo[o_]], writes=[r_junk, r_ssq])
                    P.op("act", lambda e: e.activation(out=sdd[:], in_=ssq[:], func=AF.Sqrt, scale=1.0 / 128, bias=g.eps_t[:, 0:1]),
                         reads=[r_ssq], writes=[r_sdd])
                    P.op("dve", lambda e: e.reciprocal(out=rsd[:], in_=sdd[:]), reads=[r_sdd], writes=[r_rsd])
                    P.op("dve", lambda e, o_=o_: e.scalar_tensor_tensor(out=catd[o_][:], in0=oo[o_][:], scalar=rsd[:, 0:1],
                                                                      in1=subw[:], op0=ALU.mult, op1=ALU.mult),
                         reads=[r_oo[o_], r_rsd, r_c], writes=[r_catd[o_]])
                    r0 = (4 * Q + qi) * 128
                    P.dma("pool", g.cat_d[r0:r0 + 128, 768 + h * 128:768 + (h + 1) * 128], catd[o_][:],
                          reads=[r_catd[o_]], dres=r_catd[o_])

        pend = None
        for i in range(len(steps) + 1):
            cur = None
            if i < len(steps):
                Q, kb, j = steps[i]
                cur = (Q, kb, j, front(Q, kb, j))
            if pend is not None:
                Q, kb, j, p_ = pend
                par = (st_["qpar"] + Q) % 2
                back(Q, kb, j, p_, par)
                if kb == 4 * Q + 3 and j == 1:
                    norm(Q, par)
            pend = cur
        st_["qpar"] += NQ


def phase_cat_test(P, nc, es, g, l, S):
    pass


BS = 256


def dyn_dma(e, out, src, regname):
    import re
    ins = e.dma_start(out=out, in_=src)
    m = re.search(r"R\[([A-Za-z]+)_tmp_(\d+)\]", ins.concise())
    if m:
        pre, n = m.group(1), int(m.group(2))
        for cand in range(n - 3, n + 1):
            for nm in ("%s_tmp_%d" % (pre, cand), "%s_%s_snap_%d" % (pre, regname, cand)):
                try:
                    e.free_register(bass.RegisterHandle(nm, mybir.EngineType.SP))
                except Exception:
                    pass
    return ins


def phase_wout(P, nc, es, g, l, S, x_src):
    sb = lambda n, s, d: es.enter_context(nc.sbuf_tensor(_nm(n), s, d))
    pst = lambda n: es.enter_context(nc.psum_tensor(_nm(n), [128, 512], F32))
    R = lambda n="": Res(n)
    NG = S // 512
    ident = sb("identw", [128, 128], F32)
    g1bc = sb("g1bc", [128, D], F32)
    ct = [sb("ct%d" % i, [128, D], F32) for i in range(2)]
    catT = [sb("catT%d" % i, [128, 16, 512], F32R) for i in range(2)]
    wsl = [sb("wo%d" % i, [128, 16, 512], F32R) for i in range(2)]
    xb = [sb("xb%d" % i, [128, 512], F32) for i in range(3)]
    tb = [sb("tb%d" % i, [128, 512], F32) for i in range(3)]
    tp = [pst("tpw%d" % i) for i in range(2)]
    acc = [pst("accw%d" % i) for i in range(2)]
    r_c = R()
    r_ct = [R(), R()]
    r_catT = [[[R() for r in range(4)] for i in range(4)] for s in range(2)]
    r_w = [R(), R()]
    r_xb = [R(), R(), R()]
    r_tb = [R(), R(), R()]
    r_tp = [R(), R()]
    r_acc = [R(), R()]
    P.dma("sp", ident[:], g.ident, wadd=[r_c], dres=r_c)
    P.dma("sp", g1bc[:], bc(g.mod_d[l, 2 * D:3 * D]), wadd=[r_c], dres=r_c)
    wv = g.w_out[l].rearrange("(p j) n -> p j n", j=16).bitcast(F32R)
    ck = tk = wk = ak = xk = 0
    for gi in range(NG):
        gs = gi % 2
        t0 = gi * 512
        for i in range(4):
            cs = ck % 2
            ck += 1
            r0 = t0 + i * 128
            P.dma("sp", ct[cs][:], g.cat_d[r0:r0 + 128, :], writes=[r_ct[cs]], dres=r_ct[cs])
            cv = ct[cs][:].rearrange("p (q j) -> p j q", j=16)
            for rnd in range(4):
                t_ = tk % 2
                tk += 1
                for jj in range(4):
                    j = rnd * 4 + jj
                    P.op("pe", lambda e, t_=t_, jj=jj, j=j, cv=cv: e.transpose(out=tp[t_][:, jj * 128:(jj + 1) * 128],
                                                                            in_=cv[:, j, :], identity=ident[:]),
                         reads=[r_ct[cs], r_c], writes=[r_tp[t_]] if jj == 0 else (), wadd=() if jj == 0 else [r_tp[t_]])
                eng = "dve" if rnd % 2 == 0 else "act"
                outv = catT[gs][:, rnd * 4:(rnd + 1) * 4, i * 128:(i + 1) * 128]
                inv_ = tp[t_][:].rearrange("p (a b) -> p a b", a=4)
                if eng == "dve":
                    fn = lambda e, outv=outv, inv_=inv_: e.tensor_copy(out=outv, in_=inv_)
                else:
                    fn = lambda e, outv=outv, inv_=inv_: e.activation(out=outv, in_=inv_, func=AF.Copy)
                P.op(eng, fn, reads=[r_tp[t_]], writes=[r_catT[gs][i][rnd]])
        for cb in range(4):
            ws = wk % 2
            wk += 1
            P.dma("sp", wsl[ws][:], wv[:, :, cb * 512:(cb + 1) * 512], writes=[r_w[ws]], dres=r_w[ws])
            for i in range(4):
                a = ak % 2
                ak += 1
                x_ = xk % 3
                xk += 1
                r0 = t0 + i * 128
                P.dma("sp", xb[x_][:], x_src[r0:r0 + 128, cb * 512:(cb + 1) * 512], writes=[r_xb[x_]], dres=r_xb[x_])
                for j in range(16):
                    P.op("pe", lambda e, a=a, ws=ws, i=i, j=j, gs=gs: e.matmul(
                        acc[a][:], catT[gs][:, j, i * 128:(i + 1) * 128], wsl[ws][:, j, :], start=(j == 0), stop=(j == 15)),
                        reads=[r_w[ws], r_catT[gs][i][j // 4]],
                        writes=[r_acc[a]] if j == 0 else (), wadd=() if j == 0 else [r_acc[a]])
                P.op("dve", lambda e, a=a, x_=x_, cb=cb: e.tensor_tensor(out=tb[x_][:], in0=acc[a][:],
                                                                        in1=g1bc[:, cb * 512:(cb + 1) * 512], op=ALU.mult),
                     reads=[r_acc[a], r_c], writes=[r_tb[x_]])
                P.op("pool", lambda e, x_=x_: e.tensor_tensor(out=tb[x_][:], in0=tb[x_][:], in1=xb[x_][:], op=ALU.add),
                     reads=[r_xb[x_]], writes=[r_tb[x_]])
                P.dma("pool", g.x1_d[r0:r0 + 128, cb * 512:(cb + 1) * 512], tb[x_][:], reads=[r_tb[x_]], dres=r_tb[x_])


def phase_norm2(P, nc, es, g, l, S, T):
    sb = lambda n, s, d: es.enter_context(nc.sbuf_tensor(_nm(n), s, d))
    pst = lambda n: es.enter_context(nc.psum_tensor(_nm(n), [128, 512], F32))
    R = lambda n="": Res(n)
    NT = S // 128
    ident = sb("identn", [128, 128], F32)
    a2bc = sb("a2bc", [128, D], F32)
    b2bc = sb("b2bc", [128, D], F32)
    tmpbc = sb("tmpbc", [128, D], F32)
    wr = sb("wr", [128, 16, 36], F32)
    rbb = sb("rbb", [128, 36], F32)
    zrow = sb("zrow", [1, D], F32)
    xt = [sb("x1t%d" % i, [128, D], F32) for i in range(2)]
    h2 = [sb("h2t%d" % i, [128, D], F32) for i in range(2)]
    junk = sb("junkn", [128, D], BF16)
    ss = sb("ssn", [128, 1], F32)
    sq = sb("sqn", [128, 1], F32)
    rstd = sb("rstdn", [128, 1], F32)
    h2T = [sb("h2T%d" % i, [128, 16, 128], F32) for i in range(2)]
    lgsA = sb("lgsA", [128, NT, 36], F32)
    io8 = sb("io8", [128, 8], F32)
    gmax = sb("gmax", [128, NT], F32)
    oh4 = sb("oh4", [128, NT, 4], F32)
    ex4 = sb("ex4", [128, NT, 4], F32)
    sume = sb("sume", [128, NT], F32)
    pg = sb("pg", [128, NT], F32)
    gif = sb("gif", [128, NT], F32)
    tmp48 = sb("tmp48", [128, NT, 4, 8], F32)
    els = sb("els", [128, NT, 8], F32)
    els2 = sb("els2", [128, NT, 8], F32)
    top1 = sb("top1", [128, NT], F32)
    top2 = sb("top2", [128, NT], F32)
    mk1 = sb("mk1", [128, NT, 8], F32)
    mk2 = sb("mk2", [128, NT, 8], F32)
    dd = sb("dd", [128, NT], F32)
    w0 = sb("w0", [128, NT], F32)
    tp = [pst("tpn%d" % i) for i in range(2)]
    lgp = pst("lgp")
    r_c, r_x, r_h2, r_tp, r_h2T, r_lg = R(), [R(), R()], [R(), R()], [R(), R()], [R(), R()], R()
    r_s = R()
    r_lgs = R()
    r_junk = R()
    P.dma("sp", ident[:], g.ident, wadd=[r_c], dres=r_c)
    P.dma("sp", a2bc[:], bc(g.mod_d[l, 4 * D:5 * D]), wadd=[r_c], dres=r_c)
    P.dma("sp", b2bc[:], bc(g.mod_d[l, 3 * D:4 * D]), wadd=[r_c], dres=r_c)
    P.dma("sp", tmpbc[:], bc(g.ffn_norm_w[l]), wadd=[r_c], dres=r_c)
    P.dma("sp", wr[:, :, 0:4], g.router_group_w[l].rearrange("(p j) n -> p j n", j=16), wadd=[r_c], dres=r_c)
    P.dma("sp", wr[:, :, 4:36], g.router_expert_w[l].rearrange("(p j) n -> p j n", j=16), wadd=[r_c], dres=r_c)
    P.dma("sp", rbb[:, 0:4], bc(g.router_group_b[l]), wadd=[r_c], dres=r_c)
    P.dma("sp", rbb[:, 4:36], bc(g.router_expert_b[l]), wadd=[r_c], dres=r_c)
    r_c2 = R()
    P.op("dve", lambda e: e.scalar_tensor_tensor(out=a2bc[:], in0=a2bc[:], scalar=1.0, in1=tmpbc[:], op0=ALU.add, op1=ALU.mult),
         reads=[r_c], writes=[r_c2])
    P.op("dve", lambda e: e.memset(zrow[:], 0.0), writes=[r_s])
    P.dma("pool", g.h2_d[S:S + 1, :], zrow[:], reads=[r_s], dres=r_s)
    tk = 0
    for i in range(NT):
        s = i % 2
        r0 = i * 128
        P.dma("sp", xt[s][:], g.x1_d[r0:r0 + 128, :], writes=[r_x[s]], dres=r_x[s])
        P.op("act", lambda e, s=s: e.activation(out=junk[:], in_=xt[s][:], func=AF.Square, accum_out=ss[:]),
             reads=[r_x[s]], writes=[r_junk, r_s])
        P.op("act", lambda e: e.activation(out=sq[:], in_=ss[:], func=AF.Sqrt, scale=1.0 / D, bias=g.eps_t[:, 0:1]),
             reads=[r_s], writes=[r_s])
        P.op("dve", lambda e: e.reciprocal(out=rstd[:], in_=sq[:]), reads=[r_s], writes=[r_s])
        P.op("dve", lambda e, s=s: e.scalar_tensor_tensor(out=h2[s][:], in0=xt[s][:], scalar=rstd[:, 0:1], in1=a2bc[:],
                                                        op0=ALU.mult, op1=ALU.mult),
             reads=[r_x[s], r_s, r_c2], writes=[r_h2[s]])
        P.op("pool", lambda e, s=s: e.tensor_tensor(out=h2[s][:], in0=h2[s][:], in1=b2bc[:], op=ALU.add),
             reads=[r_c], writes=[r_h2[s]])
        P.dma("pool", g.h2_d[r0:r0 + 128, :], h2[s][:], reads=[r_h2[s]], dres=r_h2[s])
        hv = h2[s][:].rearrange("p (q j) -> p j q", j=16)
        for rnd in range(4):
            t_ = tk % 2
            tk += 1
            for jj in range(4):
                j = rnd * 4 + jj
                P.op("pe", lambda e, t_=t_, jj=jj, j=j, hv=hv: e.transpose(out=tp[t_][:, jj * 128:(jj + 1) * 128],
                                                                        in_=hv[:, j, :], identity=ident[:]),
                     reads=[r_h2[s], r_c], writes=[r_tp[t_]] if jj == 0 else (), wadd=() if jj == 0 else [r_tp[t_]])
            outv = h2T[s][:, rnd * 4:(rnd + 1) * 4, :]
            inv_ = tp[t_][:].rearrange("p (a b) -> p a b", a=4)
            if rnd == 0:
                P.new_epoch(("dve", "act"), r_h2T[s])
            if rnd % 2 == 0:
                P.op("dve", lambda e, outv=outv, inv_=inv_: e.tensor_copy(out=outv, in_=inv_), reads=[r_tp[t_]], wadd=[r_h2T[s]])
            else:
                P.op("act", lambda e, outv=outv, inv_=inv_: e.activation(out=outv, in_=inv_, func=AF.Copy),
                     reads=[r_tp[t_]], wadd=[r_h2T[s]])
        for j in range(16):
            P.op("pe", lambda e, s=s, j=j: e.matmul(lgp[:, 0:36], h2T[s][:, j, :], wr[:, j, :], start=(j == 0), stop=(j == 15)),
                 reads=[r_h2T[s], r_c], writes=[r_lg] if j == 0 else (), wadd=() if j == 0 else [r_lg])
        P.op("dve", lambda e, i=i: e.tensor_tensor(out=lgsA[:, i, :], in0=lgp[:, 0:36], in1=rbb[:], op=ALU.add),
             reads=[r_lg, r_c], wadd=[r_lgs])
    X = lambda eng, fn: P.op(eng, fn, reads=[r_s, r_lgs], writes=[r_s])
    glv = lgsA[:, :, 0:4]
    elv = lgsA[:, :, 4:36].rearrange("p n (g e) -> p n g e", g=4)
    bcn = lambda t, k: t[:, :].unsqueeze(2).to_broadcast([128, NT, k])
    X("pool", lambda e: e.iota(io8[:], [[1, 8]], base=0, channel_multiplier=0, allow_small_or_imprecise_dtypes=True))
    io4b = bass.AP(io8, 0, [[8, 128], [0, NT], [1, 4]])
    io8b = bass.AP(io8, 0, [[8, 128], [0, NT], [1, 8]])
    X("dve", lambda e: e.tensor_reduce(out=gmax[:], in_=glv, axis=AX.X, op=ALU.max))
    X("dve", lambda e: e.tensor_tensor(out=oh4[:], in0=glv, in1=bcn(gmax, 4), op=ALU.is_equal))
    X("dve", lambda e: e.tensor_tensor(out=ex4[:], in0=glv, in1=bcn(gmax, 4), op=ALU.subtract))
    X("act", lambda e: e.activation(out=ex4[:], in_=ex4[:], func=AF.Exp))
    X("dve", lambda e: e.tensor_reduce(out=sume[:], in_=ex4[:], axis=AX.X, op=ALU.add))
    X("dve", lambda e: e.reciprocal(out=pg[:], in_=sume[:]))
    X("dve", lambda e: e.tensor_tensor(out=ex4[:], in0=oh4[:], in1=io4b, op=ALU.mult))
    X("dve", lambda e: e.tensor_reduce(out=gif[:], in_=ex4[:], axis=AX.X, op=ALU.add))
    X("dve", lambda e: e.tensor_tensor(out=tmp48[:], in0=elv, in1=oh4[:, :, :].unsqueeze(3).to_broadcast([128, NT, 4, 8]),
                                       op=ALU.mult))
    X("dve", lambda e: e.tensor_reduce(out=els[:], in_=tmp48[:].rearrange("p n g e -> p n e g"), axis=AX.X, op=ALU.add))
    X("dve", lambda e: e.tensor_reduce(out=top1[:], in_=els[:], axis=AX.X, op=ALU.max))
    X("dve", lambda e: e.tensor_tensor(out=mk1[:], in0=els[:], in1=bcn(top1, 8), op=ALU.is_equal))
    X("dve", lambda e: e.scalar_tensor_tensor(out=els2[:], in0=mk1[:], scalar=-1e30, in1=els[:], op0=ALU.mult, op1=ALU.add))
    X("dve", lambda e: e.tensor_reduce(out=top2[:], in_=els2[:], axis=AX.X, op=ALU.max))
    X("dve", lambda e: e.tensor_tensor(out=mk2[:], in0=els2[:], in1=bcn(top2, 8), op=ALU.is_equal))
    X("dve", lambda e: e.tensor_tensor(out=mk1[:], in0=mk1[:], in1=io8b, op=ALU.mult))
    X("dve", lambda e: e.tensor_tensor(out=mk2[:], in0=mk2[:], in1=io8b, op=ALU.mult))
    X("dve", lambda e: e.tensor_reduce(out=T.eidA[:, :, 0], in_=mk1[:], axis=AX.X, op=ALU.add))
    X("dve", lambda e: e.tensor_reduce(out=T.eidA[:, :, 1], in_=mk2[:], axis=AX.X, op=ALU.add))
    X("dve", lambda e: e.tensor_scalar(out=gif[:], in0=gif[:], scalar1=8.0, scalar2=None, op0=ALU.mult))
    X("dve", lambda e: e.tensor_tensor(out=T.eidA[:], in0=T.eidA[:], in1=bcn(gif, 2), op=ALU.add))
    X("dve", lambda e: e.tensor_tensor(out=dd[:], in0=top2[:], in1=top1[:], op=ALU.subtract))
    X("act", lambda e: e.activation(out=dd[:], in_=dd[:], func=AF.Exp))
    X("dve", lambda e: e.tensor_scalar(out=w0[:], in0=dd[:], scalar1=1.0, scalar2=None, op0=ALU.add))
    X("dve", lambda e: e.reciprocal(out=w0[:], in_=w0[:]))
    X("dve", lambda e: e.tensor_tensor(out=dd[:], in0=dd[:], in1=w0[:], op=ALU.mult))
    X("dve", lambda e: e.tensor_tensor(out=T.gateA[:, :, 0], in0=w0[:], in1=pg[:], op=ALU.mult))
    X("dve", lambda e: e.tensor_tensor(out=T.gateA[:, :, 1], in0=dd[:], in1=pg[:], op=ALU.mult))

def phase_route(P, nc, es, g, l, S, T):
    sb = lambda n, s, d: es.enter_context(nc.sbuf_tensor(_nm(n), s, d))
    pst = lambda n: es.enter_context(nc.psum_tensor(_nm(n), [128, 512], F32))
    R = lambda n="": Res(n)
    NT = S // 128
    NBLK = (2 * S) // BS + NEXP
    NSLOT = NBLK * BS
    JM = S // BS
    iota32 = sb("iota32", [128, 32], F32)
    thr = sb("thr", [128, JM], F32)
    biota = sb("biota", [128, NBLK], F32)
    oh = sb("oh", [128, NT, 2, 32], F32)
    M = sb("M", [128, NT, 32], F32)
    Mb = sb("Mb", [128, NT, 32], BF16)
    U = sb("U", [128, 128], BF16)
    ones = sb("onesr", [128, 128], BF16)
    onesf = sb("onesf", [128, 32], F32)
    rank = sb("rank", [128, NT, 32], F32)
    Rtot = sb("Rtot", [128, 32], F32)
    cmp_ = sb("cmp", [128, 32, JM], F32)
    nbk = sb("nbk", [128, 32], F32)
    pendb = sb("pendb", [128, 32], F32)
    pstart = sb("pstart", [128, 32], F32)
    tmp4 = sb("tmp4", [128, NT, 2, 32], F32)
    destf = sb("destf", [128, NT, 2], F32)
    cmpb = sb("cmpb", [128, NBLK, 32], F32)
    bexf = sb("bexf", [128, NBLK], F32)
    tokid = sb("tokid", [128, NT], I32)
    initt = sb("initt", [128, NSLOT // 128], I32)
    wps = [pst("wps%d" % i) for i in range(2)]
    r_s = R()
    r_M = R()
    r_w = [R(), R()]
    r_rank = R()
    X = lambda eng, fn, extra=(), wr=None: P.op(eng, fn, reads=[r_s] + list(extra), writes=[r_s] if wr is None else wr)
    X("pool", lambda e: e.iota(iota32[:], [[1, 32]], base=0, channel_multiplier=0, allow_small_or_imprecise_dtypes=True))
    X("pool", lambda e: e.iota(thr[:], [[BS, JM]], base=0, channel_multiplier=0, allow_small_or_imprecise_dtypes=True))
    X("pool", lambda e: e.iota(biota[:], [[1, NBLK]], base=0, channel_multiplier=0, allow_small_or_imprecise_dtypes=True))
    X("pool", lambda e: e.iota(tokid[:], [[128, NT]], base=0, channel_multiplier=1))
    X("pool", lambda e: e.memset(initt[:], S))
    X("pool", lambda e: e.memset(ones[:], 1.0))
    X("pool", lambda e: e.memset(onesf[:], 1.0))
    X("pool", lambda e: e.memset(Rtot[:], 0.0))
    X("pool", lambda e: e.affine_select(out=U[:], in_=ones[:], pattern=[[1, 128]], compare_op=ALU.is_ge, fill=0.0,
                                        base=-1, channel_multiplier=-1))
    P.dma("sp", g.slot_tok_d.rearrange("(p j) o -> p (j o)", p=128), initt[:], reads=[r_s], dres=r_s)
    iob = bass.AP(iota32, 0, [[32, 128], [0, NT], [0, 2], [1, 32]])
    X("dve", lambda e: e.tensor_tensor(out=oh[:], in0=T.eidA[:, :, :].unsqueeze(3).to_broadcast([128, NT, 2, 32]),
                                       in1=iob, op=ALU.is_equal))
    X("dve", lambda e: e.tensor_tensor(out=M[:], in0=oh[:, :, 0, :], in1=oh[:, :, 1, :], op=ALU.add))
    X("dve", lambda e: e.tensor_copy(out=Mb[:], in_=M[:]), wr=[r_s, r_M])
    for i in range(NT):
        w_ = i % 2
        P.op("pe", lambda e, w_=w_, i=i: e.matmul(wps[w_][:, 0:32], U[:], Mb[:, i, :], start=True, stop=True),
             reads=[r_M, r_s], writes=[r_w[w_]])
        P.op("pe", lambda e, w_=w_, i=i: e.matmul(wps[w_][:, 32:64], ones[:], Mb[:, i, :], start=True, stop=True),
             reads=[r_M, r_s], wadd=[r_w[w_]])
        X("dve", lambda e, w_=w_, i=i: e.tensor_tensor(out=rank[:, i, :], in0=wps[w_][:, 0:32], in1=Rtot[:], op=ALU.add),
          extra=[r_w[w_]])
        X("dve", lambda e, w_=w_: e.tensor_tensor(out=Rtot[:], in0=wps[w_][:, 32:64], in1=Rtot[:], op=ALU.add),
          extra=[r_w[w_]])
    X("dve", lambda e: e.tensor_tensor(out=cmp_[:], in0=Rtot[:, :].unsqueeze(2).to_broadcast([128, 32, JM]),
                                       in1=bass.AP(thr, 0, [[JM, 128], [0, 32], [1, JM]]), op=ALU.is_gt))
    X("dve", lambda e: e.tensor_reduce(out=nbk[:], in_=cmp_[:], axis=AX.X, op=ALU.add))
    X("dve", lambda e: e.tensor_tensor_scan(out=pendb[:], data0=onesf[:], data1=nbk[:], initial=0.0, op0=ALU.mult, op1=ALU.add))
    X("dve", lambda e: e.tensor_tensor(out=pstart[:], in0=pendb[:], in1=nbk[:], op=ALU.subtract))
    X("dve", lambda e: e.tensor_scalar(out=pstart[:], in0=pstart[:], scalar1=float(BS), scalar2=None, op0=ALU.mult))
    X("dve", lambda e: e.tensor_tensor(out=rank[:], in0=rank[:], in1=bass.AP(pstart, 0, [[32, 128], [0, NT], [1, 32]]),
                                       op=ALU.add))
    X("dve", lambda e: e.tensor_tensor(out=tmp4[:], in0=oh[:],
                                       in1=bass.AP(rank, 0, [[NT * 32, 128], [32, NT], [0, 2], [1, 32]]), op=ALU.mult))
    X("dve", lambda e: e.tensor_reduce(out=destf[:], in_=tmp4[:], axis=AX.X, op=ALU.add))
    X("dve", lambda e: e.tensor_copy(out=T.destA[:], in_=destf[:]))
    X("dve", lambda e: e.tensor_tensor(out=cmpb[:], in0=bass.AP(pendb, 0, [[32, 128], [0, NBLK], [1, 32]]),
                                       in1=biota[:, :].unsqueeze(2).to_broadcast([128, NBLK, 32]), op=ALU.is_le))
    X("dve", lambda e: e.tensor_reduce(out=bexf[:], in_=cmpb[:], axis=AX.X, op=ALU.add))
    X("dve", lambda e: e.tensor_scalar(out=bexf[:], in0=bexf[:], scalar1=31.0, scalar2=None, op0=ALU.min))
    X("dve", lambda e: e.tensor_copy(out=T.blkexp[:], in_=bexf[:]))
    r_sc = R()
    for i in range(NT):
        for k in range(2):
            P.raw("pool", lambda e, i=i, k=k: e.indirect_dma_start(
                out=g.slot_tok_d, out_offset=bass.IndirectOffsetOnAxis(ap=T.destA[:, i, k:k + 1], axis=0),
                in_=tokid[:, i:i + 1], in_offset=None),
                dres=r_sc, reads=[r_s])


def phase_experts(P, nc, es, g, l, S, T):
    sb = lambda n, s, d: es.enter_context(nc.sbuf_tensor(_nm(n), s, d))
    pst = lambda n: es.enter_context(nc.psum_tensor(_nm(n), [128, 512], F32))
    R = lambda n="": Res(n)
    NBLK = (2 * S) // BS + NEXP
    ident = sb("idente", [128, 128], F32)
    idx = [sb("idx%d" % i, [128, 2], I32) for i in range(2)]
    xg = [sb("xg%d" % i, [128, D], F32) for i in range(2)]
    xbT = [sb("xbT%d" % i, [128, 16, BS], F32R) for i in range(2)]
    wgu = [sb("wgu%d" % i, [128, 16, 2, 256], F32R) for i in range(2)]
    wdn = [sb("wdn%d" % i, [128, 8, 512], F32R) for i in range(2)]
    sa = [sb("sa%d" % i, [128, BS], F32) for i in range(2)]
    hid = [sb("hid%d" % i, [128, EH], F32) for i in range(2)]
    hT = [sb("hTe%d" % i, [128, 8, BS], F32R) for i in range(2)]
    yb = [sb("yb%d" % i, [128, D], F32) for i in range(2)]
    tp = [pst("tpe%d" % i) for i in range(2)]
    gu = [pst("gu%d" % i) for i in range(2)]
    dn = [pst("dn%d" % i) for i in range(2)]
    r_c = R()
    r_idx = [R(), R()]
    r_xg = [R(), R()]
    r_xbT = [[[R() for r in range(4)] for s in range(2)] for b in range(2)]
    r_wgu = [R(), R()]
    r_wdn = [R(), R()]
    r_sa = [R(), R()]
    r_hid = [[R() for q in range(4)] for s in range(2)]
    r_hT2 = [[[R() for r in range(2)] for s in range(2)] for b in range(2)]
    r_yb = [R(), R()]
    r_tp = [R(), R()]
    r_gu = [R(), R()]
    r_dn = [R(), R()]
    P.dma("sp", ident[:], g.ident, writes=[r_c], dres=r_c)
    GU_E = D * D
    DN_E = EH * D
    if not hasattr(g, "regs"):
        g.regs = {}
    regs = g.regs

    def getreg(e, name):
        if name not in regs:
            regs[name] = e.alloc_register(name)
        return regs[name]
    gk = wk = dk = tk = uk = nk = 0
    for b in range(NBLK):
        bs_ = b % 2
        P.dma("sp", idx[bs_][:], g.slot_tok_d[b * BS:(b + 1) * BS, :].rearrange("(s p) o -> p (s o)", p=128),
              writes=[r_idx[bs_]], dres=r_idx[bs_], allow_slow_non_contiguous=True)
        for s in range(2):
            x_ = gk % 2
            gk += 1
            P.raw("pool", lambda e, x_=x_, bs_=bs_, s=s: e.indirect_dma_start(
                out=xg[x_][:], out_offset=None, in_=g.h2_d,
                in_offset=bass.IndirectOffsetOnAxis(ap=idx[bs_][:, s:s + 1], axis=0)),
                dres=r_xg[x_], reads=[r_idx[bs_]], writes=[r_xg[x_]])
            xv = xg[x_][:].rearrange("p (q j) -> p j q", j=16)
            for rnd in range(4):
                t_ = tk % 2
                tk += 1
                for jj in range(4):
                    j = rnd * 4 + jj
                    P.op("pe", lambda e, t_=t_, jj=jj, j=j, xv=xv: e.transpose(out=tp[t_][:, jj * 128:(jj + 1) * 128],
                                                                            in_=xv[:, j, :], identity=ident[:]),
                         reads=[r_xg[x_], r_c], writes=[r_tp[t_]] if jj == 0 else (), wadd=() if jj == 0 else [r_tp[t_]])
                outv = xbT[bs_][:, rnd * 4:(rnd + 1) * 4, s * 128:(s + 1) * 128]
                inv_ = tp[t_][:].rearrange("p (a b) -> p a b", a=4)
                if rnd % 2 == 0:
                    P.op("dve", lambda e, outv=outv, inv_=inv_: e.tensor_copy(out=outv, in_=inv_),
                         reads=[r_tp[t_]], writes=[r_xbT[bs_][s][rnd]])
                else:
                    P.op("act", lambda e, outv=outv, inv_=inv_: e.activation(out=outv, in_=inv_, func=AF.Copy),
                         reads=[r_tp[t_]], writes=[r_xbT[bs_][s][rnd]])
        for q in range(4):
            w_ = wk % 2
            wk += 1

            def ld_gu(e, b=b, q=q, w_=w_):
                rb = getreg(e, "r_eb")
                ro = getreg(e, "r_off")
                e.reg_load(rb, T.blkexp[0:1, b:b + 1])
                e.reg_mul(ro, rb, GU_E)
                e.reg_add(ro, ro, l * NEXP * GU_E + q * 256)
                src = bass.AP(g.w_gu.tensor, ro, [[16 * D, 128], [D, 16], [1024, 2], [1, 256]]).bitcast(F32R)
                return dyn_dma(e, wgu[w_][:], src, ro.name)
            P.raw("sp", ld_gu, dres=r_wgu[w_], writes=[r_wgu[w_]])
            for s in range(2):
                u_ = uk % 2
                uk += 1
                for j in range(16):
                    P.op("pe", lambda e, u_=u_, w_=w_, s=s, j=j, bs_=bs_: e.matmul(
                        gu[u_][:], xbT[bs_][:, j, s * 128:(s + 1) * 128],
                        wgu[w_][:, j, :, :].rearrange("p a c -> p (a c)"), start=(j == 0), stop=(j == 15)),
                        reads=[r_wgu[w_], r_xbT[bs_][s][j // 4]],
                        writes=[r_gu[u_]] if j == 0 else (), wadd=() if j == 0 else [r_gu[u_]])
                a_ = nk % 2
                nk += 1
                P.op("act", lambda e, a_=a_, u_=u_: e.activation(out=sa[a_][:], in_=gu[u_][:, 0:256], func=AF.Silu),
                     reads=[r_gu[u_]], writes=[r_sa[a_]])
                P.op("dve", lambda e, a_=a_, u_=u_, s=s, q=q: e.tensor_tensor(
                    out=hid[s][:, q * 256:(q + 1) * 256], in0=sa[a_][:], in1=gu[u_][:, 256:512], op=ALU.mult),
                    reads=[r_sa[a_], r_gu[u_]], writes=[r_hid[s][q]])
        for s in range(2):
            for rnd in range(2):
                t_ = tk % 2
                tk += 1
                for jj in range(4):
                    c_ = rnd * 4 + jj
                    P.op("pe", lambda e, t_=t_, jj=jj, c_=c_, s=s: e.transpose(
                        out=tp[t_][:, jj * 128:(jj + 1) * 128], in_=hid[s][:, c_ * 128:(c_ + 1) * 128], identity=ident[:]),
                        reads=[r_hid[s][c_ // 2], r_c], writes=[r_tp[t_]] if jj == 0 else (), wadd=() if jj == 0 else [r_tp[t_]])
                outv = hT[bs_][:, rnd * 4:(rnd + 1) * 4, s * 128:(s + 1) * 128]
                inv_ = tp[t_][:].rearrange("p (a b) -> p a b", a=4)
                if rnd % 2 == 0:
                    P.op("dve", lambda e, outv=outv, inv_=inv_: e.tensor_copy(out=outv, in_=inv_),
                         reads=[r_tp[t_]], writes=[r_hT2[bs_][s][rnd]])
                else:
                    P.op("act", lambda e, outv=outv, inv_=inv_: e.activation(out=outv, in_=inv_, func=AF.Copy),
                         reads=[r_tp[t_]], writes=[r_hT2[bs_][s][rnd]])
        for q in range(4):
            d_ = dk % 2
            dk += 1

            def ld_dn(e, b=b, q=q, d_=d_):
                rb = getreg(e, "r_eb")
                ro = getreg(e, "r_off")
                e.reg_load(rb, T.blkexp[0:1, b:b + 1])
                e.reg_mul(ro, rb, DN_E)
                e.reg_add(ro, ro, l * NEXP * DN_E + q * 512)
                src = bass.AP(g.w_dn.tensor, ro, [[D, 128], [128 * D, 8], [1, 512]]).bitcast(F32R)
                return dyn_dma(e, wdn[d_][:], src, ro.name)
            P.raw("sp", ld_dn, dres=r_wdn[d_], writes=[r_wdn[d_]])
            for s in range(2):
                o_ = (dk + s) % 2
                if q == 0:
                    P.new_epoch(("act", "dve"), r_yb[s])
                for c_ in range(8):
                    P.op("pe", lambda e, o_=o_, c_=c_, s=s, d_=d_, bs_=bs_: e.matmul(
                        dn[o_][:], hT[bs_][:, c_, s * 128:(s + 1) * 128], wdn[d_][:, c_, :], start=(c_ == 0), stop=(c_ == 7)),
                        reads=[r_wdn[d_], r_hT2[bs_][s][c_ // 4]],
                        writes=[r_dn[o_]] if c_ == 0 else (), wadd=() if c_ == 0 else [r_dn[o_]])
                if s == 0:
                    P.op("act", lambda e, o_=o_, s=s, q=q: e.activation(out=yb[s][:, q * 512:(q + 1) * 512], in_=dn[o_][:], func=AF.Copy),
                         reads=[r_dn[o_]], wadd=[r_yb[s]])
                else:
                    P.op("dve", lambda e, o_=o_, s=s, q=q: e.tensor_copy(out=yb[s][:, q * 512:(q + 1) * 512], in_=dn[o_][:]),
                         reads=[r_dn[o_]], wadd=[r_yb[s]])
        for s in range(2):
            r0 = b * BS + s * 128
            P.dma("pool", g.yslot_d[r0:r0 + 128, :], yb[s][:], reads=[r_yb[s]], dres=r_yb[s])


def phase_combine(P, nc, es, g, l, S, T, x_dst):
    sb = lambda n, s, d: es.enter_context(nc.sbuf_tensor(_nm(n), s, d))
    R = lambda n="": Res(n)
    NT = S // 128
    NB = 3
    g2bc = sb("g2bc", [128, D], F32)
    y0 = [sb("y0_%d" % i, [128, D], F32) for i in range(NB)]
    y1 = [sb("y1_%d" % i, [128, D], F32) for i in range(NB)]
    xt = [sb("xc%d" % i, [128, D], F32) for i in range(NB)]
    ac = [sb("ac%d" % i, [128, D], F32) for i in range(2)]
    r_c = R()
    r_y0, r_y1, r_x = [R() for _ in range(NB)], [R() for _ in range(NB)], [R() for _ in range(NB)]
    r_ac = [R(), R()]
    P.dma("sp", g2bc[:], bc(g.mod_d[l, 5 * D:6 * D]), writes=[r_c], dres=r_c)

    def loads(i):
        s = i % NB
        r0 = i * 128
        P.raw("pool", lambda e, s=s, i=i: e.indirect_dma_start(
            out=y0[s][:], out_offset=None, in_=g.yslot_d,
            in_offset=bass.IndirectOffsetOnAxis(ap=T.destA[:, i, 0:1], axis=0)),
            dres=r_y0[s], writes=[r_y0[s]])
        P.raw("pool", lambda e, s=s, i=i: e.indirect_dma_start(
            out=y1[s][:], out_offset=None, in_=g.yslot_d,
            in_offset=bass.IndirectOffsetOnAxis(ap=T.destA[:, i, 1:2], axis=0)),
            dres=r_y1[s], writes=[r_y1[s]])
        P.dma("sp", xt[s][:], g.x1_d[r0:r0 + 128, :], writes=[r_x[s]], dres=r_x[s])
    loads(0)
    if NT > 1:
        loads(1)
    for i in range(NT):
        if i + 2 < NT:
            loads(i + 2)
        s = i % NB
        a = i % 2
        r0 = i * 128
        P.op("act", lambda e, s=s, i=i: e.activation(out=y0[s][:], in_=y0[s][:], func=AF.Copy, scale=T.gateA[:, i, 0:1]),
             reads=[r_y0[s]], writes=[r_y0[s]])
        P.op("dve", lambda e, s=s, a=a, i=i: e.scalar_tensor_tensor(out=ac[a][:], in0=y1[s][:], scalar=T.gateA[:, i, 1:2], in1=y0[s][:],
                                                                   op0=ALU.mult, op1=ALU.add),
             reads=[r_y1[s], r_y0[s]], writes=[r_ac[a]])
        P.op("dve", lambda e, a=a: e.tensor_tensor(out=ac[a][:], in0=ac[a][:], in1=g2bc[:], op=ALU.mult),
             reads=[r_c], writes=[r_ac[a]])
        P.op("dve", lambda e, s=s, a=a: e.tensor_tensor(out=ac[a][:], in0=ac[a][:], in1=xt[s][:], op=ALU.add),
             reads=[r_x[s]], writes=[r_ac[a]])
        P.dma("sp", x_dst[r0:r0 + 128, :], ac[a][:], reads=[r_ac[a]], dres=r_ac[a])


_CONST_KEYS = ["ident", "PT", "blk64", "rot", "xitab", "decayT", "zeta", "maskT", "tril", "identb"]
_S = 4096
_NCORES = 8


def kernel(**inputs):
    S = _S
    nc = build(S, debug=False, layers=(0, 1))
    cst = host_consts(S)
    x = np.ascontiguousarray(np.asarray(inputs["x"], dtype=np.float32))
    c = np.ascontiguousarray(np.asarray(inputs["c"], dtype=np.float32))
    shared = {}
    for k, v in inputs.items():
        if k in ("x", "c"):
            continue
        shared[k] = np.ascontiguousarray(np.asarray(v, dtype=np.float32))
    for k in _CONST_KEYS:
        shared[k] = cst[k]
    in_maps = []
    for b in range(_NCORES):
        m = dict(shared)
        m["x"] = x[b]
        m["c"] = c[b]
        in_maps.append(m)
    res = run_bass_kernel_spmd(nc, in_maps, core_ids=list(range(_NCORES)))
    out = np.stack([np.asarray(r["out"]) for r in res.results], axis=0)
    return out.astype(np.float32)
```
